# Optimizing a Trainium2 kernel written in Bass

```python
import jax, jax.numpy as jnp
from jax import lax
import numpy as np

D_MODEL = 1024
BATCH = 1
SEQ = 16384
DEPTH = 4

N_MIXERS = 2
EXPAND = 2
D_INNER = EXPAND * D_MODEL
M_HEADS = 4
M_HEAD_DIM = D_INNER // M_HEADS
CONV_WIDTH = 4
G_HEADS = 4
G_KEY_DIM = D_INNER // 2
G_KEY_HEAD = G_KEY_DIM // G_HEADS
G_VAL_HEAD = D_INNER // G_HEADS
G_GATE_RANK = 16
G_GATE_TAU = 16.0
G_IN_WIDTH = 2 * G_KEY_DIM + 2 * D_INNER + G_GATE_RANK
CHUNK = 64
NORM_EPS = 1e-6
N_MLSTM_LAYERS = (DEPTH + 1) // 2
N_GLA_LAYERS = DEPTH // 2

kernel_name = 'hybrid_mlstm_gla_sandwich'


def _f32(a):
    return a.astype(jnp.float32)


def rms_norm(x, w):
    xf = _f32(x)
    y = xf * lax.rsqrt(jnp.mean(xf * xf, axis=-1, keepdims=True) + NORM_EPS)
    return (y * _f32(w)).astype(x.dtype)


def to_chunks(a):
    B, T, H = a.shape[:3]
    rest = a.shape[3:]
    a = a.reshape((B, T // CHUNK, CHUNK, H) + rest)
    perm = (1, 0, 3, 2) + tuple(range(4, a.ndim))
    return a.transpose(perm)


def from_chunks(a):
    NC, B, H, L, D = a.shape
    return a.transpose(1, 0, 3, 2, 4).reshape(B, NC * L, H, D)


def causal_depthwise_conv(u, w, b):
    y = lax.conv_general_dilated(u, w[:, None, :], window_strides=(1,),
                                 padding=[(CONV_WIDTH - 1, 0)],
                                 dimension_numbers=('NWC', 'WIO', 'NWC'),
                                 feature_group_count=u.shape[-1])
    return y + b


def mlstm_chunked(q, k, v, log_i, log_f):
    B, T, H, Dk = q.shape
    Dv = v.shape[-1]
    mask = jnp.tril(jnp.ones((CHUNK, CHUNK), dtype=bool))

    def step(carry, xs):
        C, n, m = carry
        qc, kc, vc, ic, fc = xs
        g = jnp.cumsum(fc, axis=-1)
        logw = jnp.where(mask, g[..., :, None] - g[..., None, :] + ic[..., None, :], -jnp.inf)
        log_prev = g + m[..., None]
        m_row = jnp.maximum(jnp.max(logw, axis=-1), log_prev)
        w_intra = jnp.exp(logw - m_row[..., None])
        w_prev = jnp.exp(log_prev - m_row)
        s = jnp.einsum('bhld,bhsd->bhls', qc, kc) * w_intra
        num = (jnp.einsum('bhls,bhsv->bhlv', s, vc)
               + w_prev[..., None] * jnp.einsum('bhvd,bhld->bhlv', C, qc))
        den = jnp.sum(s, axis=-1) + w_prev * jnp.einsum('bhd,bhld->bhl', n, qc)
        h = num / jnp.maximum(jnp.abs(den), jnp.exp(-m_row))[..., None]
        g_last = g[..., -1]
        log_src = g_last[..., None] - g + ic
        m_new = jnp.maximum(g_last + m, jnp.max(log_src, axis=-1))
        decay = jnp.exp(g_last + m - m_new)
        kw = kc * jnp.exp(log_src - m_new[..., None])[..., None]
        C_new = decay[..., None, None] * C + jnp.einsum('bhlv,bhld->bhvd', vc, kw)
        n_new = decay[..., None] * n + jnp.sum(kw, axis=-2)
        return (C_new, n_new, m_new), h

    init = (jnp.zeros((B, H, Dv, Dk), jnp.float32),
            jnp.zeros((B, H, Dk), jnp.float32),
            jnp.zeros((B, H), jnp.float32))
    _, h = lax.scan(step, init, (to_chunks(q), to_chunks(k), to_chunks(v),
                                 to_chunks(log_i), to_chunks(log_f)))
    return from_chunks(h)


def gla_chunked(q, k, v, log_a):
    B, T, H, Dk = q.shape
    Dv = v.shape[-1]
    mask = jnp.tril(jnp.ones((CHUNK, CHUNK), dtype=bool))

    def step(S, xs):
        qc, kc, vc, ac = xs
        G = jnp.cumsum(ac, axis=-2)
        diff = jnp.where(mask[..., None], G[..., :, None, :] - G[..., None, :, :], -jnp.inf)
        A = jnp.einsum('bhld,bhsd,bhlsd->bhls', qc, kc, jnp.exp(diff))
        o = (jnp.einsum('bhls,bhsv->bhlv', A, vc)
             + jnp.einsum('bhld,bhdv->bhlv', qc * jnp.exp(G), S))
        G_last = G[..., -1:, :]
        S_new = (jnp.exp(G_last[..., 0, :])[..., None] * S
                 + jnp.einsum('bhsd,bhsv->bhdv', kc * jnp.exp(G_last - G), vc))
        return S_new, o

    init = jnp.zeros((B, H, Dk, Dv), jnp.float32)
    _, o = lax.scan(step, init, (to_chunks(q), to_chunks(k), to_chunks(v), to_chunks(log_a)))
    return from_chunks(o)


def mlstm_mixer(h, w_in, conv_w, conv_b, wq, wk, wv, w_gate, b_gate, norm_w, skip, w_out):
    B, T, _ = h.shape
    uz = jnp.einsum('btd,de->bte', _f32(h), _f32(w_in))
    u, z = uz[..., :D_INNER], uz[..., D_INNER:]
    uc = jax.nn.silu(causal_depthwise_conv(u, _f32(conv_w), _f32(conv_b)))
    uch = uc.reshape(B, T, M_HEADS, M_HEAD_DIM)
    q = jnp.einsum('bthd,hde->bthe', uch, _f32(wq))
    k = jnp.einsum('bthd,hde->bthe', uch, _f32(wk))
    v = jnp.einsum('bthd,hde->bthe', u.reshape(B, T, M_HEADS, M_HEAD_DIM), _f32(wv))
    wg = _f32(w_gate)
    gates = (jnp.einsum('bte,eg->btg', q.reshape(B, T, D_INNER), wg[:D_INNER])
             + jnp.einsum('bte,eg->btg', k.reshape(B, T, D_INNER), wg[D_INNER:2 * D_INNER])
             + jnp.einsum('bte,eg->btg', v.reshape(B, T, D_INNER), wg[2 * D_INNER:])
             + _f32(b_gate))
    log_i = gates[..., :M_HEADS]
    log_f = jax.nn.log_sigmoid(gates[..., M_HEADS:])
    hh = mlstm_chunked(q, k * (M_HEAD_DIM ** -0.5), v, log_i, log_f)
    mu = jnp.mean(hh, axis=-1, keepdims=True)
    var = jnp.mean(jnp.square(hh - mu), axis=-1, keepdims=True)
    hn = ((hh - mu) * lax.rsqrt(var + NORM_EPS)).reshape(B, T, D_INNER)
    y = (hn * _f32(norm_w) + _f32(skip) * uc) * jax.nn.silu(z)
    return jnp.einsum('bte,ed->btd', y, _f32(w_out))


def gla_mixer(h, w_in, w_gate_up, b_gate, norm_w, w_out):
    B, T, _ = h.shape
    p = jnp.einsum('btd,de->bte', _f32(h), _f32(w_in))
    q = p[..., :G_KEY_DIM]
    k = p[..., G_KEY_DIM:2 * G_KEY_DIM]
    v = p[..., 2 * G_KEY_DIM:2 * G_KEY_DIM + D_INNER]
    z = p[..., 2 * G_KEY_DIM + D_INNER:2 * G_KEY_DIM + 2 * D_INNER]
    r = p[..., 2 * G_KEY_DIM + 2 * D_INNER:]
    log_a = jax.nn.log_sigmoid(jnp.einsum('btr,rk->btk', r, _f32(w_gate_up)) + _f32(b_gate)) / G_GATE_TAU
    o = gla_chunked((q * (G_KEY_HEAD ** -0.5)).reshape(B, T, G_HEADS, G_KEY_HEAD),
                    k.reshape(B, T, G_HEADS, G_KEY_HEAD),
                    v.reshape(B, T, G_HEADS, G_VAL_HEAD),
                    log_a.reshape(B, T, G_HEADS, G_KEY_HEAD))
    on = o * lax.rsqrt(jnp.mean(o * o, axis=-1, keepdims=True) + NORM_EPS)
    y = on.reshape(B, T, D_INNER) * _f32(norm_w) * jax.nn.silu(z)
    return jnp.einsum('bte,ed->btd', y, _f32(w_out))


def setup_inputs(seed: int = 0) -> dict:
    key = jax.random.key(seed)
    ks = jax.random.split(key, 24)
    nA, nB, E, D = N_MLSTM_LAYERS, N_GLA_LAYERS, D_INNER, D_MODEL

    def nrm(k, shape, scale=1.0):
        return jax.random.normal(k, shape, jnp.float32) * scale

    f_bias = jnp.linspace(3.0, 6.0, M_HEADS, dtype=jnp.float32)[None, :] + nrm(ks[10], (nA, M_HEADS), 0.1)
    i_bias = nrm(ks[11], (nA, M_HEADS), 0.1)
    return {
        'x': nrm(ks[0], (BATCH, SEQ, D)),
        'pre_norm_w': 1.0 + nrm(ks[1], (DEPTH, D), 0.02),
        'post_norm_w': 1.0 + nrm(ks[2], (DEPTH, D), 0.02),
        'm_w_in': nrm(ks[3], (nA, D, 2 * E), D ** -0.5),
        'm_conv_w': nrm(ks[4], (nA, CONV_WIDTH, E), CONV_WIDTH ** -0.5),
        'm_conv_b': nrm(ks[5], (nA, E), 0.02),
        'm_wq': nrm(ks[6], (nA, M_HEADS, M_HEAD_DIM, M_HEAD_DIM), M_HEAD_DIM ** -0.5),
        'm_wk': nrm(ks[7], (nA, M_HEADS, M_HEAD_DIM, M_HEAD_DIM), M_HEAD_DIM ** -0.5),
        'm_wv': nrm(ks[8], (nA, M_HEADS, M_HEAD_DIM, M_HEAD_DIM), M_HEAD_DIM ** -0.5),
        'm_w_gate': nrm(ks[9], (nA, 3 * E, 2 * M_HEADS), 0.1 * (3 * E) ** -0.5),
        'm_b_gate': jnp.concatenate([i_bias, f_bias], axis=-1),
        'm_norm_w': 1.0 + nrm(ks[12], (nA, E), 0.02),
        'm_skip': 1.0 + nrm(ks[13], (nA, E), 0.02),
        'm_w_out': nrm(ks[14], (nA, E, D), E ** -0.5),
        'g_w_in': nrm(ks[15], (nB, D, G_IN_WIDTH), D ** -0.5),
        'g_w_gate_up': nrm(ks[16], (nB, G_GATE_RANK, G_KEY_DIM), G_GATE_RANK ** -0.5),
        'g_b_gate': nrm(ks[17], (nB, G_KEY_DIM), 0.1),
        'g_norm_w': 1.0 + nrm(ks[18], (nB, E), 0.02),
        'g_w_out': nrm(ks[19], (nB, E, D), E ** -0.5),
    }


def reference(x, pre_norm_w, post_norm_w,
              m_w_in, m_conv_w, m_conv_b, m_wq, m_wk, m_wv, m_w_gate, m_b_gate,
              m_norm_w, m_skip, m_w_out,
              g_w_in, g_w_gate_up, g_b_gate, g_norm_w, g_w_out):
    for i in range(DEPTH):
        h = rms_norm(x, pre_norm_w[i])
        j = i // N_MIXERS
        if i % N_MIXERS == 0:
            out = mlstm_mixer(h, m_w_in[j], m_conv_w[j], m_conv_b[j], m_wq[j], m_wk[j], m_wv[j],
                              m_w_gate[j], m_b_gate[j], m_norm_w[j], m_skip[j], m_w_out[j])
        else:
            out = gla_mixer(h, g_w_in[j], g_w_gate_up[j], g_b_gate[j], g_norm_w[j], g_w_out[j])
        x = x + rms_norm(out, post_norm_w[i]).astype(x.dtype)
    return x
```

```python
import math
from contextlib import ExitStack

import ml_dtypes
import numpy as np

import concourse.bass as bass
import concourse.mybir as mybir
from concourse.bass_utils import run_bass_kernel_spmd

F32 = mybir.dt.float32
BF16 = mybir.dt.bfloat16
AF = mybir.ActivationFunctionType
ALU = mybir.AluOpType
AX = mybir.AxisListType

NCORES = 8
SEQ = 16384
TL = SEQ // NCORES
NT = TL // 128
D = 1024
KD = D // 128
E = 2048
NFT = E // 128
H = 4
DH = 512
GK = 256
NG = 8
EPS = 1e-6
TAU = 16.0

GSC_M = 5 * 64
NVM = 8 + 64 + 16 + 16 + 16
NVG = 8 + 8 + 16
CST_W = 128 + 512 + 128 + 8


class Prog:
    ENGS = ['pe', 'act', 'dve', 'pool', 'sp']
    NDS = 8

    def __init__(self, nc):
        self.nc = nc
        self.q = {e: [] for e in self.ENGS}
        self.last_w = {}
        self.readers = {}
        self.ndma = {e: 0 for e in self.ENGS}
        self.dma_ops = {e: [] for e in self.ENGS}
        self.pending_bar = {}

    def add(self, eng, fn, reads=(), writes=(), dma=False):
        op = dict(eng=eng, fn=fn, idx=len(self.q[eng]), dma=dma, sig=False, depo={})
        depo = op['depo']
        for b in reads:
            w = self.last_w.get(b)
            if w is not None:
                depo[id(w)] = w
        for b in writes:
            w = self.last_w.get(b)
            if w is not None:
                depo[id(w)] = w
            for r in self.readers.get(b, ()):
                depo[id(r)] = r
        bar = self.pending_bar.pop(eng, None)
        if bar:
            for d in bar:
                depo[id(d)] = d
        if dma:
            k = self.ndma[eng]
            self.ndma[eng] += 1
            op['dk'] = k
            if k >= self.NDS:
                prev = self.dma_ops[eng][k - self.NDS]
                depo[id(prev)] = prev
            self.dma_ops[eng].append(op)
        for b in reads:
            self.readers.setdefault(b, []).append(op)
        for b in writes:
            self.last_w[b] = op
            self.readers[b] = []
        self.q[eng].append(op)
        return op

    def barrier(self):
        ops = []
        for e in self.ENGS:
            comp = [o for o in self.q[e] if not o['dma']]
            if comp:
                ops.append(comp[-1])
            ops += self.dma_ops[e][-self.NDS:]
        for e in self.ENGS:
            self.pending_bar[e] = list(ops)

    def emit(self, final_wait_ops=()):
        nc = self.nc
        for e in self.ENGS:
            for op in self.q[e]:
                need = []
                best = {}
                for d in op['depo'].values():
                    if d is op:
                        continue
                    if d['dma']:
                        need.append(d)
                    else:
                        if d['eng'] == e and (e == 'pe' or d['idx'] > op['idx']):
                            continue
                        if d['eng'] not in best or best[d['eng']]['idx'] < d['idx']:
                            best[d['eng']] = d
                need += list(best.values())
                for d in need:
                    d['sig'] = True
                op['need'] = need
                op['depo'] = None
        for e in self.ENGS:
            c = 0
            for op in self.q[e]:
                if op['dma']:
                    op['semslot'] = op['dk'] % self.NDS
                    op['semval'] = 16 * (op['dk'] // self.NDS + 1)
                elif op['sig']:
                    c += 1
                    op['semval'] = c
        with ExitStack() as st:
            csem = {e: st.enter_context(nc.semaphore(f"c_{e}")) for e in self.ENGS}
            dsem = {e: ([st.enter_context(nc.semaphore(f"d_{e}{i}")) for i in range(self.NDS)]
                        if self.ndma[e] else []) for e in self.ENGS}
            block = st.enter_context(nc.Block())
            handles = {'pe': block.tensor, 'act': block.scalar, 'dve': block.vector,
                       'pool': block.gpsimd, 'sp': block.sync}

            def mk(e):
                def body(eng):
                    waited = {}
                    for op in self.q[e]:
                        for d in op['need']:
                            if d['dma']:
                                key = ('d', d['eng'], d['semslot'])
                                sem = dsem[d['eng']][d['semslot']]
                            else:
                                key = ('c', d['eng'])
                                sem = csem[d['eng']]
                            if waited.get(key, 0) >= d['semval']:
                                continue
                            eng.wait_ge(sem, d['semval'])
                            waited[key] = d['semval']
                        ins = op['fn'](eng)
                        if op['dma']:
                            ins.then_inc(dsem[e][op['semslot']], 16)
                        elif op['sig']:
                            ins.then_inc(csem[e], 1)
                    if e == 'sp':
                        for d in final_wait_ops:
                            if d['dma']:
                                eng.wait_ge(dsem[d['eng']][d['semslot']], d['semval'])
                            else:
                                eng.wait_ge(csem[d['eng']], d['semval'])
                return body

            for e in self.ENGS:
                if self.q[e] or e == 'sp':
                    handles[e](mk(e))


class Arena:
    def __init__(self, t, nwords):
        self.t = t
        self.cap = nwords
        self.off = 0

    def alloc(self, free_shape, dtype):
        n = 1
        for s in free_shape:
            n *= s
        nbytes = n * (4 if dtype == F32 else 2)
        words = (nbytes + 3) // 4
        words = (words + 15) // 16 * 16
        off = self.off
        self.off += words
        assert self.off <= self.cap, f"arena overflow {self.off * 4} > {self.cap * 4}"
        v = self.t[:, off:off + (nbytes + 3) // 4]
        if dtype != F32:
            v = v.bitcast(dtype)
        if len(free_shape) == 2:
            v = v.rearrange("p (a b) -> p a b", a=free_shape[0], b=free_shape[1])
        elif len(free_shape) == 3:
            v = v.rearrange("p (a b c) -> p a b c", a=free_shape[0], b=free_shape[1], c=free_shape[2])
        return v

    def mark(self):
        return self.off

    def release(self, m):
        self.off = m


class KB:
    def __init__(self, nc, stages):
        self.nc = nc
        self.P = Prog(nc)
        self.stages = stages
        self.outs = []
        self.rr = 0

    def dma(self, eng, out, in_, reads=(), writes=()):
        return self.P.add(eng, lambda e: e.dma_start(out=out, in_=in_), reads, writes, dma=True)

    def mm(self, out, pairs, reads=(), writes=()):
        n = len(pairs)

        def fn(e):
            ins = None
            for i, (l, r) in enumerate(pairs):
                ins = e.matmul(out, lhsT=l, rhs=r, start=(i == 0), stop=(i == n - 1))
            return ins
        return self.P.add('pe', fn, reads, writes)

    def mms(self, groups, reads=(), writes=()):
        def fn(e):
            ins = None
            for out, pairs in groups:
                n = len(pairs)
                for i, (l, r) in enumerate(pairs):
                    ins = e.matmul(out, lhsT=l, rhs=r, start=(i == 0), stop=(i == n - 1))
            return ins
        return self.P.add('pe', fn, reads, writes)

    def transposes(self, items, ident, reads=(), writes=()):
        def fn(e):
            ins = None
            for out, in_ in items:
                ins = e.transpose(out, in_, ident)
            return ins
        return self.P.add('pe', fn, reads, writes)

    def act(self, out, in_, func, reads=(), writes=(), bias=None, scale=None, accum=None):
        kw = {}
        if bias is not None:
            kw['bias'] = bias
        if scale is not None:
            kw['scale'] = scale
        if accum is not None:
            kw['accum_out'] = accum
        return self.P.add('act', lambda e: e.activation(out=out, in_=in_, func=func, **kw), reads, writes)

    def ts(self, eng, out, in0, s1, s2, op0, op1=None, reads=(), writes=()):
        if op1 is None:
            return self.P.add(eng, lambda e: e.tensor_scalar(out=out, in0=in0, scalar1=s1, scalar2=None, op0=op0),
                              reads, writes)
        return self.P.add(eng, lambda e: e.tensor_scalar(out=out, in0=in0, scalar1=s1, scalar2=s2, op0=op0, op1=op1),
                          reads, writes)

    def tt(self, eng, out, in0, in1, op, reads=(), writes=()):
        return self.P.add(eng, lambda e: e.tensor_tensor(out=out, in0=in0, in1=in1, op=op), reads, writes)

    def stt(self, out, in0, scalar, in1, op0, op1, reads=(), writes=()):
        return self.P.add('dve', lambda e: e.scalar_tensor_tensor(out=out, in0=in0, scalar=scalar, in1=in1,
                                                                  op0=op0, op1=op1), reads, writes)

    def cp(self, eng, out, in_, reads=(), writes=()):
        if eng == 'act':
            return self.P.add('act', lambda e: e.copy(out=out, in_=in_), reads, writes)
        return self.P.add(eng, lambda e: e.tensor_copy(out=out, in_=in_), reads, writes)

    def memset(self, eng, ap, val, writes=()):
        return self.P.add(eng, lambda e: e.memset(ap, val), (), writes)

    def evac(self, out, in_, reads=(), writes=()):
        self.rr += 1
        return self.cp('act' if self.rr % 2 else 'dve', out, in_, reads, writes)

    def wstream(self, bufs, keys, srcs):
        kb = self

        class WS:
            nxt = 0

            def start(ws, i):
                while ws.nxt <= i + len(bufs) - 1 and ws.nxt < len(srcs):
                    j = ws.nxt
                    kb.dma('pool', bufs[j % len(bufs)], srcs[j].rearrange("(k p) c -> p k c", p=128),
                           writes=[keys[j % len(bufs)]])
                    ws.nxt += 1

            def get(ws, i):
                return bufs[i % len(bufs)], keys[i % len(bufs)]
        return WS()

    def dram(self, name, shape, dtype, kind):
        t = self.nc.dram_tensor(name, list(shape), dtype, kind=kind)
        return t.ap()

    def build(self):
        nc = self.nc
        with ExitStack() as st:
            arena_t = st.enter_context(nc.sbuf_tensor("arena", [128, 53000], F32))
            self.A = Arena(arena_t, 53000)
            self.ps = [st.enter_context(nc.psum_tensor(f"ps{i}", [128, 512], F32)) for i in range(8) if i != 1]
            self.ps.insert(1, None)
            self.psb = st.enter_context(nc.psum_tensor("psb", [128, 1024], BF16))
            A = self.A
            self.cst = A.alloc([CST_W], F32)
            self.ident = self.cst[:, 0:128]
            self.tri4 = self.cst[:, 128:640]
            self.tri = self.cst[:, 128:256]
            self.ones = self.cst[:, 640:768]
            self.sel = self.cst[:, 768:776]
            self.identb = A.alloc([128], BF16)
            self.onesb = A.alloc([128], BF16)
            self.x = A.alloc([NT, D], F32)
            cst_d = self.dram("cst", [128, CST_W], F32, "ExternalInput")
            x_d = self.dram("x", [TL, D], F32, "ExternalInput")
            self.dma('sp', self.cst, cst_d, writes=['cst'])
            self.cp('dve', self.identb, self.ident, reads=['cst'], writes=['identb'])
            self.cp('dve', self.onesb, self.ones, reads=['cst'], writes=['onesb'])
            for t in range(NT):
                self.dma('sp', self.x[:, t, :], x_d[t * 128:(t + 1) * 128, :], writes=[f'x{t}'])
            final = []
            for si, (kind, mix) in enumerate(self.stages):
                pre = f"s{si}_"
                self.stage_tag = [pre]
                m = A.mark()
                if kind == 'A' and mix == 'm':
                    self.stage_A_m(pre)
                elif kind == 'B' and mix == 'm':
                    final += self.stage_B(pre, 'm')
                elif kind == 'A' and mix == 'g':
                    self.stage_A_g(pre)
                else:
                    final += self.stage_B(pre, 'g')
                A.release(m)
                self.P.barrier()
            final += self.outs
            self.P.emit(final_wait_ops=final)

    def prenorm(self, hT, pnw, xtiles, ntiles, tagp, veck):
        A = self.A
        ss = A.alloc([ntiles], F32)
        rstd = A.alloc([ntiles], F32)
        if not hasattr(self, '_pn_tmp') or self._pn_tmp[0] != id(self.stage_tag):
            self._pn_tmp = (id(self.stage_tag), A.alloc([D], BF16), [A.alloc([D], F32) for _ in range(2)])
        junk = self._pn_tmp[1]
        xs = self._pn_tmp[2]
        pk = self.stage_tag[0]
        for t, (xa, key) in enumerate(xtiles):
            self.act(junk, xa, AF.Square, reads=[key], writes=[pk + 'junk', tagp + 'ss'], accum=ss[:, t:t + 1])
        self.ts('dve', rstd, ss, 1.0 / D, EPS, ALU.mult, ALU.add, reads=[tagp + 'ss'], writes=[tagp + 'rstd'])
        self.act(rstd, rstd, AF.Sqrt, reads=[tagp + 'rstd'], writes=[tagp + 'rstd'])
        self.P.add('dve', lambda e: e.reciprocal(out=rstd, in_=rstd), [tagp + 'rstd'], [tagp + 'rstd'])
        for t, (xa, key) in enumerate(xtiles):
            xst = xs[t % 2]
            self.ts('dve', xst, xa, rstd[:, t:t + 1], None, ALU.mult, reads=[key, tagp + 'rstd'],
                    writes=[pk + f'xs{t % 2}'])
            for half in range(2):
                bank = self.ps[0] if half == 0 else self.ps[7]
                bk = 'ps0' if half == 0 else 'ps7'
                self.transposes([(bank[:, i * 128:(i + 1) * 128], xst[:, (half * 4 + i) * 128:(half * 4 + i + 1) * 128])
                                 for i in range(4)], self.ident, reads=[pk + f'xs{t % 2}', 'cst'], writes=[bk])
                for i in range(4):
                    k = half * 4 + i
                    o = hT[:, k, t * 128:(t + 1) * 128]
                    if i % 2 == 0:
                        self.act(o, bank[:, i * 128:(i + 1) * 128], AF.Copy, reads=[bk, veck],
                                 writes=[tagp + f'hT{t}'], scale=pnw[:, k:k + 1])
                    else:
                        self.ts('dve', o, bank[:, i * 128:(i + 1) * 128], pnw[:, k:k + 1], None, ALU.mult,
                                reads=[bk, veck], writes=[tagp + f'hT{t}'])

    def stage_A_m(self, pre):
        A = self.A
        P = self.P
        ps = self.ps
        IN, OUT = "ExternalInput", "ExternalOutput"
        w_in = self.dram(pre + "w_in", [D, 2 * E], F32, IN)
        wq_d = self.dram(pre + "wq", [H, DH, DH], F32, IN)
        wk_d = self.dram(pre + "wk", [H, DH, DH], F32, IN)
        wv_d = self.dram(pre + "wv", [H, DH, DH], F32, IN)
        wvT_d = self.dram(pre + "wvT", [H, DH, DH], F32, IN)
        wg_d = self.dram(pre + "wg", [128, 48 * 8], F32, IN)
        bg_d = self.dram(pre + "bg", [1, 8], F32, IN)
        vec_d = self.dram(pre + "vec", [128, NVM], F32, IN)
        xh_d = self.dram(pre + "xh", [128, D], F32, IN)
        qT_d = self.dram(pre + "qT", [NT, 128, NFT * 128], BF16, OUT)
        kT_d = self.dram(pre + "kT", [NT, 128, NFT * 128], BF16, OUT)
        v_d = self.dram(pre + "v", [NT, 128, E], BF16, OUT)
        sz_d = self.dram(pre + "sz", [NT, 128, NFT * 128], BF16, OUT)
        uc_d = self.dram(pre + "uc", [NT, 128, NFT * 128], BF16, OUT)
        gsc_d = self.dram(pre + "gsc", [128, GSC_M], F32, OUT)
        sumS_d = self.dram(pre + "sumS", [NFT * 128, DH], F32, OUT)
        sumn_d = self.dram(pre + "sumn", [128, 20], F32, OUT)

        vec = A.alloc([NVM], F32)
        pnw = vec[:, 0:8]
        cw = vec[:, 8:72]
        cb = vec[:, 72:88]
        self.dma('sp', vec, vec_d, writes=[pre + 'vec'])
        wg = A.alloc([48 * 8], BF16)
        self.dma('pool', wg, wg_d, writes=[pre + 'wg'])
        bgb = A.alloc([NT, 8], F32)
        self.dma('sp', bgb, bg_d.partition_broadcast(128).broadcast_to([128, NT, 8]) if False else
                 bass.AP(bg_d.tensor, 0, [[0, 128], [0, NT], [1, 8]]), writes=[pre + 'bgb'])
        gacc = A.alloc([NT, 8], F32)
        self.memset('dve', gacc, 0.0, writes=[pre + 'gacc'])
        wvg = A.alloc([128], BF16)
        gsc = A.alloc([GSC_M], F32)
        kap = gsc[:, 0:64]
        kap2 = gsc[:, 64:128]
        invlam = gsc[:, 128:192]
        rho = gsc[:, 192:256]
        rhon = gsc[:, 256:320]
        G = A.alloc([NT, 8], F32)
        sp = A.alloc([NT, 4], F32)
        cum = A.alloc([64], F32)
        tot = A.alloc([64], F32)
        totn = A.alloc([64], F32)
        e1 = A.alloc([64], F32)
        e2 = A.alloc([64], F32)
        sumn = A.alloc([20], F32)
        mP1 = A.mark()
        hT = A.alloc([KD, TL], BF16)
        hTh = A.alloc([KD, 128], BF16)
        xh = A.alloc([D], F32)
        stg = [A.alloc([TL], BF16) for _ in range(2)]

        wvT = stg[0][:].rearrange("p (a b) -> p a b", a=4)
        for h in range(H):
            self.dma('pool', wvT, wvT_d[h].rearrange("(t p) d -> p t d", p=128), writes=[pre + 'stg0'])
            groups = []
            for dt in range(4):
                groups.append((ps[2][:, (h * 4 + dt) * 8:(h * 4 + dt + 1) * 8],
                               [(wvT[:, et, dt * 128:(dt + 1) * 128], wg[:, (32 + h * 4 + et) * 8:(32 + h * 4 + et + 1) * 8])
                                for et in range(4)]))
            self.mms(groups, reads=[pre + 'stg0', pre + 'wg'], writes=['ps2'])
        self.cp('dve', wvg, ps[2][:, 0:128], reads=['ps2'], writes=[pre + 'wvg'])

        self.dma('sp', xh, xh_d, writes=[pre + 'xh'])
        self.prenorm(hT, pnw, [(self.x[:, t, :], f'x{t}') for t in range(NT)], NT, pre, pre + 'vec')
        self.prenorm(hTh, pnw, [(xh, pre + 'xh')], 1, pre + 'h', pre + 'vec')
        hkeys = [pre + f'hT{t}' for t in range(NT)]

        wblk = [A.alloc([KD, 512], BF16) for _ in range(2)]
        wq = A.alloc([4, DH], BF16)
        wk = A.alloc([4, DH], BF16)
        wv = A.alloc([4, DH], BF16)
        ucT = A.alloc([4, TL], BF16)
        uT = A.alloc([4, TL], BF16)
        uf = A.alloc([TL + 3], F32)
        acc = [A.alloc([512], F32) for _ in range(2)]
        vst = [A.alloc([512], BF16) for _ in range(2)]
        nblk = 0
        nstg = 0
        nv = 0
        bank_rr = 0

        def next_bank():
            nonlocal bank_rr
            b = 3 + bank_rr % 4
            bank_rr += 1
            return b

        ws = self.wstream(wblk, [pre + 'wblk0', pre + 'wblk1'],
                          [w_in[:, i * 512:(i + 1) * 512] for i in range(8)])
        self.dma('pool', wq, wq_d[0].rearrange("(k p) e -> p k e", p=128), writes=[pre + 'wq'])
        self.dma('pool', wk, wk_d[0].rearrange("(k p) e -> p k e", p=128), writes=[pre + 'wk'])
        self.dma('pool', wv, wv_d[0].rearrange("(k p) e -> p k e", p=128), writes=[pre + 'wv'])
        for h in range(H):
            ws.start(h)
            wb, wbk = ws.get(h)
            for ft in range(4):
                ftg = h * 4 + ft
                self.mm(ps[2][:, 128 + ftg * 4:128 + ftg * 4 + 3],
                        [(wb[:, k, ft * 128:(ft + 1) * 128], hTh[:, k, 0:3]) for k in range(KD)],
                        reads=[wbk, pre + 'hhT0'], writes=['ps2'])
                self.cp('act', uf[:, 0:3], ps[2][:, 128 + ftg * 4:128 + ftg * 4 + 3], reads=['ps2'],
                        writes=[pre + 'uf_h'])
                for st4 in range(4):
                    b = next_bank()
                    bk = f'ps{b}'
                    self.mm(ps[b][:], [(wb[:, k, ft * 128:(ft + 1) * 128], hT[:, k, st4 * 512:(st4 + 1) * 512])
                                       for k in range(KD)],
                            reads=[wbk] + hkeys[st4 * 4:(st4 + 1) * 4], writes=[bk])
                    self.cp('act', uf[:, 3 + st4 * 512:3 + (st4 + 1) * 512], ps[b][:], reads=[bk],
                            writes=[pre + f'uf{st4}'])
                    self.cp('dve', uT[:, ft, st4 * 512:(st4 + 1) * 512], uf[:, 3 + st4 * 512:3 + (st4 + 1) * 512],
                            reads=[pre + f'uf{st4}'], writes=[pre + f'uT{st4}'])
                    a = acc[st4 % 2]
                    ak = pre + f'acc{st4 % 2}'
                    rk = [pre + f'uf{st4}', pre + (f'uf{st4 - 1}' if st4 else 'uf_h'), pre + 'vec']
                    o0 = st4 * 512
                    self.ts('dve', a, uf[:, o0:o0 + 512], cw[:, ftg:ftg + 1], None, ALU.mult, reads=rk, writes=[ak])
                    for kk in range(1, 4):
                        self.stt(a, uf[:, o0 + kk:o0 + kk + 512], cw[:, kk * 16 + ftg:kk * 16 + ftg + 1], a,
                                 ALU.mult, ALU.add, reads=rk + [ak], writes=[ak])
                    self.act(ucT[:, ft, st4 * 512:(st4 + 1) * 512], a, AF.Silu, reads=[ak, pre + 'vec'],
                             writes=[pre + f'ucT{st4}'], bias=cb[:, ftg:ftg + 1])
                self.dma('pool', uc_d[:, :, ftg * 128:(ftg + 1) * 128].rearrange("c p t -> p c t"),
                         ucT[:, ft, :].rearrange("p (c t) -> p c t", t=128),
                         reads=[pre + f'ucT{s}' for s in range(4)], writes=[pre + 'uc_d'])
            uck = [pre + f'ucT{s}' for s in range(4)]
            utk = [pre + f'uT{s}' for s in range(4)]
            for which, wsb, wkey, dst, goff in ((0, wq, pre + 'wq', qT_d, 0), (1, wk, pre + 'wk', kT_d, 16)):
                for et in range(4):
                    sg = stg[nstg % 2]
                    sk = pre + f'stg{nstg % 2}'
                    nstg += 1
                    for st4 in range(4):
                        b = next_bank()
                        bk = f'ps{b}'
                        self.mm(ps[b][:], [(wsb[:, k, et * 128:(et + 1) * 128], ucT[:, k, st4 * 512:(st4 + 1) * 512])
                                           for k in range(4)], reads=[wkey, uck[st4]], writes=[bk])
                        self.evac(sg[:, st4 * 512:(st4 + 1) * 512], ps[b][:], reads=[bk], writes=[sk])
                    tile_i = h * 4 + et
                    self.dma('pool', dst[:, :, tile_i * 128:(tile_i + 1) * 128].rearrange("c p t -> p c t"),
                             sg[:].rearrange("p (c t) -> p c t", t=128), reads=[sk], writes=[pre + f'qk_d{which}'])
                    gt = goff + tile_i
                    self.mms([(ps[2][:, tt * 8:(tt + 1) * 8], [(sg[:, tt * 128:(tt + 1) * 128], wg[:, gt * 8:(gt + 1) * 8])])
                              for tt in range(NT)], reads=[sk, pre + 'wg'], writes=['ps2'])
                    self.tt('dve', gacc[:].rearrange("p a b -> p (a b)"), gacc[:].rearrange("p a b -> p (a b)"),
                            ps[2][:, 0:128], ALU.add, reads=['ps2', pre + 'gacc'], writes=[pre + 'gacc'])
            if h + 1 < H:
                self.dma('pool', wq, wq_d[h + 1].rearrange("(k p) e -> p k e", p=128), writes=[pre + 'wq'])
                self.dma('pool', wk, wk_d[h + 1].rearrange("(k p) e -> p k e", p=128), writes=[pre + 'wk'])
            for tt in range(NT):
                b = next_bank()
                bk = f'ps{b}'
                self.mm(ps[b][:], [(uT[:, k, tt * 128:(tt + 1) * 128], wv[:, k, :]) for k in range(4)],
                        reads=[pre + 'wv', utk[tt // 4]], writes=[bk])
                vs = vst[nv % 2]
                vk = pre + f'vst{nv % 2}'
                nv += 1
                self.evac(vs, ps[b][:], reads=[bk], writes=[vk])
                self.dma('pool', v_d[tt, :, h * 512:(h + 1) * 512], vs, reads=[vk], writes=[pre + 'v_d'])
            if h + 1 < H:
                self.dma('pool', wv, wv_d[h + 1].rearrange("(k p) e -> p k e", p=128), writes=[pre + 'wv'])
            self.mms([(ps[2][:, tt * 8:(tt + 1) * 8],
                       [(uT[:, k, tt * 128:(tt + 1) * 128], wvg[:, (h * 4 + k) * 8:(h * 4 + k + 1) * 8]) for k in range(4)])
                      for tt in range(NT)], reads=utk + [pre + 'wvg'], writes=['ps2'])
            self.tt('dve', gacc[:].rearrange("p a b -> p (a b)"), gacc[:].rearrange("p a b -> p (a b)"),
                    ps[2][:, 0:128], ALU.add, reads=['ps2', pre + 'gacc'], writes=[pre + 'gacc'])

        for zb in range(4):
            ws.start(4 + zb)
            wb, wbk = ws.get(4 + zb)
            for ft in range(4):
                sg = stg[nstg % 2]
                sk = pre + f'stg{nstg % 2}'
                nstg += 1
                for st4 in range(4):
                    b = next_bank()
                    bk = f'ps{b}'
                    self.mm(ps[b][:], [(wb[:, k, ft * 128:(ft + 1) * 128], hT[:, k, st4 * 512:(st4 + 1) * 512])
                                       for k in range(KD)], reads=[wbk] + hkeys[st4 * 4:(st4 + 1) * 4], writes=[bk])
                    self.act(sg[:, st4 * 512:(st4 + 1) * 512], ps[b][:], AF.Silu, reads=[bk], writes=[sk])
                tile_i = zb * 4 + ft
                self.dma('pool', sz_d[:, :, tile_i * 128:(tile_i + 1) * 128].rearrange("c p t -> p c t"),
                         sg[:].rearrange("p (c t) -> p c t", t=128), reads=[sk], writes=[pre + 'sz_d'])

        gk = pre + 'gs'
        self.tt('dve', G, gacc, bgb, ALU.add, reads=[pre + 'gacc', pre + 'bgb'], writes=[gk])
        self.act(sp, G[:, :, 4:8], AF.Exp, reads=[gk], writes=[gk], scale=-1.0)
        self.act(sp, sp, AF.Ln, reads=[gk], writes=[gk], bias=1.0)
        spf = sp[:].rearrange("p a b -> p (a b)")
        self.mms([(ps[2][:, c * 4:(c + 1) * 4], [(self.tri, spf[:, c * 4:(c + 1) * 4])]) for c in range(NT)] +
                 [(ps[2][:, 64 + c * 4:64 + (c + 1) * 4], [(self.ones, spf[:, c * 4:(c + 1) * 4])]) for c in range(NT)],
                 reads=[gk, 'cst'], writes=['ps2'])
        self.cp('dve', cum, ps[2][:, 0:64], reads=['ps2'], writes=[gk])
        self.cp('dve', tot, ps[2][:, 64:128], reads=['ps2'], writes=[gk])
        self.memset('dve', totn, 0.0, writes=[gk])
        self.cp('dve', totn[:, 0:60], tot[:, 4:64], reads=[gk], writes=[gk])
        li = G[:, :, 0:4]
        e1v = e1[:].rearrange("p (a b) -> p a b", b=4)
        self.tt('dve', e1v, li, cum[:].rearrange("p (a b) -> p a b", b=4), ALU.add, reads=[gk], writes=[gk])
        self.tt('dve', e1, e1, tot, ALU.subtract, reads=[gk], writes=[gk])
        lnsc = math.log(DH ** -0.5)
        self.act(kap, e1, AF.Exp, reads=[gk], writes=[gk], bias=lnsc)
        self.tt('dve', e2, e1, totn, ALU.subtract, reads=[gk], writes=[gk])
        self.act(kap2, e2, AF.Exp, reads=[gk], writes=[gk], bias=lnsc)
        self.tt('dve', e2, cum, tot, ALU.subtract, reads=[gk], writes=[gk])
        self.act(invlam, e2, AF.Exp, reads=[gk], writes=[gk])
        self.act(rho, tot, AF.Exp, reads=[gk], writes=[gk], scale=-1.0)
        self.act(rhon, totn, AF.Exp, reads=[gk], writes=[gk], scale=-1.0)
        self.P.add('dve', lambda e: e.tensor_reduce(out=sumn[:, 16:20], in_=tot[:].rearrange("p (c h) -> p h c", h=4),
                                                    axis=AX.X, op=ALU.add), [gk], [pre + 'sumn'])
        self.outs.append(self.dma('sp', gsc_d, gsc, reads=[gk], writes=[pre + 'gsc_d']))

        A.release(mP1)
        self.P.barrier()
        S = A.alloc([NFT, DH], F32)
        nst = sumn[:, 0:16]
        self.memset('dve', S, 0.0, writes=[pre + f'S{i}' for i in range(NFT)])
        self.memset('dve', nst, 0.0, writes=[pre + 'sumn'])
        self.scan_m(pre, kT_d, v_d, None, gsc, S, None, nst, None, summary_only=True)
        for i in range(NFT):
            self.outs.append(self.dma('sp', sumS_d[i * 128:(i + 1) * 128, :], S[:, i, :], reads=[pre + f'S{i}'],
                                      writes=[pre + 'sumS_d']))
        self.outs.append(self.dma('sp', sumn_d, sumn, reads=[pre + 'sumn'], writes=[pre + 'sumn_d']))

    def scan_m(self, pre, kT_d, v_d, qT_d, gsc, S, Sb, nst, nb, summary_only, extra=None):
        A = self.A
        ps = self.ps
        kap = gsc[:, 0:64]
        kap2 = gsc[:, 64:128]
        invlam = gsc[:, 128:192]
        rhon = gsc[:, 256:320]
        gk = pre + 'gs'
        kc = [A.alloc([NFT, 128], BF16) for _ in range(2)]
        vc = [A.alloc([E], BF16) for _ in range(2)]
        kt = [A.alloc([E], BF16) for _ in range(2)]
        if not summary_only:
            qc = [A.alloc([NFT, 128], BF16) for _ in range(2)]
            sTb = [A.alloc([512], BF16) for _ in range(2)]
            hn = [A.alloc([DH], F32) for _ in range(2)]
            sm = A.alloc([H, 16], F32)
            (szc, ucc, yc, y_d, nw, skp) = extra
        dsr = 0
        pending = None
        for c in range(NT):
            s = c % 2
            kck, vck, ktk = pre + f'kc{s}', pre + f'vc{s}', pre + f'kt{s}'
            self.dma('sp', kc[s][:].rearrange("p a b -> p (a b)"), kT_d[c], reads=[pre + 'qk_d1'], writes=[kck])
            self.dma('sp', vc[s], v_d[c], reads=[pre + 'v_d'], writes=[vck])
            if not summary_only:
                qck = pre + f'qc{s}'
                self.dma('sp', qc[s][:].rearrange("p a b -> p (a b)"), qT_d[c], reads=[pre + 'qk_d0'], writes=[qck])
                self.dma('sp', szc[s][:].rearrange("p a b -> p (a b)"), extra_sz(self, pre)[c], reads=[pre + 'sz_d'],
                         writes=[pre + f'szc{s}'])
                self.dma('sp', ucc[s][:].rearrange("p a b -> p (a b)"), extra_uc(self, pre)[c], reads=[pre + 'uc_d'],
                         writes=[pre + f'ucc{s}'])
                self.mms([(ps[0][:, h * 128:(h + 1) * 128],
                           [(kc[s][:, h * 4 + dt, :], qc[s][:, h * 4 + dt, :]) for dt in range(4)]) for h in range(H)],
                         reads=[kck, qck], writes=['ps0'])
                for h in range(H):
                    self.stt(sTb[s][:, h * 128:(h + 1) * 128], ps[0][:, h * 128:(h + 1) * 128],
                             kap[:, c * 4 + h:c * 4 + h + 1], self.tri, ALU.mult, ALU.mult,
                             reads=['ps0', gk, 'cst'], writes=[pre + f'sTb{s}'])
            for r in range(2):
                self.transposes([(self.psb[:, i * 128:(i + 1) * 128], kc[s][:, r * 8 + i, :]) for i in range(8)],
                                self.identb, reads=[kck, 'identb'], writes=['psb'])
                for hh in range(2):
                    h = r * 2 + hh
                    self.act(kt[s][:, h * 512:(h + 1) * 512], self.psb[:, hh * 512:(hh + 1) * 512], AF.Copy,
                             reads=['psb', gk], writes=[ktk], scale=kap2[:, c * 4 + h:c * 4 + h + 1])
            for h in range(H):
                vh = vc[s][:, h * 512:(h + 1) * 512]
                if not summary_only:
                    nb_ = 3 + h % 2
                    nbk = f'ps{nb_}'
                    self.mm(ps[nb_][:], [(sTb[s][:, h * 128:(h + 1) * 128], vh)] +
                            [(qc[s][:, h * 4 + dt, :], Sb[:, h * 4 + dt, :]) for dt in range(4)],
                            reads=[pre + f'sTb{s}', vck, qck] + [pre + f'Sb{h * 4 + dt}' for dt in range(4)],
                            writes=[nbk])
                    self.mm(ps[2][:, 64 + h:64 + h + 1], [(sTb[s][:, h * 128:(h + 1) * 128], self.onesb[:, 0:1])] +
                            [(qc[s][:, h * 4 + dt, :], nb[:, h * 4 + dt:h * 4 + dt + 1]) for dt in range(4)],
                            reads=[pre + f'sTb{s}', qck, 'onesb', pre + 'nb'], writes=['ps2'])
                for dt in range(4):
                    i = h * 4 + dt
                    db = 5 + dsr % 2
                    dsr += 1
                    dbk = f'ps{db}'
                    self.mm(ps[db][:], [(kt[s][:, i * 128:(i + 1) * 128], vh)], reads=[ktk, vck], writes=[dbk])
                    self.stt(S[:, i, :], S[:, i, :], rhon[:, c * 4 + h:c * 4 + h + 1], ps[db][:], ALU.mult, ALU.add,
                             reads=[dbk, gk, pre + f'S{i}'], writes=[pre + f'S{i}'])
                    if not summary_only:
                        self.cp('act', Sb[:, i, :], S[:, i, :], reads=[pre + f'S{i}'], writes=[pre + f'Sb{i}'])
                self.mms([(ps[2][:, h * 4 + dt:h * 4 + dt + 1], [(kt[s][:, (h * 4 + dt) * 128:(h * 4 + dt + 1) * 128],
                                                                  self.onesb[:, 0:1])]) for dt in range(4)],
                         reads=[ktk, 'onesb'], writes=['ps2'])
                self.stt(nst[:, h * 4:(h + 1) * 4], nst[:, h * 4:(h + 1) * 4], rhon[:, c * 4 + h:c * 4 + h + 1],
                         ps[2][:, h * 4:(h + 1) * 4], ALU.mult, ALU.add, reads=['ps2', gk, pre + 'sumn'],
                         writes=[pre + 'sumn'])
                if not summary_only:
                    self.cp('dve', nb[:, h * 4:(h + 1) * 4], nst[:, h * 4:(h + 1) * 4], reads=[pre + 'sumn'],
                            writes=[pre + 'nb'])
                    p2 = self.head_out_m(pre, c, s, h, ps[nb_], nbk, sm, invlam, hn[h % 2], pre + f'hn{h % 2}',
                                         szc[s], ucc[s], yc[s], nw, skp, y_d)
                    if pending is not None:
                        pending()
                    pending = p2
        if pending is not None:
            pending()

    def head_out_m(self, pre, c, s, h, numb, nbk, sm, invlam, hn, hnk, szc, ucc, yc, nw, skp, y_d):
        ps = self.ps
        gk = pre + 'gs'
        smk = pre + 'sm'
        v = sm[:, h, :]
        self.cp('dve', v[:, 14:15], ps[2][:, 64 + h:64 + h + 1], reads=['ps2'], writes=[smk])
        self.ts('dve', v[:, 0:1], v[:, 14:15], -1.0, v[:, 14:15], ALU.mult, ALU.max, reads=[smk], writes=[smk])
        self.tt('dve', v[:, 0:1], v[:, 0:1], invlam[:, c * 4 + h:c * 4 + h + 1], ALU.max, reads=[smk, gk], writes=[smk])
        self.P.add('dve', lambda e: e.reciprocal(out=v[:, 1:2], in_=v[:, 0:1]), [smk], [smk])
        self.P.add('dve', lambda e: e.bn_stats(out=v[:, 2:8], in_=numb[:]), [nbk], [smk])
        self.P.add('dve', lambda e: e.bn_aggr(out=v[:, 8:10], in_=v[:, 2:8]), [smk], [smk])
        self.ts('dve', v[:, 10:11], v[:, 9:10], v[:, 1:2], v[:, 1:2], ALU.mult, ALU.mult, reads=[smk], writes=[smk])
        self.ts('dve', v[:, 10:11], v[:, 10:11], EPS, None, ALU.add, reads=[smk], writes=[smk])
        self.act(v[:, 10:11], v[:, 10:11], AF.Sqrt, reads=[smk], writes=[smk])
        self.P.add('dve', lambda e: e.reciprocal(out=v[:, 11:12], in_=v[:, 10:11]), [smk], [smk])
        self.tt('dve', v[:, 12:13], v[:, 11:12], v[:, 1:2], ALU.mult, reads=[smk], writes=[smk])
        self.ts('dve', v[:, 13:14], v[:, 8:9], v[:, 12:13], -1.0, ALU.mult, ALU.mult, reads=[smk], writes=[smk])
        self.act(hn, numb[:], AF.Identity, reads=[nbk, smk], writes=[hnk], bias=v[:, 13:14], scale=v[:, 12:13])

        def part2():
            self.transposes([(ps[7][:, i * 128:(i + 1) * 128], hn[:, i * 128:(i + 1) * 128]) for i in range(4)],
                            self.ident, reads=[hnk, 'cst'], writes=['ps7'])
            yk = pre + f'yc{s}'
            for i in range(4):
                ft = h * 4 + i
                tmp = self.gt[(h * 4 + i) % 2]
                tk = pre + f'gt{(h * 4 + i) % 2}'
                self.act(tmp, ps[7][:, i * 128:(i + 1) * 128], AF.Copy, reads=['ps7', pre + 'vec'], writes=[tk],
                         scale=nw[:, ft:ft + 1])
                self.stt(tmp, ucc[:, ft, :], skp[:, ft:ft + 1], tmp, ALU.mult, ALU.add,
                         reads=[tk, pre + f'ucc{s}', pre + 'vec'], writes=[tk])
                self.tt('dve', yc[:, ft, :], tmp, szc[:, ft, :], ALU.mult, reads=[tk, pre + f'szc{s}'], writes=[yk])
            if h == H - 1:
                self.dma('pool', y_d[c], yc[:].rearrange("p a b -> p (a b)"), reads=[yk], writes=[pre + 'y_d'])
        return part2

    def stage_B(self, pre, mix):
        A = self.A
        ps = self.ps
        IN, OUT = "ExternalInput", "ExternalOutput"
        ntile = NFT if mix == 'm' else NG
        nsm = 20 if mix == 'm' else 8
        v_d = self.dram(pre + "v", [NT, 128, E], BF16, IN)
        sz_d = self.dram(pre + "sz", [NT, 128, NFT * 128], BF16, IN)
        sumS_all = self.dram(pre + "sumS_all", [NCORES, ntile * 128, DH], F32, IN)
        sumn_all = self.dram(pre + "sumn_all", [128, NCORES * nsm], F32, IN)
        wout_d = self.dram(pre + "w_out", [E, D], F32, IN)
        postw_d = self.dram(pre + "post_w", [1, D], F32, IN)
        xo_d = self.dram(pre + "xo", [TL, D], F32, OUT)
        y_d = self.dram(pre + "y", [NT, 128, NFT * 128], BF16, "Internal")
        self._sz_d = sz_d
        if mix == 'm':
            qT_d = self.dram(pre + "qT", [NT, 128, NFT * 128], BF16, IN)
            kT_d = self.dram(pre + "kT", [NT, 128, NFT * 128], BF16, IN)
            uc_d = self.dram(pre + "uc", [NT, 128, NFT * 128], BF16, IN)
            gsc_d = self.dram(pre + "gsc", [128, GSC_M], F32, IN)
            vec_d = self.dram(pre + "vec", [128, NVM], F32, IN)
            self._uc_d = uc_d
            nvec = NVM
        else:
            qg_d = self.dram(pre + "qg", [NT, 128, NG * 128], BF16, IN)
            kg_d = self.dram(pre + "kg", [NT, 128, NG * 128], BF16, IN)
            kh_d = self.dram(pre + "kh", [NT, 128, NG * 128], BF16, IN)
            egl_d = self.dram(pre + "egl", [128, NG * NT], F32, IN)
            vec_d = self.dram(pre + "vec", [128, NVG], F32, IN)
            nvec = NVG
        vec = A.alloc([nvec], F32)
        self.dma('sp', vec, vec_d, writes=[pre + 'vec'])
        gk = pre + 'gs'
        if mix == 'm':
            gsc = A.alloc([GSC_M], F32)
            self.dma('sp', gsc, gsc_d, writes=[gk])
            nw = vec[:, 88:104]
            skp = vec[:, 104:120]
        else:
            egl = A.alloc([NG, NT], F32)
            self.dma('sp', egl[:].rearrange("p a b -> p (a b)"), egl_d, writes=[gk])
            nw = vec[:, 16:32]
        S = A.alloc([ntile, DH], F32)
        Sb = A.alloc([ntile, DH], BF16)
        sna = A.alloc([NCORES, nsm], F32)
        mj = A.alloc([NCORES, 8], F32)
        self.dma('sp', sna[:].rearrange("p a b -> p (a b)"), sumn_all, writes=[pre + 'sna'])
        nd = 4 if mix == 'm' else 8
        dec = sna[:, :, 16:20] if mix == 'm' else sna[:, :, 0:8]
        self.act(mj[:, :, 0:nd], dec, AF.Exp, reads=[pre + 'sna'], writes=[pre + 'mj'],
                 scale=(-1.0 if mix == 'm' else -1.0 / TAU))
        self.ts('dve', mj[:, :, 0:nd], mj[:, :, 0:nd], -1.0, None, ALU.add, reads=[pre + 'mj'], writes=[pre + 'mj'])
        for j in range(NCORES):
            self.ts('dve', mj[:, j, 0:nd], mj[:, j, 0:nd], self.sel[:, j:j + 1], 1.0, ALU.mult, ALU.add,
                    reads=[pre + 'mj', 'cst'], writes=[pre + 'mj'])
        Skeys = [pre + f'S{i}' for i in range(ntile)]
        self.memset('dve', S, 0.0, writes=Skeys)
        if mix == 'm':
            nst = A.alloc([16], F32)
            nb = A.alloc([16], BF16)
            self.memset('dve', nst, 0.0, writes=[pre + 'sumn'])
        m0 = A.mark()
        gld = [A.alloc([4, DH], F32) for _ in range(2)]
        nl = 0
        for j in range(NCORES - 1):
            for q4 in range(ntile // 4):
                g = gld[nl % 2]
                gkey = pre + f'gld{nl % 2}'
                nl += 1
                self.dma('sp', g, sumS_all[j, q4 * 512:(q4 + 1) * 512, :].rearrange("(a p) d -> p a d", p=128),
                         writes=[gkey])
                for a4 in range(4):
                    i = q4 * 4 + a4
                    hh = (i // 4) if mix == 'm' else i
                    self.act(g[:, a4, :], g[:, a4, :], AF.Copy, reads=[gkey, 'cst'], writes=[gkey],
                             scale=self.sel[:, j:j + 1])
                    self.stt(S[:, i, :], S[:, i, :], mj[:, j, hh:hh + 1], g[:, a4, :], ALU.mult, ALU.add,
                             reads=[gkey, pre + 'mj', Skeys[i]], writes=[Skeys[i]])
            if mix == 'm':
                for h in range(H):
                    self.ts('dve', sna[:, j, h * 4:(h + 1) * 4], sna[:, j, h * 4:(h + 1) * 4], self.sel[:, j:j + 1],
                            None, ALU.mult, reads=[pre + 'sna', 'cst'], writes=[pre + 'sna'])
                    self.stt(nst[:, h * 4:(h + 1) * 4], nst[:, h * 4:(h + 1) * 4], mj[:, j, h:h + 1],
                             sna[:, j, h * 4:(h + 1) * 4], ALU.mult, ALU.add, reads=[pre + 'sna', pre + 'mj', pre + 'sumn'],
                             writes=[pre + 'sumn'])
        A.release(m0)
        self.P.barrier()
        if mix == 'm':
            rho = gsc[:, 192:256]
            for i in range(ntile):
                h = i // 4
                self.ts('dve', S[:, i, :], S[:, i, :], rho[:, h:h + 1], None, ALU.mult, reads=[Skeys[i], gk],
                        writes=[Skeys[i]])
            for h in range(H):
                self.ts('dve', nst[:, h * 4:(h + 1) * 4], nst[:, h * 4:(h + 1) * 4], rho[:, h:h + 1], None, ALU.mult,
                        reads=[pre + 'sumn', gk], writes=[pre + 'sumn'])
            self.cp('dve', nb, nst, reads=[pre + 'sumn'], writes=[pre + 'nb'])
        for i in range(ntile):
            self.cp('act', Sb[:, i, :], S[:, i, :], reads=[Skeys[i]], writes=[pre + f'Sb{i}'])

        m1 = A.mark()
        szc = [A.alloc([NFT, 128], BF16) for _ in range(2)]
        yc = [A.alloc([NFT, 128], BF16) for _ in range(2)]
        self.gt = [A.alloc([128], F32) for _ in range(2)]
        if mix == 'm':
            ucc = [A.alloc([NFT, 128], BF16) for _ in range(2)]
            self.scan_m(pre, kT_d, v_d, qT_d, gsc, S, Sb, nst, nb, summary_only=False,
                        extra=(szc, ucc, yc, y_d, nw, skp))
        else:
            self.scan_g(pre, qg_d, kg_d, kh_d, v_d, egl, S, Sb, summary_only=False, extra=(szc, yc, y_d, nw))
        A.release(m1)
        self.P.barrier()

        wo = A.alloc([NFT, D], BF16)
        for q4 in range(4):
            self.dma('pool', wo[:, q4 * 4:(q4 + 1) * 4, :],
                     wout_d[q4 * 512:(q4 + 1) * 512, :].rearrange("(k p) d -> p k d", p=128), writes=[pre + 'wo'])
        pw = A.alloc([D], F32)
        self.dma('sp', pw, bass.AP(postw_d.tensor, 0, [[0, 128], [1, D]]), writes=[pre + 'pw'])
        yl = [A.alloc([NFT, 128], BF16) for _ in range(2)]
        tmp = [A.alloc([512], F32) for _ in range(2)]
        junk = A.alloc([512], F32)
        s2 = A.alloc([NT, 4], F32)
        fin = []
        for c in range(NT):
            s = c % 2
            self.dma('sp', yl[s][:].rearrange("p a b -> p (a b)"), y_d[c], reads=[pre + 'y_d'], writes=[pre + f'yl{s}'])
            for half in range(2):
                b = 3 + half + 2 * s
                self.mm(ps[b][:], [(yl[s][:, ft, :], wo[:, ft, half * 512:(half + 1) * 512]) for ft in range(NFT)],
                        reads=[pre + f'yl{s}', pre + 'wo'], writes=[f'ps{b}'])
                self.act(junk, ps[b][:], AF.Square, reads=[f'ps{b}'], writes=[pre + 'junk2', pre + 's2'],
                         accum=s2[:, c, half:half + 1])
            self.tt('dve', s2[:, c, 2:3], s2[:, c, 0:1], s2[:, c, 1:2], ALU.add, reads=[pre + 's2'], writes=[pre + 's2'])
            self.ts('dve', s2[:, c, 2:3], s2[:, c, 2:3], 1.0 / D, EPS, ALU.mult, ALU.add, reads=[pre + 's2'],
                    writes=[pre + 's2'])
            self.act(s2[:, c, 2:3], s2[:, c, 2:3], AF.Sqrt, reads=[pre + 's2'], writes=[pre + 's2'])
            self.P.add('dve', lambda e, c=c: e.reciprocal(out=s2[:, c, 3:4], in_=s2[:, c, 2:3]), [pre + 's2'], [pre + 's2'])
            for half in range(2):
                b = 3 + half + 2 * s
                t_ = tmp[half]
                self.stt(t_, ps[b][:], s2[:, c, 3:4], pw[:, half * 512:(half + 1) * 512], ALU.mult, ALU.mult,
                         reads=[f'ps{b}', pre + 's2', pre + 'pw'], writes=[pre + f'tmp{half}'])
                self.tt('dve', self.x[:, c, half * 512:(half + 1) * 512], self.x[:, c, half * 512:(half + 1) * 512], t_,
                        ALU.add, reads=[pre + f'tmp{half}', f'x{c}'], writes=[f'x{c}'])
            fin.append(self.dma('sp', xo_d[c * 128:(c + 1) * 128, :], self.x[:, c, :], reads=[f'x{c}'],
                                writes=[pre + 'xo_d']))
        return fin

    def stage_A_g(self, pre):
        A = self.A
        ps = self.ps
        IN, OUT = "ExternalInput", "ExternalOutput"
        GW = 2 * 1024 + 2 * E + 16
        w_in = self.dram(pre + "w_in", [D, GW], F32, IN)
        wgu_d = self.dram(pre + "wgu", [16, 1024], F32, IN)
        vec_d = self.dram(pre + "vec", [128, NVG], F32, IN)
        qg_d = self.dram(pre + "qg", [NT, 128, NG * 128], BF16, OUT)
        kg_d = self.dram(pre + "kg", [NT, 128, NG * 128], BF16, OUT)
        kh_d = self.dram(pre + "kh", [NT, 128, NG * 128], BF16, OUT)
        v_d = self.dram(pre + "v", [NT, 128, E], BF16, OUT)
        sz_d = self.dram(pre + "sz", [NT, 128, NFT * 128], BF16, OUT)
        egl_d = self.dram(pre + "egl", [128, NG * NT], F32, OUT)
        sumS_d = self.dram(pre + "sumS", [NG * 128, DH], F32, OUT)
        sumn_d = self.dram(pre + "sumn", [128, 8], F32, OUT)

        vec = A.alloc([NVG], F32)
        pnw = vec[:, 0:8]
        nbg = vec[:, 8:16]
        self.dma('sp', vec, vec_d, writes=[pre + 'vec'])
        egl = A.alloc([NG, NT], F32)
        gseg = A.alloc([NG], F32)
        mP1 = A.mark()
        hT = A.alloc([KD, TL], BF16)
        self.prenorm(hT, pnw, [(self.x[:, t, :], f'x{t}') for t in range(NT)], NT, pre, pre + 'vec')
        hkeys = [pre + f'hT{t}' for t in range(NT)]
        wr = A.alloc([KD, 16], BF16)
        self.dma('pool', wr, w_in[:, GW - 16:GW].rearrange("(k p) c -> p k c", p=128), writes=[pre + 'wr'])
        wgu = A.alloc([1024], F32)
        self.dma('sp', wgu[0:16, :], wgu_d, writes=[pre + 'wgu'])
        r_sb = A.alloc([TL], F32)
        for st4 in range(4):
            self.mm(ps[2][0:16, :], [(wr[:, k, :], hT[:, k, st4 * 512:(st4 + 1) * 512]) for k in range(KD)],
                    reads=[pre + 'wr'] + hkeys[st4 * 4:(st4 + 1) * 4], writes=['ps2'])
            self.cp('act', r_sb[0:16, st4 * 512:(st4 + 1) * 512], ps[2][0:16, :], reads=['ps2'], writes=[pre + 'r'])
        spt = A.alloc([TL], F32)
        cs = A.alloc([TL], F32)
        ex = [A.alloc([TL], F32) for _ in range(3)]
        cl = A.alloc([NT], F32)
        ncl = A.alloc([NT], F32)
        wblk = [A.alloc([KD, 512], BF16) for _ in range(3)]
        wkeys = [pre + f'wblk{i}' for i in range(3)]
        srcs = [w_in[:, 0:512], w_in[:, 1024:1536], w_in[:, 512:1024], w_in[:, 1536:2048]] + \
            [w_in[:, 2048 + i * 512:2048 + (i + 1) * 512] for i in range(8)]
        ws = self.wstream(wblk, wkeys, srcs)
        stg = [A.alloc([TL], BF16) for _ in range(3)]
        vst = [A.alloc([512], BF16) for _ in range(2)]
        bank_rr = 0

        def next_bank():
            nonlocal bank_rr
            b = 3 + bank_rr % 4
            bank_rr += 1
            return b
        lq = math.log(GK ** -0.5)
        gk = pre + 'gs'
        for blk in range(2):
            ws.start(2 * blk)
            wqb, wqk_ = ws.get(2 * blk)
            wkb, wkk_ = ws.get(2 * blk + 1)
            for gi in range(4):
                g = blk * 4 + gi
                for st4 in range(4):
                    b = next_bank()
                    self.mm(ps[b][:], [(wgu[0:16, g * 128:(g + 1) * 128], r_sb[0:16, st4 * 512:(st4 + 1) * 512])],
                            reads=[pre + 'wgu', pre + 'r'], writes=[f'ps{b}'])
                    self.act(spt[:, st4 * 512:(st4 + 1) * 512], ps[b][:], AF.Exp, reads=[f'ps{b}', pre + 'vec'],
                             writes=[pre + 'spt'], bias=nbg[:, g:g + 1], scale=-1.0)
                self.act(spt, spt, AF.Ln, reads=[pre + 'spt'], writes=[pre + 'spt'], bias=1.0)
                for c in range(NT):
                    self.P.add('dve', lambda e, c=c: e.tensor_tensor_scan(
                        out=cs[:, c * 128:(c + 1) * 128], data0=self.ones, data1=spt[:, c * 128:(c + 1) * 128],
                        initial=0.0, op0=ALU.mult, op1=ALU.add), [pre + 'spt', 'cst'], [pre + 'cs'])
                self.cp('dve', cl[:].rearrange("p (c o) -> p c o", o=1), cs[:].rearrange("p (c t) -> p c t", t=128)[:, :, 127:128], reads=[pre + 'cs'],
                        writes=[pre + 'cl'])
                self.ts('dve', ncl, cl, -1.0 / TAU, None, ALU.mult, reads=[pre + 'cl'], writes=[pre + 'cl'])
                self.act(egl[:, g, :], cl, AF.Exp, reads=[pre + 'cl'], writes=[gk], scale=-1.0 / TAU)
                self.P.add('dve', lambda e, g=g: e.tensor_reduce(out=gseg[:, g:g + 1], in_=cl, axis=AX.X, op=ALU.add),
                           [pre + 'cl'], [pre + 'gseg'])
                self.act(ex[0], cs, AF.Exp, reads=[pre + 'cs'], writes=[pre + 'ex0'], scale=-1.0 / TAU, bias=lq)
                self.act(ex[1], cs, AF.Exp, reads=[pre + 'cs'], writes=[pre + 'ex1'], scale=1.0 / TAU)
                for c in range(NT):
                    self.act(ex[2][:, c * 128:(c + 1) * 128], cs[:, c * 128:(c + 1) * 128], AF.Exp,
                             reads=[pre + 'cs', pre + 'cl'], writes=[pre + 'ex2'], scale=1.0 / TAU, bias=ncl[:, c:c + 1])
                for st4 in range(4):
                    sl = slice(st4 * 512, (st4 + 1) * 512)
                    b = next_bank()
                    self.mm(ps[b][:], [(wqb[:, k, gi * 128:(gi + 1) * 128], hT[:, k, sl]) for k in range(KD)],
                            reads=[wqk_] + hkeys[st4 * 4:(st4 + 1) * 4], writes=[f'ps{b}'])
                    self.tt('dve', stg[0][:, sl], ps[b][:], ex[0][:, sl], ALU.mult, reads=[f'ps{b}', pre + 'ex0'],
                            writes=[pre + 'stg0'])
                    b = next_bank()
                    self.mm(ps[b][:], [(wkb[:, k, gi * 128:(gi + 1) * 128], hT[:, k, sl]) for k in range(KD)],
                            reads=[wkk_] + hkeys[st4 * 4:(st4 + 1) * 4], writes=[f'ps{b}'])
                    self.tt('dve', stg[1][:, sl], ps[b][:], ex[1][:, sl], ALU.mult, reads=[f'ps{b}', pre + 'ex1'],
                            writes=[pre + 'stg1'])
                    self.tt('dve', stg[2][:, sl], ps[b][:], ex[2][:, sl], ALU.mult, reads=[f'ps{b}', pre + 'ex2'],
                            writes=[pre + 'stg2'])
                for w_, dst in enumerate((qg_d, kg_d, kh_d)):
                    self.dma('pool', dst[:, :, g * 128:(g + 1) * 128].rearrange("c p t -> p c t"),
                             stg[w_][:].rearrange("p (c t) -> p c t", t=128), reads=[pre + f'stg{w_}'],
                             writes=[pre + f'qk_d{w_}'])
        self.outs.append(self.dma('sp', egl_d, egl[:].rearrange("p a b -> p (a b)"), reads=[gk], writes=[pre + 'egl_d']))
        self.outs.append(self.dma('sp', sumn_d, gseg, reads=[pre + 'gseg'], writes=[pre + 'sumn_d']))
        nblk = 0
        nv = 0
        for vb in range(4):
            ws.start(4 + vb)
            wb, wbk = ws.get(4 + vb)
            for tt in range(NT):
                b = next_bank()
                self.mm(ps[b][:], [(hT[:, k, tt * 128:(tt + 1) * 128], wb[:, k, :]) for k in range(KD)],
                        reads=[wbk, hkeys[tt]], writes=[f'ps{b}'])
                vs = vst[nv % 2]
                vk = pre + f'vst{nv % 2}'
                nv += 1
                self.evac(vs, ps[b][:], reads=[f'ps{b}'], writes=[vk])
                self.dma('pool', v_d[tt, :, vb * 512:(vb + 1) * 512], vs, reads=[vk], writes=[pre + 'v_d'])
        nstg = 0
        for zb in range(4):
            ws.start(8 + zb)
            wb, wbk = ws.get(8 + zb)
            for ft in range(4):
                sg = stg[nstg % 2]
                sk = pre + f'stg{nstg % 2}'
                nstg += 1
                for st4 in range(4):
                    b = next_bank()
                    self.mm(ps[b][:], [(wb[:, k, ft * 128:(ft + 1) * 128], hT[:, k, st4 * 512:(st4 + 1) * 512])
                                       for k in range(KD)], reads=[wbk] + hkeys[st4 * 4:(st4 + 1) * 4], writes=[f'ps{b}'])
                    self.act(sg[:, st4 * 512:(st4 + 1) * 512], ps[b][:], AF.Silu, reads=[f'ps{b}'], writes=[sk])
                tile_i = zb * 4 + ft
                self.dma('pool', sz_d[:, :, tile_i * 128:(tile_i + 1) * 128].rearrange("c p t -> p c t"),
                         sg[:].rearrange("p (c t) -> p c t", t=128), reads=[sk], writes=[pre + 'sz_d'])
        A.release(mP1)
        self.P.barrier()
        S = A.alloc([NG, DH], F32)
        self.memset('dve', S, 0.0, writes=[pre + f'S{i}' for i in range(NG)])
        self.scan_g(pre, None, None, kh_d, v_d, egl, S, None, summary_only=True)
        for i in range(NG):
            self.outs.append(self.dma('sp', sumS_d[i * 128:(i + 1) * 128, :], S[:, i, :], reads=[pre + f'S{i}'],
                                      writes=[pre + 'sumS_d']))

    def scan_g(self, pre, qg_d, kg_d, kh_d, v_d, egl, S, Sb, summary_only, extra=None):
        A = self.A
        ps = self.ps
        gk = pre + 'gs'
        khc = [A.alloc([NG, 128], BF16) for _ in range(2)]
        vc = [A.alloc([E], BF16) for _ in range(2)]
        kh = [A.alloc([1024], BF16) for _ in range(2)]
        if not summary_only:
            qc = [A.alloc([NG, 128], BF16) for _ in range(2)]
            kc = [A.alloc([NG, 128], BF16) for _ in range(2)]
            Ab = [A.alloc([512], BF16) for _ in range(2)]
            on = [A.alloc([DH], F32) for _ in range(2)]
            sm = A.alloc([H, 8], F32)
            junk = A.alloc([DH], F32)
            (szc, yc, y_d, nw) = extra
        dsr = 0
        pending = None
        for c in range(NT):
            s = c % 2
            khk, vck, kk = pre + f'khc{s}', pre + f'vc{s}', pre + f'kh{s}'
            self.dma('sp', khc[s][:].rearrange("p a b -> p (a b)"), kh_d[c], reads=[pre + 'qk_d2'], writes=[khk])
            self.dma('sp', vc[s], v_d[c], reads=[pre + 'v_d'], writes=[vck])
            if not summary_only:
                qck, kck = pre + f'qc{s}', pre + f'kc{s}'
                self.dma('sp', qc[s][:].rearrange("p a b -> p (a b)"), qg_d[c], reads=[pre + 'qk_d0'], writes=[qck])
                self.dma('sp', kc[s][:].rearrange("p a b -> p (a b)"), kg_d[c], reads=[pre + 'qk_d1'], writes=[kck])
                self.dma('sp', szc[s][:].rearrange("p a b -> p (a b)"), self._sz_d[c], reads=[pre + 'sz_d'],
                         writes=[pre + f'szc{s}'])
                self.mms([(ps[0][:, h * 128:(h + 1) * 128],
                           [(kc[s][:, h * 2 + dt, :], qc[s][:, h * 2 + dt, :]) for dt in range(2)]) for h in range(H)],
                         reads=[kck, qck], writes=['ps0'])
                self.tt('dve', Ab[s], ps[0][:], self.tri4, ALU.mult, reads=['ps0', 'cst'], writes=[pre + f'Ab{s}'])
            self.transposes([(self.psb[:, i * 128:(i + 1) * 128], khc[s][:, i, :]) for i in range(NG)], self.identb,
                            reads=[khk, 'identb'], writes=['psb'])
            self.cp('act', kh[s], self.psb[:], reads=['psb'], writes=[kk])
            for h in range(H):
                vh = vc[s][:, h * 512:(h + 1) * 512]
                if not summary_only:
                    nb_ = 3 + h % 2
                    nbk = f'ps{nb_}'
                    self.mm(ps[nb_][:], [(Ab[s][:, h * 128:(h + 1) * 128], vh)] +
                            [(qc[s][:, h * 2 + dt, :], Sb[:, h * 2 + dt, :]) for dt in range(2)],
                            reads=[pre + f'Ab{s}', vck, qck] + [pre + f'Sb{h * 2 + dt}' for dt in range(2)], writes=[nbk])
                for dt in range(2):
                    i = h * 2 + dt
                    db = 5 + dsr % 2
                    dsr += 1
                    dbk = f'ps{db}'
                    self.mm(ps[db][:], [(kh[s][:, i * 128:(i + 1) * 128], vh)], reads=[kk, vck], writes=[dbk])
                    self.stt(S[:, i, :], S[:, i, :], egl[:, i, c:c + 1], ps[db][:], ALU.mult, ALU.add,
                             reads=[dbk, gk, pre + f'S{i}'], writes=[pre + f'S{i}'])
                    if not summary_only:
                        self.cp('act', Sb[:, i, :], S[:, i, :], reads=[pre + f'S{i}'], writes=[pre + f'Sb{i}'])
                if not summary_only:
                    v = sm[:, h, :]
                    smk = pre + 'sm'
                    o = on[h % 2]
                    ok = pre + f'on{h % 2}'
                    self.act(junk, ps[nb_][:], AF.Square, reads=[nbk], writes=[pre + 'junk3', smk], accum=v[:, 0:1])
                    self.ts('dve', v[:, 1:2], v[:, 0:1], 1.0 / DH, EPS, ALU.mult, ALU.add, reads=[smk], writes=[smk])
                    self.act(v[:, 1:2], v[:, 1:2], AF.Sqrt, reads=[smk], writes=[smk])
                    self.P.add('dve', lambda e, v=v: e.reciprocal(out=v[:, 2:3], in_=v[:, 1:2]), [smk], [smk])
                    self.act(o, ps[nb_][:], AF.Copy, reads=[nbk, smk], writes=[ok], scale=v[:, 2:3])

                    def part2(c=c, s=s, h=h, o=o, ok=ok):
                        self.transposes([(ps[7][:, i * 128:(i + 1) * 128], o[:, i * 128:(i + 1) * 128])
                                         for i in range(4)], self.ident, reads=[ok, 'cst'], writes=['ps7'])
                        for i in range(4):
                            ft = h * 4 + i
                            self.stt(yc[s][:, ft, :], ps[7][:, i * 128:(i + 1) * 128], nw[:, ft:ft + 1],
                                     szc[s][:, ft, :], ALU.mult, ALU.mult,
                                     reads=['ps7', pre + 'vec', pre + f'szc{s}'], writes=[pre + f'yc{s}'])
                        if h == H - 1:
                            self.dma('pool', y_d[c], yc[s][:].rearrange("p a b -> p (a b)"), reads=[pre + f'yc{s}'],
                                     writes=[pre + 'y_d'])
                    if pending is not None:
                        pending()
                    pending = part2
        if pending is not None:
            pending()


def extra_sz(kb, pre):
    return kb._sz_d


def extra_uc(kb, pre):
    return kb._uc_d


_PROGS = {}


def get_prog(stages):
    key = tuple(stages)
    if key not in _PROGS:
        nc = bass.Bass("TRN2", target_bir_lowering=False)
        kb = KB(nc, list(stages))
        kb.build()
        _PROGS[key] = nc
    return _PROGS[key]


def _consts(core):
    c = np.zeros((128, CST_W), np.float32)
    c[:, 0:128] = np.eye(128, dtype=np.float32)
    tri = np.triu(np.ones((128, 128), np.float32))
    c[:, 128:640] = np.tile(tri, (1, 4))
    c[:, 640:768] = 1.0
    c[:, 768:776] = (np.arange(8) < core).astype(np.float32)[None, :]
    return c


def _cols(v, ntile):
    return np.ascontiguousarray(v.reshape(ntile, 128).T)


def _f(a):
    return np.ascontiguousarray(a, dtype=np.float32)


def kernel(x, pre_norm_w, post_norm_w,
           m_w_in, m_conv_w, m_conv_b, m_wq, m_wk, m_wv, m_w_gate, m_b_gate,
           m_norm_w, m_skip, m_w_out,
           g_w_in, g_w_gate_up, g_b_gate, g_norm_w, g_w_out, _depth=4, _debug=None):
    x = _f(x)
    xs = x.reshape(SEQ, D)
    cur = [np.ascontiguousarray(xs[c * TL:(c + 1) * TL]) for c in range(NCORES)]
    consts = [_consts(c) for c in range(NCORES)]

    def vec_m(i, j):
        v = np.zeros((128, NVM), np.float32)
        v[:, 0:8] = _cols(pre_norm_w[i], 8)
        for k in range(4):
            v[:, 8 + k * 16:8 + (k + 1) * 16] = _cols(m_conv_w[j, k], 16)
        v[:, 72:88] = _cols(m_conv_b[j], 16)
        v[:, 88:104] = _cols(m_norm_w[j], 16)
        v[:, 104:120] = _cols(m_skip[j], 16)
        return v

    def vec_g(i, j):
        v = np.zeros((128, NVG), np.float32)
        v[:, 0:8] = _cols(pre_norm_w[i], 8)
        v[:, 8:16] = -_cols(g_b_gate[j], 8)
        v[:, 16:32] = _cols(g_norm_w[j], 16)
        return v

    def a_inputs(i, pre, c, full_x):
        j = i // 2
        d = {}
        if i % 2 == 0:
            xh = np.zeros((128, D), np.float32)
            if c > 0:
                xh[0:3] = full_x[c * TL - 3:c * TL]
            d.update({pre + "w_in": _f(m_w_in[j]), pre + "wq": _f(m_wq[j]), pre + "wk": _f(m_wk[j]),
                      pre + "wv": _f(m_wv[j]), pre + "wvT": _f(np.transpose(m_wv[j], (0, 2, 1))),
                      pre + "wg": _f(m_w_gate[j].reshape(48, 128, 8).transpose(1, 0, 2).reshape(128, 384)),
                      pre + "bg": _f(m_b_gate[j].reshape(1, 8)), pre + "vec": vec_m(i, j), pre + "xh": xh})
        else:
            d.update({pre + "w_in": _f(g_w_in[j]), pre + "wgu": _f(g_w_gate_up[j]), pre + "vec": vec_g(i, j)})
        return d

    def b_inputs(i, pre, c, aout, sumS_all, sumn_all):
        j = i // 2
        d = {pre + "sumS_all": sumS_all, pre + "sumn_all": sumn_all,
             pre + "post_w": _f(post_norm_w[i].reshape(1, D))}
        if i % 2 == 0:
            d.update({pre + "w_out": _f(m_w_out[j]), pre + "vec": vec_m(i, j)})
            names = ["qT", "kT", "v", "sz", "uc", "gsc"]
        else:
            d.update({pre + "w_out": _f(g_w_out[j]), pre + "vec": vec_g(i, j)})
            names = ["qg", "kg", "kh", "v", "sz", "egl"]
        for n in names:
            d[pre + n] = aout[c][n]
        return d

    def grab(res, pre, i):
        names = (["qT", "kT", "v", "sz", "uc", "gsc"] if i % 2 == 0 else ["qg", "kg", "kh", "v", "sz", "egl"]) + \
            ["sumS", "sumn"]
        return [{n: res.results[c][pre + n] for n in names} for c in range(NCORES)]

    def launch(stages, maps):
        nc = get_prog(stages)
        return run_bass_kernel_spmd(nc, maps, core_ids=list(range(NCORES)))

    def gathered(aout):
        sumS_all = np.ascontiguousarray(np.stack([aout[c]["sumS"] for c in range(NCORES)], axis=0))
        sumn_all = np.ascontiguousarray(np.concatenate([aout[c]["sumn"] for c in range(NCORES)], axis=1))
        return sumS_all, sumn_all

    dbg = {}
    plan = [[('A', 0)]]
    for i in range(_depth):
        if i + 1 < _depth and (i + 1) % 2 == 1:
            plan.append([('B', i), ('A', i + 1)])
        else:
            plan.append([('B', i)])
            if i + 1 < _depth:
                plan.append([('A', i + 1)])
    aout = None
    pend = None
    for stages in plan:
        full_x = np.concatenate(cur, axis=0)
        prog = tuple((k, 'm' if i % 2 == 0 else 'g') for k, i in stages)
        if any(k == 'B' for k, _ in stages):
            sumS_all, sumn_all = gathered(aout)
        maps = []
        for c in range(NCORES):
            d = {"cst": consts[c], "x": cur[c]}
            for si, (k, i) in enumerate(stages):
                pre = f"s{si}_"
                if k == 'A':
                    d.update(a_inputs(i, pre, c, full_x))
                else:
                    d.update(b_inputs(i, pre, c, aout, sumS_all, sumn_all))
            maps.append(d)
        res = launch(prog, maps)
        for si, (k, i) in enumerate(stages):
            pre = f"s{si}_"
            if k == 'A':
                aout = grab(res, pre, i)
                if _debug is not None:
                    dbg[f"A{i}"] = aout
            else:
                cur = [res.results[c][pre + "xo"] for c in range(NCORES)]
    out = np.concatenate(cur, axis=0).reshape(1, SEQ, D).astype(np.float32)
    if _debug is not None:
        _debug.update(dbg)
    return out
```

```python
import math
from contextlib import ExitStack

import ml_dtypes
import numpy as np

import concourse.bass as bass
import concourse.mybir as mybir
from concourse.bass_utils import run_bass_kernel_spmd

F32 = mybir.dt.float32
BF16 = mybir.dt.bfloat16
AF = mybir.ActivationFunctionType
ALU = mybir.AluOpType
AX = mybir.AxisListType

NCORES = 8
SEQ = 16384
TL = SEQ // NCORES
NT = TL // 128
D = 1024
KD = D // 128
E = 2048
NFT = E // 128
H = 4
DH = 512
GK = 256
NG = 8
EPS = 1e-6
TAU = 16.0

GSC_M = 5 * 64
NVM = 8 + 64 + 16 + 16 + 16
NVG = 8 + 8 + 16
CST_W = 128 + 512 + 128 + 8


class Prog:
    ENGS = ['pe', 'act', 'dve', 'pool', 'sp']
    NDS = 8

    def __init__(self, nc):
        self.nc = nc
        self.q = {e: [] for e in self.ENGS}
        self.last_w = {}
        self.readers = {}
        self.ndma = {e: 0 for e in self.ENGS}
        self.dma_ops = {e: [] for e in self.ENGS}
        self.pending_bar = {}

    def add(self, eng, fn, reads=(), writes=(), dma=False):
        op = dict(eng=eng, fn=fn, idx=len(self.q[eng]), dma=dma, sig=False, depo={})
        depo = op['depo']
        for b in reads:
            w = self.last_w.get(b)
            if w is not None:
                depo[id(w)] = w
        for b in writes:
            w = self.last_w.get(b)
            if w is not None:
                depo[id(w)] = w
            for r in self.readers.get(b, ()):
                depo[id(r)] = r
        bar = self.pending_bar.pop(eng, None)
        if bar:
            for d in bar:
                depo[id(d)] = d
        if dma:
            k = self.ndma[eng]
            self.ndma[eng] += 1
            op['dk'] = k
            if k >= self.NDS:
                prev = self.dma_ops[eng][k - self.NDS]
                depo[id(prev)] = prev
            self.dma_ops[eng].append(op)
        for b in reads:
            self.readers.setdefault(b, []).append(op)
        for b in writes:
            self.last_w[b] = op
            self.readers[b] = []
        self.q[eng].append(op)
        return op

    def barrier(self):
        ops = []
        for e in self.ENGS:
            comp = [o for o in self.q[e] if not o['dma']]
            if comp:
                ops.append(comp[-1])
            ops += self.dma_ops[e][-self.NDS:]
        for e in self.ENGS:
            self.pending_bar[e] = list(ops)

    def emit(self, final_wait_ops=()):
        nc = self.nc
        for e in self.ENGS:
            for op in self.q[e]:
                need = []
                best = {}
                for d in op['depo'].values():
                    if d is op:
                        continue
                    if d['dma']:
                        need.append(d)
                    else:
                        if d['eng'] == e and (e == 'pe' or d['idx'] > op['idx']):
                            continue
                        if d['eng'] not in best or best[d['eng']]['idx'] < d['idx']:
                            best[d['eng']] = d
                need += list(best.values())
                for d in need:
                    d['sig'] = True
                op['need'] = need
                op['depo'] = None
        for e in self.ENGS:
            c = 0
            for op in self.q[e]:
                if op['dma']:
                    op['semslot'] = op['dk'] % self.NDS
                    op['semval'] = 16 * (op['dk'] // self.NDS + 1)
                elif op['sig']:
                    c += 1
                    op['semval'] = c
        with ExitStack() as st:
            csem = {e: st.enter_context(nc.semaphore(f"c_{e}")) for e in self.ENGS}
            dsem = {e: ([st.enter_context(nc.semaphore(f"d_{e}{i}")) for i in range(self.NDS)]
                        if self.ndma[e] else []) for e in self.ENGS}
            block = st.enter_context(nc.Block())
            handles = {'pe': block.tensor, 'act': block.scalar, 'dve': block.vector,
                       'pool': block.gpsimd, 'sp': block.sync}

            def mk(e):
                def body(eng):
                    waited = {}
                    for op in self.q[e]:
                        for d in op['need']:
                            if d['dma']:
                                key = ('d', d['eng'], d['semslot'])
                                sem = dsem[d['eng']][d['semslot']]
                            else:
                                key = ('c', d['eng'])
                                sem = csem[d['eng']]
                            if waited.get(key, 0) >= d['semval']:
                                continue
                            eng.wait_ge(sem, d['semval'])
                            waited[key] = d['semval']
                        ins = op['fn'](eng)
                        if op['dma']:
                            ins.then_inc(dsem[e][op['semslot']], 16)
                        elif op['sig']:
                            ins.then_inc(csem[e], 1)
                    if e == 'sp':
                        for d in final_wait_ops:
                            if d['dma']:
                                eng.wait_ge(dsem[d['eng']][d['semslot']], d['semval'])
                            else:
                                eng.wait_ge(csem[d['eng']], d['semval'])
                return body

            for e in self.ENGS:
                if self.q[e] or e == 'sp':
                    handles[e](mk(e))


class Arena:
    def __init__(self, t, nwords):
        self.t = t
        self.cap = nwords
        self.off = 0

    def alloc(self, free_shape, dtype):
        n = 1
        for s in free_shape:
            n *= s
        nbytes = n * (4 if dtype == F32 else 2)
        words = (nbytes + 3) // 4
        words = (words + 15) // 16 * 16
        off = self.off
        self.off += words
        assert self.off <= self.cap, f"arena overflow {self.off * 4} > {self.cap * 4}"
        v = self.t[:, off:off + (nbytes + 3) // 4]
        if dtype != F32:
            v = v.bitcast(dtype)
        if len(free_shape) == 2:
            v = v.rearrange("p (a b) -> p a b", a=free_shape[0], b=free_shape[1])
        elif len(free_shape) == 3:
            v = v.rearrange("p (a b c) -> p a b c", a=free_shape[0], b=free_shape[1], c=free_shape[2])
        return v

    def mark(self):
        return self.off

    def release(self, m):
        self.off = m


class KB:
    def __init__(self, nc, stages):
        self.nc = nc
        self.P = Prog(nc)
        self.stages = stages
        self.outs = []
        self.rr = 0

    def dma(self, eng, out, in_, reads=(), writes=()):
        return self.P.add(eng, lambda e: e.dma_start(out=out, in_=in_), reads, writes, dma=True)

    def mm(self, out, pairs, reads=(), writes=()):
        n = len(pairs)

        def fn(e):
            ins = None
            for i, (l, r) in enumerate(pairs):
                ins = e.matmul(out, lhsT=l, rhs=r, start=(i == 0), stop=(i == n - 1))
            return ins
        return self.P.add('pe', fn, reads, writes)

    def mms(self, groups, reads=(), writes=()):
        def fn(e):
            ins = None
            for out, pairs in groups:
                n = len(pairs)
                for i, (l, r) in enumerate(pairs):
                    ins = e.matmul(out, lhsT=l, rhs=r, start=(i == 0), stop=(i == n - 1))
            return ins
        return self.P.add('pe', fn, reads, writes)

    def transposes(self, items, ident, reads=(), writes=()):
        def fn(e):
            ins = None
            for out, in_ in items:
                ins = e.transpose(out, in_, ident)
            return ins
        return self.P.add('pe', fn, reads, writes)

    def act(self, out, in_, func, reads=(), writes=(), bias=None, scale=None, accum=None):
        kw = {}
        if bias is not None:
            kw['bias'] = bias
        if scale is not None:
            kw['scale'] = scale
        if accum is not None:
            kw['accum_out'] = accum
        return self.P.add('act', lambda e: e.activation(out=out, in_=in_, func=func, **kw), reads, writes)

    def ts(self, eng, out, in0, s1, s2, op0, op1=None, reads=(), writes=()):
        if op1 is None:
            return self.P.add(eng, lambda e: e.tensor_scalar(out=out, in0=in0, scalar1=s1, scalar2=None, op0=op0),
                              reads, writes)
        return self.P.add(eng, lambda e: e.tensor_scalar(out=out, in0=in0, scalar1=s1, scalar2=s2, op0=op0, op1=op1),
                          reads, writes)

    def tt(self, eng, out, in0, in1, op, reads=(), writes=()):
        return self.P.add(eng, lambda e: e.tensor_tensor(out=out, in0=in0, in1=in1, op=op), reads, writes)

    def stt(self, out, in0, scalar, in1, op0, op1, reads=(), writes=()):
        return self.P.add('dve', lambda e: e.scalar_tensor_tensor(out=out, in0=in0, scalar=scalar, in1=in1,
                                                                  op0=op0, op1=op1), reads, writes)

    def cp(self, eng, out, in_, reads=(), writes=()):
        if eng == 'act':
            return self.P.add('act', lambda e: e.copy(out=out, in_=in_), reads, writes)
        return self.P.add(eng, lambda e: e.tensor_copy(out=out, in_=in_), reads, writes)

    def memset(self, eng, ap, val, writes=()):
        return self.P.add(eng, lambda e: e.memset(ap, val), (), writes)

    def evac(self, out, in_, reads=(), writes=()):
        self.rr += 1
        return self.cp('act' if self.rr % 2 else 'dve', out, in_, reads, writes)

    def wstream(self, bufs, keys, srcs):
        kb = self

        class WS:
            nxt = 0

            def start(ws, i):
                while ws.nxt <= i + len(bufs) - 1 and ws.nxt < len(srcs):
                    j = ws.nxt
                    kb.dma('pool', bufs[j % len(bufs)], srcs[j].rearrange("(k p) c -> p k c", p=128),
                           writes=[keys[j % len(bufs)]])
                    ws.nxt += 1

            def get(ws, i):
                return bufs[i % len(bufs)], keys[i % len(bufs)]
        return WS()

    def dram(self, name, shape, dtype, kind):
        t = self.nc.dram_tensor(name, list(shape), dtype, kind=kind)
        return t.ap()

    def build(self):
        nc = self.nc
        with ExitStack() as st:
            arena_t = st.enter_context(nc.sbuf_tensor("arena", [128, 53000], F32))
            self.A = Arena(arena_t, 53000)
            self.ps = [st.enter_context(nc.psum_tensor(f"ps{i}", [128, 512], F32)) for i in range(8) if i != 1]
            self.ps.insert(1, None)
            self.psb = st.enter_context(nc.psum_tensor("psb", [128, 1024], BF16))
            A = self.A
            self.cst = A.alloc([CST_W], F32)
            self.ident = self.cst[:, 0:128]
            self.tri4 = self.cst[:, 128:640]
            self.tri = self.cst[:, 128:256]
            self.ones = self.cst[:, 640:768]
            self.sel = self.cst[:, 768:776]
            self.identb = A.alloc([128], BF16)
            self.onesb = A.alloc([128], BF16)
            self.x = A.alloc([NT, D], F32)
            cst_d = self.dram("cst", [128, CST_W], F32, "ExternalInput")
            x_d = self.dram("x", [TL, D], F32, "ExternalInput")
            self.dma('sp', self.cst, cst_d, writes=['cst'])
            self.cp('dve', self.identb, self.ident, reads=['cst'], writes=['identb'])
            self.cp('dve', self.onesb, self.ones, reads=['cst'], writes=['onesb'])
            for t in range(NT):
                self.dma('sp', self.x[:, t, :], x_d[t * 128:(t + 1) * 128, :], writes=[f'x{t}'])
            final = []
            for si, (kind, mix) in enumerate(self.stages):
                pre = f"s{si}_"
                self.stage_tag = [pre]
                m = A.mark()
                if kind == 'A' and mix == 'm':
                    self.stage_A_m(pre)
                elif kind == 'B' and mix == 'm':
                    final += self.stage_B(pre, 'm')
                elif kind == 'A' and mix == 'g':
                    self.stage_A_g(pre)
                else:
                    final += self.stage_B(pre, 'g')
                A.release(m)
                self.P.barrier()
            final += self.outs
            self.P.emit(final_wait_ops=final)

    def prenorm(self, hT, pnw, xtiles, ntiles, tagp, veck):
        A = self.A
        ss = A.alloc([ntiles], F32)
        rstd = A.alloc([ntiles], F32)
        if not hasattr(self, '_pn_tmp') or self._pn_tmp[0] != id(self.stage_tag):
            self._pn_tmp = (id(self.stage_tag), A.alloc([D], BF16), [A.alloc([D], F32) for _ in range(2)])
        junk = self._pn_tmp[1]
        xs = self._pn_tmp[2]
        pk = self.stage_tag[0]
        for t, (xa, key) in enumerate(xtiles):
            self.act(junk, xa, AF.Square, reads=[key], writes=[pk + 'junk', tagp + 'ss'], accum=ss[:, t:t + 1])
        self.ts('dve', rstd, ss, 1.0 / D, EPS, ALU.mult, ALU.add, reads=[tagp + 'ss'], writes=[tagp + 'rstd'])
        self.act(rstd, rstd, AF.Sqrt, reads=[tagp + 'rstd'], writes=[tagp + 'rstd'])
        self.P.add('dve', lambda e: e.reciprocal(out=rstd, in_=rstd), [tagp + 'rstd'], [tagp + 'rstd'])
        for t, (xa, key) in enumerate(xtiles):
            xst = xs[t % 2]
            self.ts('dve', xst, xa, rstd[:, t:t + 1], None, ALU.mult, reads=[key, tagp + 'rstd'],
                    writes=[pk + f'xs{t % 2}'])
            for half in range(2):
                bank = self.ps[0] if half == 0 else self.ps[7]
                bk = 'ps0' if half == 0 else 'ps7'
                self.transposes([(bank[:, i * 128:(i + 1) * 128], xst[:, (half * 4 + i) * 128:(half * 4 + i + 1) * 128])
                                 for i in range(4)], self.ident, reads=[pk + f'xs{t % 2}', 'cst'], writes=[bk])
                for i in range(4):
                    k = half * 4 + i
                    o = hT[:, k, t * 128:(t + 1) * 128]
                    if i % 2 == 0:
                        self.act(o, bank[:, i * 128:(i + 1) * 128], AF.Copy, reads=[bk, veck],
                                 writes=[tagp + f'hT{t}'], scale=pnw[:, k:k + 1])
                    else:
                        self.ts('dve', o, bank[:, i * 128:(i + 1) * 128], pnw[:, k:k + 1], None, ALU.mult,
                                reads=[bk, veck], writes=[tagp + f'hT{t}'])

    def stage_A_m(self, pre):
        A = self.A
        P = self.P
        ps = self.ps
        IN, OUT = "ExternalInput", "ExternalOutput"
        w_in = self.dram(pre + "w_in", [D, 2 * E], F32, IN)
        wq_d = self.dram(pre + "wq", [H, DH, DH], F32, IN)
        wk_d = self.dram(pre + "wk", [H, DH, DH], F32, IN)
        wv_d = self.dram(pre + "wv", [H, DH, DH], F32, IN)
        wvT_d = self.dram(pre + "wvT", [H, DH, DH], F32, IN)
        wg_d = self.dram(pre + "wg", [128, 48 * 8], F32, IN)
        bg_d = self.dram(pre + "bg", [1, 8], F32, IN)
        vec_d = self.dram(pre + "vec", [128, NVM], F32, IN)
        xh_d = self.dram(pre + "xh", [128, D], F32, IN)
        qT_d = self.dram(pre + "qT", [NT, 128, NFT * 128], BF16, OUT)
        kT_d = self.dram(pre + "kT", [NT, 128, NFT * 128], BF16, OUT)
        v_d = self.dram(pre + "v", [NT, 128, E], BF16, OUT)
        sz_d = self.dram(pre + "sz", [NT, 128, NFT * 128], BF16, OUT)
        uc_d = self.dram(pre + "uc", [NT, 128, NFT * 128], BF16, OUT)
        gsc_d = self.dram(pre + "gsc", [128, GSC_M], F32, OUT)
        sumS_d = self.dram(pre + "sumS", [NFT * 128, DH], F32, OUT)
        sumn_d = self.dram(pre + "sumn", [128, 20], F32, OUT)

        vec = A.alloc([NVM], F32)
        pnw = vec[:, 0:8]
        cw = vec[:, 8:72]
        cb = vec[:, 72:88]
        self.dma('sp', vec, vec_d, writes=[pre + 'vec'])
        wg = A.alloc([48 * 8], BF16)
        self.dma('pool', wg, wg_d, writes=[pre + 'wg'])
        bgb = A.alloc([NT, 8], F32)
        self.dma('sp', bgb, bg_d.partition_broadcast(128).broadcast_to([128, NT, 8]) if False else
                 bass.AP(bg_d.tensor, 0, [[0, 128], [0, NT], [1, 8]]), writes=[pre + 'bgb'])
        gacc = A.alloc([NT, 8], F32)
        self.memset('dve', gacc, 0.0, writes=[pre + 'gacc'])
        wvg = A.alloc([128], BF16)
        gsc = A.alloc([GSC_M], F32)
        kap = gsc[:, 0:64]
        kap2 = gsc[:, 64:128]
        invlam = gsc[:, 128:192]
        rho = gsc[:, 192:256]
        rhon = gsc[:, 256:320]
        G = A.alloc([NT, 8], F32)
        sp = A.alloc([NT, 4], F32)
        cum = A.alloc([64], F32)
        tot = A.alloc([64], F32)
        totn = A.alloc([64], F32)
        e1 = A.alloc([64], F32)
        e2 = A.alloc([64], F32)
        sumn = A.alloc([20], F32)
        mP1 = A.mark()
        hT = A.alloc([KD, TL], BF16)
        hTh = A.alloc([KD, 128], BF16)
        xh = A.alloc([D], F32)
        stg = [A.alloc([TL], BF16) for _ in range(2)]

        wvT = stg[0][:].rearrange("p (a b) -> p a b", a=4)
        for h in range(H):
            self.dma('pool', wvT, wvT_d[h].rearrange("(t p) d -> p t d", p=128), writes=[pre + 'stg0'])
            groups = []
            for dt in range(4):
                groups.append((ps[2][:, (h * 4 + dt) * 8:(h * 4 + dt + 1) * 8],
                               [(wvT[:, et, dt * 128:(dt + 1) * 128], wg[:, (32 + h * 4 + et) * 8:(32 + h * 4 + et + 1) * 8])
                                for et in range(4)]))
            self.mms(groups, reads=[pre + 'stg0', pre + 'wg'], writes=['ps2'])
        self.cp('dve', wvg, ps[2][:, 0:128], reads=['ps2'], writes=[pre + 'wvg'])

        self.dma('sp', xh, xh_d, writes=[pre + 'xh'])
        self.prenorm(hT, pnw, [(self.x[:, t, :], f'x{t}') for t in range(NT)], NT, pre, pre + 'vec')
        self.prenorm(hTh, pnw, [(xh, pre + 'xh')], 1, pre + 'h', pre + 'vec')
        hkeys = [pre + f'hT{t}' for t in range(NT)]

        wblk = [A.alloc([KD, 512], BF16) for _ in range(2)]
        wq = A.alloc([4, DH], BF16)
        wk = A.alloc([4, DH], BF16)
        wv = A.alloc([4, DH], BF16)
        ucT = A.alloc([4, TL], BF16)
        uT = A.alloc([4, TL], BF16)
        uf = A.alloc([TL + 3], F32)
        acc = [A.alloc([512], F32) for _ in range(2)]
        vst = [A.alloc([512], BF16) for _ in range(2)]
        nblk = 0
        nstg = 0
        nv = 0
        bank_rr = 0

        def next_bank():
            nonlocal bank_rr
            b = 3 + bank_rr % 4
            bank_rr += 1
            return b

        ws = self.wstream(wblk, [pre + 'wblk0', pre + 'wblk1'],
                          [w_in[:, i * 512:(i + 1) * 512] for i in range(8)])
        self.dma('pool', wq, wq_d[0].rearrange("(k p) e -> p k e", p=128), writes=[pre + 'wq'])
        self.dma('pool', wk, wk_d[0].rearrange("(k p) e -> p k e", p=128), writes=[pre + 'wk'])
        self.dma('pool', wv, wv_d[0].rearrange("(k p) e -> p k e", p=128), writes=[pre + 'wv'])
        for h in range(H):
            ws.start(h)
            wb, wbk = ws.get(h)
            for ft in range(4):
                ftg = h * 4 + ft
                self.mm(ps[2][:, 128 + ftg * 4:128 + ftg * 4 + 3],
                        [(wb[:, k, ft * 128:(ft + 1) * 128], hTh[:, k, 0:3]) for k in range(KD)],
                        reads=[wbk, pre + 'hhT0'], writes=['ps2'])
                self.cp('act', uf[:, 0:3], ps[2][:, 128 + ftg * 4:128 + ftg * 4 + 3], reads=['ps2'],
                        writes=[pre + 'uf_h'])
                for st4 in range(4):
                    b = next_bank()
                    bk = f'ps{b}'
                    self.mm(ps[b][:], [(wb[:, k, ft * 128:(ft + 1) * 128], hT[:, k, st4 * 512:(st4 + 1) * 512])
                                       for k in range(KD)],
                            reads=[wbk] + hkeys[st4 * 4:(st4 + 1) * 4], writes=[bk])
                    self.cp('act', uf[:, 3 + st4 * 512:3 + (st4 + 1) * 512], ps[b][:], reads=[bk],
                            writes=[pre + f'uf{st4}'])
                    self.cp('dve', uT[:, ft, st4 * 512:(st4 + 1) * 512], uf[:, 3 + st4 * 512:3 + (st4 + 1) * 512],
                            reads=[pre + f'uf{st4}'], writes=[pre + f'uT{st4}'])
                    a = acc[st4 % 2]
                    ak = pre + f'acc{st4 % 2}'
                    rk = [pre + f'uf{st4}', pre + (f'uf{st4 - 1}' if st4 else 'uf_h'), pre + 'vec']
                    o0 = st4 * 512
                    self.ts('dve', a, uf[:, o0:o0 + 512], cw[:, ftg:ftg + 1], None, ALU.mult, reads=rk, writes=[ak])
                    for kk in range(1, 4):
                        self.stt(a, uf[:, o0 + kk:o0 + kk + 512], cw[:, kk * 16 + ftg:kk * 16 + ftg + 1], a,
                                 ALU.mult, ALU.add, reads=rk + [ak], writes=[ak])
                    self.act(ucT[:, ft, st4 * 512:(st4 + 1) * 512], a, AF.Silu, reads=[ak, pre + 'vec'],
                             writes=[pre + f'ucT{st4}'], bias=cb[:, ftg:ftg + 1])
                self.dma('pool', uc_d[:, :, ftg * 128:(ftg + 1) * 128].rearrange("c p t -> p c t"),
                         ucT[:, ft, :].rearrange("p (c t) -> p c t", t=128),
                         reads=[pre + f'ucT{s}' for s in range(4)], writes=[pre + 'uc_d'])
            uck = [pre + f'ucT{s}' for s in range(4)]
            utk = [pre + f'uT{s}' for s in range(4)]
            for which, wsb, wkey, dst, goff in ((0, wq, pre + 'wq', qT_d, 0), (1, wk, pre + 'wk', kT_d, 16)):
                for et in range(4):
                    sg = stg[nstg % 2]
                    sk = pre + f'stg{nstg % 2}'
                    nstg += 1
                    for st4 in range(4):
                        b = next_bank()
                        bk = f'ps{b}'
                        self.mm(ps[b][:], [(wsb[:, k, et * 128:(et + 1) * 128], ucT[:, k, st4 * 512:(st4 + 1) * 512])
                                           for k in range(4)], reads=[wkey, uck[st4]], writes=[bk])
                        self.evac(sg[:, st4 * 512:(st4 + 1) * 512], ps[b][:], reads=[bk], writes=[sk])
                    tile_i = h * 4 + et
                    self.dma('pool', dst[:, :, tile_i * 128:(tile_i + 1) * 128].rearrange("c p t -> p c t"),
                             sg[:].rearrange("p (c t) -> p c t", t=128), reads=[sk], writes=[pre + f'qk_d{which}'])
                    gt = goff + tile_i
                    self.mms([(ps[2][:, tt * 8:(tt + 1) * 8], [(sg[:, tt * 128:(tt + 1) * 128], wg[:, gt * 8:(gt + 1) * 8])])
                              for tt in range(NT)], reads=[sk, pre + 'wg'], writes=['ps2'])
                    self.tt('dve', gacc[:].rearrange("p a b -> p (a b)"), gacc[:].rearrange("p a b -> p (a b)"),
                            ps[2][:, 0:128], ALU.add, reads=['ps2', pre + 'gacc'], writes=[pre + 'gacc'])
            if h + 1 < H:
                self.dma('pool', wq, wq_d[h + 1].rearrange("(k p) e -> p k e", p=128), writes=[pre + 'wq'])
                self.dma('pool', wk, wk_d[h + 1].rearrange("(k p) e -> p k e", p=128), writes=[pre + 'wk'])
            for tt in range(NT):
                b = next_bank()
                bk = f'ps{b}'
                self.mm(ps[b][:], [(uT[:, k, tt * 128:(tt + 1) * 128], wv[:, k, :]) for k in range(4)],
                        reads=[pre + 'wv', utk[tt // 4]], writes=[bk])
                vs = vst[nv % 2]
                vk = pre + f'vst{nv % 2}'
                nv += 1
                self.evac(vs, ps[b][:], reads=[bk], writes=[vk])
                self.dma('pool', v_d[tt, :, h * 512:(h + 1) * 512], vs, reads=[vk], writes=[pre + 'v_d'])
            if h + 1 < H:
                self.dma('pool', wv, wv_d[h + 1].rearrange("(k p) e -> p k e", p=128), writes=[pre + 'wv'])
            self.mms([(ps[2][:, tt * 8:(tt + 1) * 8],
                       [(uT[:, k, tt * 128:(tt + 1) * 128], wvg[:, (h * 4 + k) * 8:(h * 4 + k + 1) * 8]) for k in range(4)])
                      for tt in range(NT)], reads=utk + [pre + 'wvg'], writes=['ps2'])
            self.tt('dve', gacc[:].rearrange("p a b -> p (a b)"), gacc[:].rearrange("p a b -> p (a b)"),
                    ps[2][:, 0:128], ALU.add, reads=['ps2', pre + 'gacc'], writes=[pre + 'gacc'])

        for zb in range(4):
            ws.start(4 + zb)
            wb, wbk = ws.get(4 + zb)
            for ft in range(4):
                sg = stg[nstg % 2]
                sk = pre + f'stg{nstg % 2}'
                nstg += 1
                for st4 in range(4):
                    b = next_bank()
                    bk = f'ps{b}'
                    self.mm(ps[b][:], [(wb[:, k, ft * 128:(ft + 1) * 128], hT[:, k, st4 * 512:(st4 + 1) * 512])
                                       for k in range(KD)], reads=[wbk] + hkeys[st4 * 4:(st4 + 1) * 4], writes=[bk])
                    self.act(sg[:, st4 * 512:(st4 + 1) * 512], ps[b][:], AF.Silu, reads=[bk], writes=[sk])
                tile_i = zb * 4 + ft
                self.dma('pool', sz_d[:, :, tile_i * 128:(tile_i + 1) * 128].rearrange("c p t -> p c t"),
                         sg[:].rearrange("p (c t) -> p c t", t=128), reads=[sk], writes=[pre + 'sz_d'])

        gk = pre + 'gs'
        self.tt('dve', G, gacc, bgb, ALU.add, reads=[pre + 'gacc', pre + 'bgb'], writes=[gk])
        self.act(sp, G[:, :, 4:8], AF.Exp, reads=[gk], writes=[gk], scale=-1.0)
        self.act(sp, sp, AF.Ln, reads=[gk], writes=[gk], bias=1.0)
        spf = sp[:].rearrange("p a b -> p (a b)")
        self.mms([(ps[2][:, c * 4:(c + 1) * 4], [(self.tri, spf[:, c * 4:(c + 1) * 4])]) for c in range(NT)] +
                 [(ps[2][:, 64 + c * 4:64 + (c + 1) * 4], [(self.ones, spf[:, c * 4:(c + 1) * 4])]) for c in range(NT)],
                 reads=[gk, 'cst'], writes=['ps2'])
        self.cp('dve', cum, ps[2][:, 0:64], reads=['ps2'], writes=[gk])
        self.cp('dve', tot, ps[2][:, 64:128], reads=['ps2'], writes=[gk])
        self.memset('dve', totn, 0.0, writes=[gk])
        self.cp('dve', totn[:, 0:60], tot[:, 4:64], reads=[gk], writes=[gk])
        li = G[:, :, 0:4]
        e1v = e1[:].rearrange("p (a b) -> p a b", b=4)
        self.tt('dve', e1v, li, cum[:].rearrange("p (a b) -> p a b", b=4), ALU.add, reads=[gk], writes=[gk])
        self.tt('dve', e1, e1, tot, ALU.subtract, reads=[gk], writes=[gk])
        lnsc = math.log(DH ** -0.5)
        self.act(kap, e1, AF.Exp, reads=[gk], writes=[gk], bias=lnsc)
        self.tt('dve', e2, e1, totn, ALU.subtract, reads=[gk], writes=[gk])
        self.act(kap2, e2, AF.Exp, reads=[gk], writes=[gk], bias=lnsc)
        self.tt('dve', e2, cum, tot, ALU.subtract, reads=[gk], writes=[gk])
        self.act(invlam, e2, AF.Exp, reads=[gk], writes=[gk])
        self.act(rho, tot, AF.Exp, reads=[gk], writes=[gk], scale=-1.0)
        self.act(rhon, totn, AF.Exp, reads=[gk], writes=[gk], scale=-1.0)
        self.P.add('dve', lambda e: e.tensor_reduce(out=sumn[:, 16:20], in_=tot[:].rearrange("p (c h) -> p h c", h=4),
                                                    axis=AX.X, op=ALU.add), [gk], [pre + 'sumn'])
        self.outs.append(self.dma('sp', gsc_d, gsc, reads=[gk], writes=[pre + 'gsc_d']))

        A.release(mP1)
        self.P.barrier()
        S = A.alloc([NFT, DH], F32)
        nst = sumn[:, 0:16]
        self.memset('dve', S, 0.0, writes=[pre + f'S{i}' for i in range(NFT)])
        self.memset('dve', nst, 0.0, writes=[pre + 'sumn'])
        self.scan_m(pre, kT_d, v_d, None, gsc, S, None, nst, None, summary_only=True)
        for i in range(NFT):
            self.outs.append(self.dma('sp', sumS_d[i * 128:(i + 1) * 128, :], S[:, i, :], reads=[pre + f'S{i}'],
                                      writes=[pre + 'sumS_d']))
        self.outs.append(self.dma('sp', sumn_d, sumn, reads=[pre + 'sumn'], writes=[pre + 'sumn_d']))

    def scan_m(self, pre, kT_d, v_d, qT_d, gsc, S, Sb, nst, nb, summary_only, extra=None):
        A = self.A
        ps = self.ps
        kap = gsc[:, 0:64]
        kap2 = gsc[:, 64:128]
        invlam = gsc[:, 128:192]
        rhon = gsc[:, 256:320]
        gk = pre + 'gs'
        kc = [A.alloc([NFT, 128], BF16) for _ in range(2)]
        vc = [A.alloc([E], BF16) for _ in range(2)]
        kt = [A.alloc([E], BF16) for _ in range(2)]
        if not summary_only:
            qc = [A.alloc([NFT, 128], BF16) for _ in range(2)]
            sTb = [A.alloc([512], BF16) for _ in range(2)]
            hn = [A.alloc([DH], F32) for _ in range(2)]
            sm = A.alloc([H, 16], F32)
            (szc, ucc, yc, y_d, nw, skp) = extra
        dsr = 0
        pending = None
        for c in range(NT):
            s = c % 2
            kck, vck, ktk = pre + f'kc{s}', pre + f'vc{s}', pre + f'kt{s}'
            self.dma('sp', kc[s][:].rearrange("p a b -> p (a b)"), kT_d[c], reads=[pre + 'qk_d1'], writes=[kck])
            self.dma('sp', vc[s], v_d[c], reads=[pre + 'v_d'], writes=[vck])
            if not summary_only:
                qck = pre + f'qc{s}'
                self.dma('sp', qc[s][:].rearrange("p a b -> p (a b)"), qT_d[c], reads=[pre + 'qk_d0'], writes=[qck])
                self.dma('sp', szc[s][:].rearrange("p a b -> p (a b)"), extra_sz(self, pre)[c], reads=[pre + 'sz_d'],
                         writes=[pre + f'szc{s}'])
                self.dma('sp', ucc[s][:].rearrange("p a b -> p (a b)"), extra_uc(self, pre)[c], reads=[pre + 'uc_d'],
                         writes=[pre + f'ucc{s}'])
                self.mms([(ps[0][:, h * 128:(h + 1) * 128],
                           [(kc[s][:, h * 4 + dt, :], qc[s][:, h * 4 + dt, :]) for dt in range(4)]) for h in range(H)],
                         reads=[kck, qck], writes=['ps0'])
                for h in range(H):
                    self.stt(sTb[s][:, h * 128:(h + 1) * 128], ps[0][:, h * 128:(h + 1) * 128],
                             kap[:, c * 4 + h:c * 4 + h + 1], self.tri, ALU.mult, ALU.mult,
                             reads=['ps0', gk, 'cst'], writes=[pre + f'sTb{s}'])
            for r in range(2):
                self.transposes([(self.psb[:, i * 128:(i + 1) * 128], kc[s][:, r * 8 + i, :]) for i in range(8)],
                                self.identb, reads=[kck, 'identb'], writes=['psb'])
                for hh in range(2):
                    h = r * 2 + hh
                    self.act(kt[s][:, h * 512:(h + 1) * 512], self.psb[:, hh * 512:(hh + 1) * 512], AF.Copy,
                             reads=['psb', gk], writes=[ktk], scale=kap2[:, c * 4 + h:c * 4 + h + 1])
            for h in range(H):
                vh = vc[s][:, h * 512:(h + 1) * 512]
                if not summary_only:
                    nb_ = 3 + h % 2
                    nbk = f'ps{nb_}'
                    self.mm(ps[nb_][:], [(sTb[s][:, h * 128:(h + 1) * 128], vh)] +
                            [(qc[s][:, h * 4 + dt, :], Sb[:, h * 4 + dt, :]) for dt in range(4)],
                            reads=[pre + f'sTb{s}', vck, qck] + [pre + f'Sb{h * 4 + dt}' for dt in range(4)],
                            writes=[nbk])
                    self.mm(ps[2][:, 64 + h:64 + h + 1], [(sTb[s][:, h * 128:(h + 1) * 128], self.onesb[:, 0:1])] +
                            [(qc[s][:, h * 4 + dt, :], nb[:, h * 4 + dt:h * 4 + dt + 1]) for dt in range(4)],
                            reads=[pre + f'sTb{s}', qck, 'onesb', pre + 'nb'], writes=['ps2'])
                for dt in range(4):
                    i = h * 4 + dt
                    db = 5 + dsr % 2
                    dsr += 1
                    dbk = f'ps{db}'
                    self.mm(ps[db][:], [(kt[s][:, i * 128:(i + 1) * 128], vh)], reads=[ktk, vck], writes=[dbk])
                    self.stt(S[:, i, :], S[:, i, :], rhon[:, c * 4 + h:c * 4 + h + 1], ps[db][:], ALU.mult, ALU.add,
                             reads=[dbk, gk, pre + f'S{i}'], writes=[pre + f'S{i}'])
                    if not summary_only:
                        self.cp('act', Sb[:, i, :], S[:, i, :], reads=[pre + f'S{i}'], writes=[pre + f'Sb{i}'])
                self.mms([(ps[2][:, h * 4 + dt:h * 4 + dt + 1], [(kt[s][:, (h * 4 + dt) * 128:(h * 4 + dt + 1) * 128],
                                                                  self.onesb[:, 0:1])]) for dt in range(4)],
                         reads=[ktk, 'onesb'], writes=['ps2'])
                self.stt(nst[:, h * 4:(h + 1) * 4], nst[:, h * 4:(h + 1) * 4], rhon[:, c * 4 + h:c * 4 + h + 1],
                         ps[2][:, h * 4:(h + 1) * 4], ALU.mult, ALU.add, reads=['ps2', gk, pre + 'sumn'],
                         writes=[pre + 'sumn'])
                if not summary_only:
                    self.cp('dve', nb[:, h * 4:(h + 1) * 4], nst[:, h * 4:(h + 1) * 4], reads=[pre + 'sumn'],
                            writes=[pre + 'nb'])
                    p2 = self.head_out_m(pre, c, s, h, ps[nb_], nbk, sm, invlam, hn[h % 2], pre + f'hn{h % 2}',
                                         szc[s], ucc[s], yc[s], nw, skp, y_d)
                    if pending is not None:
                        pending()
                    pending = p2
        if pending is not None:
            pending()

    def head_out_m(self, pre, c, s, h, numb, nbk, sm, invlam, hn, hnk, szc, ucc, yc, nw, skp, y_d):
        ps = self.ps
        gk = pre + 'gs'
        smk = pre + 'sm'
        v = sm[:, h, :]
        self.cp('dve', v[:, 14:15], ps[2][:, 64 + h:64 + h + 1], reads=['ps2'], writes=[smk])
        self.ts('dve', v[:, 0:1], v[:, 14:15], -1.0, v[:, 14:15], ALU.mult, ALU.max, reads=[smk], writes=[smk])
        self.tt('dve', v[:, 0:1], v[:, 0:1], invlam[:, c * 4 + h:c * 4 + h + 1], ALU.max, reads=[smk, gk], writes=[smk])
        self.P.add('dve', lambda e: e.reciprocal(out=v[:, 1:2], in_=v[:, 0:1]), [smk], [smk])
        self.P.add('dve', lambda e: e.bn_stats(out=v[:, 2:8], in_=numb[:]), [nbk], [smk])
        self.P.add('dve', lambda e: e.bn_aggr(out=v[:, 8:10], in_=v[:, 2:8]), [smk], [smk])
        self.ts('dve', v[:, 10:11], v[:, 9:10], v[:, 1:2], v[:, 1:2], ALU.mult, ALU.mult, reads=[smk], writes=[smk])
        self.ts('dve', v[:, 10:11], v[:, 10:11], EPS, None, ALU.add, reads=[smk], writes=[smk])
        self.act(v[:, 10:11], v[:, 10:11], AF.Sqrt, reads=[smk], writes=[smk])
        self.P.add('dve', lambda e: e.reciprocal(out=v[:, 11:12], in_=v[:, 10:11]), [smk], [smk])
        self.tt('dve', v[:, 12:13], v[:, 11:12], v[:, 1:2], ALU.mult, reads=[smk], writes=[smk])
        self.ts('dve', v[:, 13:14], v[:, 8:9], v[:, 12:13], -1.0, ALU.mult, ALU.mult, reads=[smk], writes=[smk])
        self.act(hn, numb[:], AF.Identity, reads=[nbk, smk], writes=[hnk], bias=v[:, 13:14], scale=v[:, 12:13])

        def part2():
            self.transposes([(ps[7][:, i * 128:(i + 1) * 128], hn[:, i * 128:(i + 1) * 128]) for i in range(4)],
                            self.ident, reads=[hnk, 'cst'], writes=['ps7'])
            yk = pre + f'yc{s}'
            for i in range(4):
                ft = h * 4 + i
                tmp = self.gt[(h * 4 + i) % 2]
                tk = pre + f'gt{(h * 4 + i) % 2}'
                self.act(tmp, ps[7][:, i * 128:(i + 1) * 128], AF.Copy, reads=['ps7', pre + 'vec'], writes=[tk],
                         scale=nw[:, ft:ft + 1])
                self.stt(tmp, ucc[:, ft, :], skp[:, ft:ft + 1], tmp, ALU.mult, ALU.add,
                         reads=[tk, pre + f'ucc{s}', pre + 'vec'], writes=[tk])
                self.tt('dve', yc[:, ft, :], tmp, szc[:, ft, :], ALU.mult, reads=[tk, pre + f'szc{s}'], writes=[yk])
            if h == H - 1:
                self.dma('pool', y_d[c], yc[:].rearrange("p a b -> p (a b)"), reads=[yk], writes=[pre + 'y_d'])
        return part2

    def stage_B(self, pre, mix):
        A = self.A
        ps = self.ps
        IN, OUT = "ExternalInput", "ExternalOutput"
        ntile = NFT if mix == 'm' else NG
        nsm = 20 if mix == 'm' else 8
        v_d = self.dram(pre + "v", [NT, 128, E], BF16, IN)
        sz_d = self.dram(pre + "sz", [NT, 128, NFT * 128], BF16, IN)
        sumS_all = self.dram(pre + "sumS_all", [NCORES, ntile * 128, DH], F32, IN)
        sumn_all = self.dram(pre + "sumn_all", [128, NCORES * nsm], F32, IN)
        wout_d = self.dram(pre + "w_out", [E, D], F32, IN)
        postw_d = self.dram(pre + "post_w", [1, D], F32, IN)
        xo_d = self.dram(pre + "xo", [TL, D], F32, OUT)
        y_d = self.dram(pre + "y", [NT, 128, NFT * 128], BF16, "Internal")
        self._sz_d = sz_d
        if mix == 'm':
            qT_d = self.dram(pre + "qT", [NT, 128, NFT * 128], BF16, IN)
            kT_d = self.dram(pre + "kT", [NT, 128, NFT * 128], BF16, IN)
            uc_d = self.dram(pre + "uc", [NT, 128, NFT * 128], BF16, IN)
            gsc_d = self.dram(pre + "gsc", [128, GSC_M], F32, IN)
            vec_d = self.dram(pre + "vec", [128, NVM], F32, IN)
            self._uc_d = uc_d
            nvec = NVM
        else:
            qg_d = self.dram(pre + "qg", [NT, 128, NG * 128], BF16, IN)
            kg_d = self.dram(pre + "kg", [NT, 128, NG * 128], BF16, IN)
            kh_d = self.dram(pre + "kh", [NT, 128, NG * 128], BF16, IN)
            egl_d = self.dram(pre + "egl", [128, NG * NT], F32, IN)
            vec_d = self.dram(pre + "vec", [128, NVG], F32, IN)
            nvec = NVG
        vec = A.alloc([nvec], F32)
        self.dma('sp', vec, vec_d, writes=[pre + 'vec'])
        gk = pre + 'gs'
        if mix == 'm':
            gsc = A.alloc([GSC_M], F32)
            self.dma('sp', gsc, gsc_d, writes=[gk])
            nw = vec[:, 88:104]
            skp = vec[:, 104:120]
        else:
            egl = A.alloc([NG, NT], F32)
            self.dma('sp', egl[:].rearrange("p a b -> p (a b)"), egl_d, writes=[gk])
            nw = vec[:, 16:32]
        S = A.alloc([ntile, DH], F32)
        Sb = A.alloc([ntile, DH], BF16)
        sna = A.alloc([NCORES, nsm], F32)
        mj = A.alloc([NCORES, 8], F32)
        self.dma('sp', sna[:].rearrange("p a b -> p (a b)"), sumn_all, writes=[pre + 'sna'])
        nd = 4 if mix == 'm' else 8
        dec = sna[:, :, 16:20] if mix == 'm' else sna[:, :, 0:8]
        self.act(mj[:, :, 0:nd], dec, AF.Exp, reads=[pre + 'sna'], writes=[pre + 'mj'],
                 scale=(-1.0 if mix == 'm' else -1.0 / TAU))
        self.ts('dve', mj[:, :, 0:nd], mj[:, :, 0:nd], -1.0, None, ALU.add, reads=[pre + 'mj'], writes=[pre + 'mj'])
        for j in range(NCORES):
            self.ts('dve', mj[:, j, 0:nd], mj[:, j, 0:nd], self.sel[:, j:j + 1], 1.0, ALU.mult, ALU.add,
                    reads=[pre + 'mj', 'cst'], writes=[pre + 'mj'])
        Skeys = [pre + f'S{i}' for i in range(ntile)]
        self.memset('dve', S, 0.0, writes=Skeys)
        if mix == 'm':
            nst = A.alloc([16], F32)
            nb = A.alloc([16], BF16)
            self.memset('dve', nst, 0.0, writes=[pre + 'sumn'])
        m0 = A.mark()
        gld = [A.alloc([4, DH], F32) for _ in range(2)]
        nl = 0
        for j in range(NCORES - 1):
            for q4 in range(ntile // 4):
                g = gld[nl % 2]
                gkey = pre + f'gld{nl % 2}'
                nl += 1
                self.dma('sp', g, sumS_all[j, q4 * 512:(q4 + 1) * 512, :].rearrange("(a p) d -> p a d", p=128),
                         writes=[gkey])
                for a4 in range(4):
                    i = q4 * 4 + a4
                    hh = (i // 4) if mix == 'm' else i
                    self.act(g[:, a4, :], g[:, a4, :], AF.Copy, reads=[gkey, 'cst'], writes=[gkey],
                             scale=self.sel[:, j:j + 1])
                    self.stt(S[:, i, :], S[:, i, :], mj[:, j, hh:hh + 1], g[:, a4, :], ALU.mult, ALU.add,
                             reads=[gkey, pre + 'mj', Skeys[i]], writes=[Skeys[i]])
            if mix == 'm':
                for h in range(H):
                    self.ts('dve', sna[:, j, h * 4:(h + 1) * 4], sna[:, j, h * 4:(h + 1) * 4], self.sel[:, j:j + 1],
                            None, ALU.mult, reads=[pre + 'sna', 'cst'], writes=[pre + 'sna'])
                    self.stt(nst[:, h * 4:(h + 1) * 4], nst[:, h * 4:(h + 1) * 4], mj[:, j, h:h + 1],
                             sna[:, j, h * 4:(h + 1) * 4], ALU.mult, ALU.add, reads=[pre + 'sna', pre + 'mj', pre + 'sumn'],
                             writes=[pre + 'sumn'])
        A.release(m0)
        self.P.barrier()
        if mix == 'm':
            rho = gsc[:, 192:256]
            for i in range(ntile):
                h = i // 4
                self.ts('dve', S[:, i, :], S[:, i, :], rho[:, h:h + 1], None, ALU.mult, reads=[Skeys[i], gk],
                        writes=[Skeys[i]])
            for h in range(H):
                self.ts('dve', nst[:, h * 4:(h + 1) * 4], nst[:, h * 4:(h + 1) * 4], rho[:, h:h + 1], None, ALU.mult,
                        reads=[pre + 'sumn', gk], writes=[pre + 'sumn'])
            self.cp('dve', nb, nst, reads=[pre + 'sumn'], writes=[pre + 'nb'])
        for i in range(ntile):
            self.cp('act', Sb[:, i, :], S[:, i, :], reads=[Skeys[i]], writes=[pre + f'Sb{i}'])

        m1 = A.mark()
        szc = [A.alloc([NFT, 128], BF16) for _ in range(2)]
        yc = [A.alloc([NFT, 128], BF16) for _ in range(2)]
        self.gt = [A.alloc([128], F32) for _ in range(2)]
        if mix == 'm':
            ucc = [A.alloc([NFT, 128], BF16) for _ in range(2)]
            self.scan_m(pre, kT_d, v_d, qT_d, gsc, S, Sb, nst, nb, summary_only=False,
                        extra=(szc, ucc, yc, y_d, nw, skp))
        else:
            self.scan_g(pre, qg_d, kg_d, kh_d, v_d, egl, S, Sb, summary_only=False, extra=(szc, yc, y_d, nw))
        A.release(m1)
        self.P.barrier()

        wo = A.alloc([NFT, D], BF16)
        for q4 in range(4):
            self.dma('pool', wo[:, q4 * 4:(q4 + 1) * 4, :],
                     wout_d[q4 * 512:(q4 + 1) * 512, :].rearrange("(k p) d -> p k d", p=128), writes=[pre + 'wo'])
        pw = A.alloc([D], F32)
        self.dma('sp', pw, bass.AP(postw_d.tensor, 0, [[0, 128], [1, D]]), writes=[pre + 'pw'])
        yl = [A.alloc([NFT, 128], BF16) for _ in range(4)]
        tmp = [A.alloc([512], F32) for _ in range(8)]
        junk = A.alloc([512], BF16)
        s2 = A.alloc([NT, 4], F32)
        fin = []
        pbanks = [(ps[3][:], 'ps3'), (ps[4][:], 'ps4'), (ps[5][:], 'ps5'), (ps[6][:], 'ps6'),
                  (ps[0][:], 'ps0'), (ps[7][:], 'ps7'), (ps[2][:], 'ps2'), (self.psb[:].bitcast(F32), 'psb')]
        for c in range(NT):
            s = c % 4
            self.dma('sp', yl[s][:].rearrange("p a b -> p (a b)"), y_d[c], reads=[pre + 'y_d'], writes=[pre + f'yl{s}'])
            for half in range(2):
                bap, bkey = pbanks[2 * s + half]
                self.mm(bap, [(yl[s][:, ft, :], wo[:, ft, half * 512:(half + 1) * 512]) for ft in range(NFT)],
                        reads=[pre + f'yl{s}', pre + 'wo'], writes=[bkey])
                self.act(junk, bap, AF.Square, reads=[bkey], writes=[pre + f's2_{c}'],
                         accum=s2[:, c, half:half + 1])
            sk2 = pre + f's2_{c}'
            self.tt('dve', s2[:, c, 2:3], s2[:, c, 0:1], s2[:, c, 1:2], ALU.add, reads=[sk2], writes=[sk2])
            self.ts('dve', s2[:, c, 2:3], s2[:, c, 2:3], 1.0 / D, EPS, ALU.mult, ALU.add, reads=[sk2], writes=[sk2])
            self.act(s2[:, c, 2:3], s2[:, c, 2:3], AF.Sqrt, reads=[sk2], writes=[sk2])
            self.P.add('dve', lambda e, c=c: e.reciprocal(out=s2[:, c, 3:4], in_=s2[:, c, 2:3]), [sk2], [sk2])
            for half in range(2):
                bap, bkey = pbanks[2 * s + half]
                t_ = tmp[2 * s + half]
                tkey = pre + f'tmp{2 * s + half}'
                self.stt(t_, bap, s2[:, c, 3:4], pw[:, half * 512:(half + 1) * 512], ALU.mult, ALU.mult,
                         reads=[bkey, sk2, pre + 'pw'], writes=[tkey])
                self.tt('dve', self.x[:, c, half * 512:(half + 1) * 512], self.x[:, c, half * 512:(half + 1) * 512], t_,
                        ALU.add, reads=[tkey, f'x{c}'], writes=[f'x{c}'])
            fin.append(self.dma('sp', xo_d[c * 128:(c + 1) * 128, :], self.x[:, c, :], reads=[f'x{c}'],
                                writes=[pre + 'xo_d']))
        return fin

    def stage_A_g(self, pre):
        A = self.A
        ps = self.ps
        IN, OUT = "ExternalInput", "ExternalOutput"
        GW = 2 * 1024 + 2 * E + 16
        w_in = self.dram(pre + "w_in", [D, GW], F32, IN)
        wgu_d = self.dram(pre + "wgu", [16, 1024], F32, IN)
        vec_d = self.dram(pre + "vec", [128, NVG], F32, IN)
        qg_d = self.dram(pre + "qg", [NT, 128, NG * 128], BF16, OUT)
        kg_d = self.dram(pre + "kg", [NT, 128, NG * 128], BF16, OUT)
        kh_d = self.dram(pre + "kh", [NT, 128, NG * 128], BF16, OUT)
        v_d = self.dram(pre + "v", [NT, 128, E], BF16, OUT)
        sz_d = self.dram(pre + "sz", [NT, 128, NFT * 128], BF16, OUT)
        egl_d = self.dram(pre + "egl", [128, NG * NT], F32, OUT)
        sumS_d = self.dram(pre + "sumS", [NG * 128, DH], F32, OUT)
        sumn_d = self.dram(pre + "sumn", [128, 8], F32, OUT)

        vec = A.alloc([NVG], F32)
        pnw = vec[:, 0:8]
        nbg = vec[:, 8:16]
        self.dma('sp', vec, vec_d, writes=[pre + 'vec'])
        egl = A.alloc([NG, NT], F32)
        gseg = A.alloc([NG], F32)
        mP1 = A.mark()
        hT = A.alloc([KD, TL], BF16)
        self.prenorm(hT, pnw, [(self.x[:, t, :], f'x{t}') for t in range(NT)], NT, pre, pre + 'vec')
        hkeys = [pre + f'hT{t}' for t in range(NT)]
        wr = A.alloc([KD, 16], BF16)
        self.dma('pool', wr, w_in[:, GW - 16:GW].rearrange("(k p) c -> p k c", p=128), writes=[pre + 'wr'])
        wgu = A.alloc([1024], F32)
        self.dma('sp', wgu[0:16, :], wgu_d, writes=[pre + 'wgu'])
        r_sb = A.alloc([TL], F32)
        for st4 in range(4):
            self.mm(ps[2][0:16, :], [(wr[:, k, :], hT[:, k, st4 * 512:(st4 + 1) * 512]) for k in range(KD)],
                    reads=[pre + 'wr'] + hkeys[st4 * 4:(st4 + 1) * 4], writes=['ps2'])
            self.cp('act', r_sb[0:16, st4 * 512:(st4 + 1) * 512], ps[2][0:16, :], reads=['ps2'], writes=[pre + 'r'])
        spt = A.alloc([TL], F32)
        cs = A.alloc([TL], F32)
        ex = [A.alloc([TL], F32) for _ in range(3)]
        cl = A.alloc([NT], F32)
        ncl = A.alloc([NT], F32)
        wblk = [A.alloc([KD, 512], BF16) for _ in range(3)]
        wkeys = [pre + f'wblk{i}' for i in range(3)]
        srcs = [w_in[:, 0:512], w_in[:, 1024:1536], w_in[:, 512:1024], w_in[:, 1536:2048]] + \
            [w_in[:, 2048 + i * 512:2048 + (i + 1) * 512] for i in range(8)]
        ws = self.wstream(wblk, wkeys, srcs)
        stg = [A.alloc([TL], BF16) for _ in range(3)]
        vst = [A.alloc([512], BF16) for _ in range(2)]
        bank_rr = 0

        def next_bank():
            nonlocal bank_rr
            b = 3 + bank_rr % 4
            bank_rr += 1
            return b
        lq = math.log(GK ** -0.5)
        gk = pre + 'gs'
        for blk in range(2):
            ws.start(2 * blk)
            wqb, wqk_ = ws.get(2 * blk)
            wkb, wkk_ = ws.get(2 * blk + 1)
            for gi in range(4):
                g = blk * 4 + gi
                for st4 in range(4):
                    b = next_bank()
                    self.mm(ps[b][:], [(wgu[0:16, g * 128:(g + 1) * 128], r_sb[0:16, st4 * 512:(st4 + 1) * 512])],
                            reads=[pre + 'wgu', pre + 'r'], writes=[f'ps{b}'])
                    self.act(spt[:, st4 * 512:(st4 + 1) * 512], ps[b][:], AF.Exp, reads=[f'ps{b}', pre + 'vec'],
                             writes=[pre + 'spt'], bias=nbg[:, g:g + 1], scale=-1.0)
                self.act(spt, spt, AF.Ln, reads=[pre + 'spt'], writes=[pre + 'spt'], bias=1.0)
                for c in range(NT):
                    self.P.add('dve', lambda e, c=c: e.tensor_tensor_scan(
                        out=cs[:, c * 128:(c + 1) * 128], data0=self.ones, data1=spt[:, c * 128:(c + 1) * 128],
                        initial=0.0, op0=ALU.mult, op1=ALU.add), [pre + 'spt', 'cst'], [pre + 'cs'])
                self.cp('dve', cl[:].rearrange("p (c o) -> p c o", o=1), cs[:].rearrange("p (c t) -> p c t", t=128)[:, :, 127:128], reads=[pre + 'cs'],
                        writes=[pre + 'cl'])
                self.ts('dve', ncl, cl, -1.0 / TAU, None, ALU.mult, reads=[pre + 'cl'], writes=[pre + 'cl'])
                self.act(egl[:, g, :], cl, AF.Exp, reads=[pre + 'cl'], writes=[gk], scale=-1.0 / TAU)
                self.P.add('dve', lambda e, g=g: e.tensor_reduce(out=gseg[:, g:g + 1], in_=cl, axis=AX.X, op=ALU.add),
                           [pre + 'cl'], [pre + 'gseg'])
                self.act(ex[0], cs, AF.Exp, reads=[pre + 'cs'], writes=[pre + 'ex0'], scale=-1.0 / TAU, bias=lq)
                self.act(ex[1], cs, AF.Exp, reads=[pre + 'cs'], writes=[pre + 'ex1'], scale=1.0 / TAU)
                for c in range(NT):
                    self.act(ex[2][:, c * 128:(c + 1) * 128], cs[:, c * 128:(c + 1) * 128], AF.Exp,
                             reads=[pre + 'cs', pre + 'cl'], writes=[pre + 'ex2'], scale=1.0 / TAU, bias=ncl[:, c:c + 1])
                for st4 in range(4):
                    sl = slice(st4 * 512, (st4 + 1) * 512)
                    b = next_bank()
                    self.mm(ps[b][:], [(wqb[:, k, gi * 128:(gi + 1) * 128], hT[:, k, sl]) for k in range(KD)],
                            reads=[wqk_] + hkeys[st4 * 4:(st4 + 1) * 4], writes=[f'ps{b}'])
                    self.tt('dve', stg[0][:, sl], ps[b][:], ex[0][:, sl], ALU.mult, reads=[f'ps{b}', pre + 'ex0'],
                            writes=[pre + 'stg0'])
                    b = next_bank()
                    self.mm(ps[b][:], [(wkb[:, k, gi * 128:(gi + 1) * 128], hT[:, k, sl]) for k in range(KD)],
                            reads=[wkk_] + hkeys[st4 * 4:(st4 + 1) * 4], writes=[f'ps{b}'])
                    self.tt('dve', stg[1][:, sl], ps[b][:], ex[1][:, sl], ALU.mult, reads=[f'ps{b}', pre + 'ex1'],
                            writes=[pre + 'stg1'])
                    self.tt('dve', stg[2][:, sl], ps[b][:], ex[2][:, sl], ALU.mult, reads=[f'ps{b}', pre + 'ex2'],
                            writes=[pre + 'stg2'])
                for w_, dst in enumerate((qg_d, kg_d, kh_d)):
                    self.dma('pool', dst[:, :, g * 128:(g + 1) * 128].rearrange("c p t -> p c t"),
                             stg[w_][:].rearrange("p (c t) -> p c t", t=128), reads=[pre + f'stg{w_}'],
                             writes=[pre + f'qk_d{w_}'])
        self.outs.append(self.dma('sp', egl_d, egl[:].rearrange("p a b -> p (a b)"), reads=[gk], writes=[pre + 'egl_d']))
        self.outs.append(self.dma('sp', sumn_d, gseg, reads=[pre + 'gseg'], writes=[pre + 'sumn_d']))
        nblk = 0
        nv = 0
        for vb in range(4):
            ws.start(4 + vb)
            wb, wbk = ws.get(4 + vb)
            for tt in range(NT):
                b = next_bank()
                self.mm(ps[b][:], [(hT[:, k, tt * 128:(tt + 1) * 128], wb[:, k, :]) for k in range(KD)],
                        reads=[wbk, hkeys[tt]], writes=[f'ps{b}'])
                vs = vst[nv % 2]
                vk = pre + f'vst{nv % 2}'
                nv += 1
                self.evac(vs, ps[b][:], reads=[f'ps{b}'], writes=[vk])
                self.dma('pool', v_d[tt, :, vb * 512:(vb + 1) * 512], vs, reads=[vk], writes=[pre + 'v_d'])
        nstg = 0
        for zb in range(4):
            ws.start(8 + zb)
            wb, wbk = ws.get(8 + zb)
            for ft in range(4):
                sg = stg[nstg % 2]
                sk = pre + f'stg{nstg % 2}'
                nstg += 1
                for st4 in range(4):
                    b = next_bank()
                    self.mm(ps[b][:], [(wb[:, k, ft * 128:(ft + 1) * 128], hT[:, k, st4 * 512:(st4 + 1) * 512])
                                       for k in range(KD)], reads=[wbk] + hkeys[st4 * 4:(st4 + 1) * 4], writes=[f'ps{b}'])
                    self.act(sg[:, st4 * 512:(st4 + 1) * 512], ps[b][:], AF.Silu, reads=[f'ps{b}'], writes=[sk])
                tile_i = zb * 4 + ft
                self.dma('pool', sz_d[:, :, tile_i * 128:(tile_i + 1) * 128].rearrange("c p t -> p c t"),
                         sg[:].rearrange("p (c t) -> p c t", t=128), reads=[sk], writes=[pre + 'sz_d'])
        A.release(mP1)
        self.P.barrier()
        S = A.alloc([NG, DH], F32)
        self.memset('dve', S, 0.0, writes=[pre + f'S{i}' for i in range(NG)])
        self.scan_g(pre, None, None, kh_d, v_d, egl, S, None, summary_only=True)
        for i in range(NG):
            self.outs.append(self.dma('sp', sumS_d[i * 128:(i + 1) * 128, :], S[:, i, :], reads=[pre + f'S{i}'],
                                      writes=[pre + 'sumS_d']))

    def scan_g(self, pre, qg_d, kg_d, kh_d, v_d, egl, S, Sb, summary_only, extra=None):
        A = self.A
        ps = self.ps
        gk = pre + 'gs'
        khc = [A.alloc([NG, 128], BF16) for _ in range(2)]
        vc = [A.alloc([E], BF16) for _ in range(2)]
        kh = [A.alloc([1024], BF16) for _ in range(2)]
        if not summary_only:
            qc = [A.alloc([NG, 128], BF16) for _ in range(2)]
            kc = [A.alloc([NG, 128], BF16) for _ in range(2)]
            Ab = [A.alloc([512], BF16) for _ in range(2)]
            on = [A.alloc([DH], F32) for _ in range(2)]
            sm = A.alloc([H, 8], F32)
            junk = A.alloc([DH], F32)
            (szc, yc, y_d, nw) = extra
        dsr = 0
        pending = None
        for c in range(NT):
            s = c % 2
            khk, vck, kk = pre + f'khc{s}', pre + f'vc{s}', pre + f'kh{s}'
            self.dma('sp', khc[s][:].rearrange("p a b -> p (a b)"), kh_d[c], reads=[pre + 'qk_d2'], writes=[khk])
            self.dma('sp', vc[s], v_d[c], reads=[pre + 'v_d'], writes=[vck])
            if not summary_only:
                qck, kck = pre + f'qc{s}', pre + f'kc{s}'
                self.dma('sp', qc[s][:].rearrange("p a b -> p (a b)"), qg_d[c], reads=[pre + 'qk_d0'], writes=[qck])
                self.dma('sp', kc[s][:].rearrange("p a b -> p (a b)"), kg_d[c], reads=[pre + 'qk_d1'], writes=[kck])
                self.dma('sp', szc[s][:].rearrange("p a b -> p (a b)"), self._sz_d[c], reads=[pre + 'sz_d'],
                         writes=[pre + f'szc{s}'])
                self.mms([(ps[0][:, h * 128:(h + 1) * 128],
                           [(kc[s][:, h * 2 + dt, :], qc[s][:, h * 2 + dt, :]) for dt in range(2)]) for h in range(H)],
                         reads=[kck, qck], writes=['ps0'])
                self.tt('dve', Ab[s], ps[0][:], self.tri4, ALU.mult, reads=['ps0', 'cst'], writes=[pre + f'Ab{s}'])
            self.transposes([(self.psb[:, i * 128:(i + 1) * 128], khc[s][:, i, :]) for i in range(NG)], self.identb,
                            reads=[khk, 'identb'], writes=['psb'])
            self.cp('act', kh[s], self.psb[:], reads=['psb'], writes=[kk])
            for h in range(H):
                vh = vc[s][:, h * 512:(h + 1) * 512]
                if not summary_only:
                    nb_ = 3 + h % 2
                    nbk = f'ps{nb_}'
                    self.mm(ps[nb_][:], [(Ab[s][:, h * 128:(h + 1) * 128], vh)] +
                            [(qc[s][:, h * 2 + dt, :], Sb[:, h * 2 + dt, :]) for dt in range(2)],
                            reads=[pre + f'Ab{s}', vck, qck] + [pre + f'Sb{h * 2 + dt}' for dt in range(2)], writes=[nbk])
                for dt in range(2):
                    i = h * 2 + dt
                    db = 5 + dsr % 2
                    dsr += 1
                    dbk = f'ps{db}'
                    self.mm(ps[db][:], [(kh[s][:, i * 128:(i + 1) * 128], vh)], reads=[kk, vck], writes=[dbk])
                    self.stt(S[:, i, :], S[:, i, :], egl[:, i, c:c + 1], ps[db][:], ALU.mult, ALU.add,
                             reads=[dbk, gk, pre + f'S{i}'], writes=[pre + f'S{i}'])
                    if not summary_only:
                        self.cp('act', Sb[:, i, :], S[:, i, :], reads=[pre + f'S{i}'], writes=[pre + f'Sb{i}'])
                if not summary_only:
                    v = sm[:, h, :]
                    smk = pre + 'sm'
                    o = on[h % 2]
                    ok = pre + f'on{h % 2}'
                    self.act(junk, ps[nb_][:], AF.Square, reads=[nbk], writes=[pre + 'junk3', smk], accum=v[:, 0:1])
                    self.ts('dve', v[:, 1:2], v[:, 0:1], 1.0 / DH, EPS, ALU.mult, ALU.add, reads=[smk], writes=[smk])
                    self.act(v[:, 1:2], v[:, 1:2], AF.Sqrt, reads=[smk], writes=[smk])
                    self.P.add('dve', lambda e, v=v: e.reciprocal(out=v[:, 2:3], in_=v[:, 1:2]), [smk], [smk])
                    self.act(o, ps[nb_][:], AF.Copy, reads=[nbk, smk], writes=[ok], scale=v[:, 2:3])

                    def part2(c=c, s=s, h=h, o=o, ok=ok):
                        self.transposes([(ps[7][:, i * 128:(i + 1) * 128], o[:, i * 128:(i + 1) * 128])
                                         for i in range(4)], self.ident, reads=[ok, 'cst'], writes=['ps7'])
                        for i in range(4):
                            ft = h * 4 + i
                            self.stt(yc[s][:, ft, :], ps[7][:, i * 128:(i + 1) * 128], nw[:, ft:ft + 1],
                                     szc[s][:, ft, :], ALU.mult, ALU.mult,
                                     reads=['ps7', pre + 'vec', pre + f'szc{s}'], writes=[pre + f'yc{s}'])
                        if h == H - 1:
                            self.dma('pool', y_d[c], yc[s][:].rearrange("p a b -> p (a b)"), reads=[pre + f'yc{s}'],
                                     writes=[pre + 'y_d'])
                    if pending is not None:
                        pending()
                    pending = part2
        if pending is not None:
            pending()


def extra_sz(kb, pre):
    return kb._sz_d


def extra_uc(kb, pre):
    return kb._uc_d


_PROGS = {}


def get_prog(stages):
    key = tuple(stages)
    if key not in _PROGS:
        nc = bass.Bass("TRN2", target_bir_lowering=False)
        kb = KB(nc, list(stages))
        kb.build()
        _PROGS[key] = nc
    return _PROGS[key]


def _consts(core):
    c = np.zeros((128, CST_W), np.float32)
    c[:, 0:128] = np.eye(128, dtype=np.float32)
    tri = np.triu(np.ones((128, 128), np.float32))
    c[:, 128:640] = np.tile(tri, (1, 4))
    c[:, 640:768] = 1.0
    c[:, 768:776] = (np.arange(8) < core).astype(np.float32)[None, :]
    return c


def _cols(v, ntile):
    return np.ascontiguousarray(v.reshape(ntile, 128).T)


def _f(a):
    return np.ascontiguousarray(a, dtype=np.float32)


def kernel(x, pre_norm_w, post_norm_w,
           m_w_in, m_conv_w, m_conv_b, m_wq, m_wk, m_wv, m_w_gate, m_b_gate,
           m_norm_w, m_skip, m_w_out,
           g_w_in, g_w_gate_up, g_b_gate, g_norm_w, g_w_out, _depth=4, _debug=None):
    x = _f(x)
    xs = x.reshape(SEQ, D)
    cur = [np.ascontiguousarray(xs[c * TL:(c + 1) * TL]) for c in range(NCORES)]
    consts = [_consts(c) for c in range(NCORES)]

    def vec_m(i, j):
        v = np.zeros((128, NVM), np.float32)
        v[:, 0:8] = _cols(pre_norm_w[i], 8)
        for k in range(4):
            v[:, 8 + k * 16:8 + (k + 1) * 16] = _cols(m_conv_w[j, k], 16)
        v[:, 72:88] = _cols(m_conv_b[j], 16)
        v[:, 88:104] = _cols(m_norm_w[j], 16)
        v[:, 104:120] = _cols(m_skip[j], 16)
        return v

    def vec_g(i, j):
        v = np.zeros((128, NVG), np.float32)
        v[:, 0:8] = _cols(pre_norm_w[i], 8)
        v[:, 8:16] = -_cols(g_b_gate[j], 8)
        v[:, 16:32] = _cols(g_norm_w[j], 16)
        return v

    def a_inputs(i, pre, c, full_x):
        j = i // 2
        d = {}
        if i % 2 == 0:
            xh = np.zeros((128, D), np.float32)
            if c > 0:
                xh[0:3] = full_x[c * TL - 3:c * TL]
            d.update({pre + "w_in": _f(m_w_in[j]), pre + "wq": _f(m_wq[j]), pre + "wk": _f(m_wk[j]),
                      pre + "wv": _f(m_wv[j]), pre + "wvT": _f(np.transpose(m_wv[j], (0, 2, 1))),
                      pre + "wg": _f(m_w_gate[j].reshape(48, 128, 8).transpose(1, 0, 2).reshape(128, 384)),
                      pre + "bg": _f(m_b_gate[j].reshape(1, 8)), pre + "vec": vec_m(i, j), pre + "xh": xh})
        else:
            d.update({pre + "w_in": _f(g_w_in[j]), pre + "wgu": _f(g_w_gate_up[j]), pre + "vec": vec_g(i, j)})
        return d

    def b_inputs(i, pre, c, aout, sumS_all, sumn_all):
        j = i // 2
        d = {pre + "sumS_all": sumS_all, pre + "sumn_all": sumn_all,
             pre + "post_w": _f(post_norm_w[i].reshape(1, D))}
        if i % 2 == 0:
            d.update({pre + "w_out": _f(m_w_out[j]), pre + "vec": vec_m(i, j)})
            names = ["qT", "kT", "v", "sz", "uc", "gsc"]
        else:
            d.update({pre + "w_out": _f(g_w_out[j]), pre + "vec": vec_g(i, j)})
            names = ["qg", "kg", "kh", "v", "sz", "egl"]
        for n in names:
            d[pre + n] = aout[c][n]
        return d

    def grab(res, pre, i):
        names = (["qT", "kT", "v", "sz", "uc", "gsc"] if i % 2 == 0 else ["qg", "kg", "kh", "v", "sz", "egl"]) + \
            ["sumS", "sumn"]
        return [{n: res.results[c][pre + n] for n in names} for c in range(NCORES)]

    def launch(stages, maps):
        nc = get_prog(stages)
        return run_bass_kernel_spmd(nc, maps, core_ids=list(range(NCORES)))

    def gathered(aout):
        sumS_all = np.ascontiguousarray(np.stack([aout[c]["sumS"] for c in range(NCORES)], axis=0))
        sumn_all = np.ascontiguousarray(np.concatenate([aout[c]["sumn"] for c in range(NCORES)], axis=1))
        return sumS_all, sumn_all

    dbg = {}
    plan = [[('A', 0)]]
    for i in range(_depth):
        if i + 1 < _depth and (i + 1) % 2 == 1:
            plan.append([('B', i), ('A', i + 1)])
        else:
            plan.append([('B', i)])
            if i + 1 < _depth:
                plan.append([('A', i + 1)])
    aout = None
    pend = None
    for stages in plan:
        full_x = np.concatenate(cur, axis=0)
        prog = tuple((k, 'm' if i % 2 == 0 else 'g') for k, i in stages)
        if any(k == 'B' for k, _ in stages):
            sumS_all, sumn_all = gathered(aout)
        maps = []
        for c in range(NCORES):
            d = {"cst": consts[c], "x": cur[c]}
            for si, (k, i) in enumerate(stages):
                pre = f"s{si}_"
                if k == 'A':
                    d.update(a_inputs(i, pre, c, full_x))
                else:
                    d.update(b_inputs(i, pre, c, aout, sumS_all, sumn_all))
            maps.append(d)
        res = launch(prog, maps)
        for si, (k, i) in enumerate(stages):
            pre = f"s{si}_"
            if k == 'A':
                aout = grab(res, pre, i)
                if _debug is not None:
                    dbg[f"A{i}"] = aout
            else:
                cur = [res.results[c][pre + "xo"] for c in range(NCORES)]
    out = np.concatenate(cur, axis=0).reshape(1, SEQ, D).astype(np.float32)
    if _debug is not None:
        _debug.update(dbg)
    return out
```

```python
import math
from contextlib import ExitStack

import ml_dtypes
import numpy as np

import concourse.bass as bass
import concourse.mybir as mybir
from concourse.bass_utils import run_bass_kernel_spmd

F32 = mybir.dt.float32
BF16 = mybir.dt.bfloat16
AF = mybir.ActivationFunctionType
ALU = mybir.AluOpType
AX = mybir.AxisListType

NCORES = 8
SEQ = 16384
TL = SEQ // NCORES
NT = TL // 128
D = 1024
KD = D // 128
E = 2048
NFT = E // 128
H = 4
DH = 512
GK = 256
NG = 8
EPS = 1e-6
TAU = 16.0

GSC_M = 5 * 64
NVM = 8 + 64 + 16 + 16 + 16
NVG = 8 + 8 + 16
CST_W = 128 + 512 + 128 + 8


class Prog:
    ENGS = ['pe', 'act', 'dve', 'pool', 'sp']
    NDS = 8

    def __init__(self, nc):
        self.nc = nc
        self.q = {e: [] for e in self.ENGS}
        self.last_w = {}
        self.readers = {}
        self.ndma = {e: 0 for e in self.ENGS}
        self.dma_ops = {e: [] for e in self.ENGS}
        self.pending_bar = {}

    def add(self, eng, fn, reads=(), writes=(), dma=False):
        op = dict(eng=eng, fn=fn, idx=len(self.q[eng]), dma=dma, sig=False, depo={})
        depo = op['depo']
        for b in reads:
            w = self.last_w.get(b)
            if w is not None:
                depo[id(w)] = w
        for b in writes:
            w = self.last_w.get(b)
            if w is not None:
                depo[id(w)] = w
            for r in self.readers.get(b, ()):
                depo[id(r)] = r
        bar = self.pending_bar.pop(eng, None)
        if bar:
            for d in bar:
                depo[id(d)] = d
        if dma:
            k = self.ndma[eng]
            self.ndma[eng] += 1
            op['dk'] = k
            if k >= self.NDS:
                prev = self.dma_ops[eng][k - self.NDS]
                depo[id(prev)] = prev
            self.dma_ops[eng].append(op)
        for b in reads:
            self.readers.setdefault(b, []).append(op)
        for b in writes:
            self.last_w[b] = op
            self.readers[b] = []
        self.q[eng].append(op)
        return op

    def barrier(self):
        ops = []
        for e in self.ENGS:
            comp = [o for o in self.q[e] if not o['dma']]
            if comp:
                ops.append(comp[-1])
            ops += self.dma_ops[e][-self.NDS:]
        for e in self.ENGS:
            self.pending_bar[e] = list(ops)

    def emit(self, final_wait_ops=()):
        nc = self.nc
        for e in self.ENGS:
            for op in self.q[e]:
                need = []
                best = {}
                for d in op['depo'].values():
                    if d is op:
                        continue
                    if d['dma']:
                        need.append(d)
                    else:
                        if d['eng'] == e and (e == 'pe' or d['idx'] > op['idx']):
                            continue
                        if d['eng'] not in best or best[d['eng']]['idx'] < d['idx']:
                            best[d['eng']] = d
                need += list(best.values())
                for d in need:
                    d['sig'] = True
                op['need'] = need
                op['depo'] = None
        for e in self.ENGS:
            c = 0
            for op in self.q[e]:
                if op['dma']:
                    op['semslot'] = op['dk'] % self.NDS
                    op['semval'] = 16 * (op['dk'] // self.NDS + 1)
                elif op['sig']:
                    c += 1
                    op['semval'] = c
        with ExitStack() as st:
            csem = {e: st.enter_context(nc.semaphore(f"c_{e}")) for e in self.ENGS}
            dsem = {e: ([st.enter_context(nc.semaphore(f"d_{e}{i}")) for i in range(self.NDS)]
                        if self.ndma[e] else []) for e in self.ENGS}
            block = st.enter_context(nc.Block())
            handles = {'pe': block.tensor, 'act': block.scalar, 'dve': block.vector,
                       'pool': block.gpsimd, 'sp': block.sync}

            def mk(e):
                def body(eng):
                    waited = {}
                    for op in self.q[e]:
                        for d in op['need']:
                            if d['dma']:
                                key = ('d', d['eng'], d['semslot'])
                                sem = dsem[d['eng']][d['semslot']]
                            else:
                                key = ('c', d['eng'])
                                sem = csem[d['eng']]
                            if waited.get(key, 0) >= d['semval']:
                                continue
                            eng.wait_ge(sem, d['semval'])
                            waited[key] = d['semval']
                        ins = op['fn'](eng)
                        if op['dma']:
                            ins.then_inc(dsem[e][op['semslot']], 16)
                        elif op['sig']:
                            ins.then_inc(csem[e], 1)
                    if e == 'sp':
                        for d in final_wait_ops:
                            if d['dma']:
                                eng.wait_ge(dsem[d['eng']][d['semslot']], d['semval'])
                            else:
                                eng.wait_ge(csem[d['eng']], d['semval'])
                return body

            for e in self.ENGS:
                if self.q[e] or e == 'sp':
                    handles[e](mk(e))


class Arena:
    def __init__(self, t, nwords):
        self.t = t
        self.cap = nwords
        self.off = 0

    def alloc(self, free_shape, dtype):
        n = 1
        for s in free_shape:
            n *= s
        nbytes = n * (4 if dtype == F32 else 2)
        words = (nbytes + 3) // 4
        words = (words + 15) // 16 * 16
        off = self.off
        self.off += words
        assert self.off <= self.cap, f"arena overflow {self.off * 4} > {self.cap * 4}"
        v = self.t[:, off:off + (nbytes + 3) // 4]
        if dtype != F32:
            v = v.bitcast(dtype)
        if len(free_shape) == 2:
            v = v.rearrange("p (a b) -> p a b", a=free_shape[0], b=free_shape[1])
        elif len(free_shape) == 3:
            v = v.rearrange("p (a b c) -> p a b c", a=free_shape[0], b=free_shape[1], c=free_shape[2])
        return v

    def mark(self):
        return self.off

    def release(self, m):
        self.off = m


class KB:
    def __init__(self, nc, stages):
        self.nc = nc
        self.P = Prog(nc)
        self.stages = stages
        self.outs = []
        self.rr = 0

    def dma(self, eng, out, in_, reads=(), writes=()):
        return self.P.add(eng, lambda e: e.dma_start(out=out, in_=in_), reads, writes, dma=True)

    def mm(self, out, pairs, reads=(), writes=()):
        n = len(pairs)

        def fn(e):
            ins = None
            for i, (l, r) in enumerate(pairs):
                ins = e.matmul(out, lhsT=l, rhs=r, start=(i == 0), stop=(i == n - 1))
            return ins
        return self.P.add('pe', fn, reads, writes)

    def mms(self, groups, reads=(), writes=()):
        def fn(e):
            ins = None
            for out, pairs in groups:
                n = len(pairs)
                for i, (l, r) in enumerate(pairs):
                    ins = e.matmul(out, lhsT=l, rhs=r, start=(i == 0), stop=(i == n - 1))
            return ins
        return self.P.add('pe', fn, reads, writes)

    def transposes(self, items, ident, reads=(), writes=()):
        def fn(e):
            ins = None
            for out, in_ in items:
                ins = e.transpose(out, in_, ident)
            return ins
        return self.P.add('pe', fn, reads, writes)

    def act(self, out, in_, func, reads=(), writes=(), bias=None, scale=None, accum=None):
        kw = {}
        if bias is not None:
            kw['bias'] = bias
        if scale is not None:
            kw['scale'] = scale
        if accum is not None:
            kw['accum_out'] = accum
        return self.P.add('act', lambda e: e.activation(out=out, in_=in_, func=func, **kw), reads, writes)

    def ts(self, eng, out, in0, s1, s2, op0, op1=None, reads=(), writes=()):
        if op1 is None:
            return self.P.add(eng, lambda e: e.tensor_scalar(out=out, in0=in0, scalar1=s1, scalar2=None, op0=op0),
                              reads, writes)
        return self.P.add(eng, lambda e: e.tensor_scalar(out=out, in0=in0, scalar1=s1, scalar2=s2, op0=op0, op1=op1),
                          reads, writes)

    def tt(self, eng, out, in0, in1, op, reads=(), writes=()):
        return self.P.add(eng, lambda e: e.tensor_tensor(out=out, in0=in0, in1=in1, op=op), reads, writes)

    def stt(self, out, in0, scalar, in1, op0, op1, reads=(), writes=()):
        return self.P.add('dve', lambda e: e.scalar_tensor_tensor(out=out, in0=in0, scalar=scalar, in1=in1,
                                                                  op0=op0, op1=op1), reads, writes)

    def cp(self, eng, out, in_, reads=(), writes=()):
        if eng == 'act':
            return self.P.add('act', lambda e: e.copy(out=out, in_=in_), reads, writes)
        return self.P.add(eng, lambda e: e.tensor_copy(out=out, in_=in_), reads, writes)

    def memset(self, eng, ap, val, writes=()):
        return self.P.add(eng, lambda e: e.memset(ap, val), (), writes)

    def evac(self, out, in_, reads=(), writes=()):
        self.rr += 1
        return self.cp('act' if self.rr % 2 else 'dve', out, in_, reads, writes)

    def wstream(self, bufs, keys, srcs):
        kb = self

        class WS:
            nxt = 0

            def start(ws, i):
                while ws.nxt <= i + len(bufs) - 1 and ws.nxt < len(srcs):
                    j = ws.nxt
                    kb.dma('pool', bufs[j % len(bufs)], srcs[j].rearrange("(k p) c -> p k c", p=128),
                           writes=[keys[j % len(bufs)]])
                    ws.nxt += 1

            def get(ws, i):
                return bufs[i % len(bufs)], keys[i % len(bufs)]
        return WS()

    def dram(self, name, shape, dtype, kind):
        t = self.nc.dram_tensor(name, list(shape), dtype, kind=kind)
        return t.ap()

    def build(self):
        nc = self.nc
        with ExitStack() as st:
            arena_t = st.enter_context(nc.sbuf_tensor("arena", [128, 53000], F32))
            self.A = Arena(arena_t, 53000)
            self.ps = [st.enter_context(nc.psum_tensor(f"ps{i}", [128, 512], F32)) for i in range(8) if i != 1]
            self.ps.insert(1, None)
            self.psb = st.enter_context(nc.psum_tensor("psb", [128, 1024], BF16))
            A = self.A
            self.cst = A.alloc([CST_W], F32)
            self.ident = self.cst[:, 0:128]
            self.tri4 = self.cst[:, 128:640]
            self.tri = self.cst[:, 128:256]
            self.ones = self.cst[:, 640:768]
            self.sel = self.cst[:, 768:776]
            self.identb = A.alloc([128], BF16)
            self.onesb = A.alloc([128], BF16)
            self.x = A.alloc([NT, D], F32)
            cst_d = self.dram("cst", [128, CST_W], F32, "ExternalInput")
            x_d = self.dram("x", [TL, D], F32, "ExternalInput")
            self.dma('sp', self.cst, cst_d, writes=['cst'])
            self.cp('dve', self.identb, self.ident, reads=['cst'], writes=['identb'])
            self.cp('dve', self.onesb, self.ones, reads=['cst'], writes=['onesb'])
            for t in range(NT):
                self.dma('sp', self.x[:, t, :], x_d[t * 128:(t + 1) * 128, :], writes=[f'x{t}'])
            final = []
            for si, (kind, mix) in enumerate(self.stages):
                pre = f"s{si}_"
                self.stage_tag = [pre]
                m = A.mark()
                if kind == 'A' and mix == 'm':
                    self.stage_A_m(pre)
                elif kind == 'B' and mix == 'm':
                    final += self.stage_B(pre, 'm')
                elif kind == 'A' and mix == 'g':
                    self.stage_A_g(pre)
                else:
                    final += self.stage_B(pre, 'g')
                A.release(m)
                self.P.barrier()
            final += self.outs
            self.P.emit(final_wait_ops=final)

    def prenorm(self, hT, pnw, xtiles, ntiles, tagp, veck):
        A = self.A
        ss = A.alloc([ntiles], F32)
        rstd = A.alloc([ntiles], F32)
        if not hasattr(self, '_pn_tmp') or self._pn_tmp[0] != id(self.stage_tag):
            self._pn_tmp = (id(self.stage_tag), A.alloc([D], BF16), [A.alloc([D], F32) for _ in range(2)])
        junk = self._pn_tmp[1]
        xs = self._pn_tmp[2]
        pk = self.stage_tag[0]
        for t, (xa, key) in enumerate(xtiles):
            self.act(junk, xa, AF.Square, reads=[key], writes=[pk + 'junk', tagp + 'ss'], accum=ss[:, t:t + 1])
        self.ts('dve', rstd, ss, 1.0 / D, EPS, ALU.mult, ALU.add, reads=[tagp + 'ss'], writes=[tagp + 'rstd'])
        self.act(rstd, rstd, AF.Sqrt, reads=[tagp + 'rstd'], writes=[tagp + 'rstd'])
        self.P.add('dve', lambda e: e.reciprocal(out=rstd, in_=rstd), [tagp + 'rstd'], [tagp + 'rstd'])
        for t, (xa, key) in enumerate(xtiles):
            xst = xs[t % 2]
            self.ts('dve', xst, xa, rstd[:, t:t + 1], None, ALU.mult, reads=[key, tagp + 'rstd'],
                    writes=[pk + f'xs{t % 2}'])
            for half in range(2):
                bank = self.ps[0] if half == 0 else self.ps[7]
                bk = 'ps0' if half == 0 else 'ps7'
                self.transposes([(bank[:, i * 128:(i + 1) * 128], xst[:, (half * 4 + i) * 128:(half * 4 + i + 1) * 128])
                                 for i in range(4)], self.ident, reads=[pk + f'xs{t % 2}', 'cst'], writes=[bk])
                for i in range(4):
                    k = half * 4 + i
                    o = hT[:, k, t * 128:(t + 1) * 128]
                    if i % 2 == 0:
                        self.act(o, bank[:, i * 128:(i + 1) * 128], AF.Copy, reads=[bk, veck],
                                 writes=[tagp + f'hT{t}'], scale=pnw[:, k:k + 1])
                    else:
                        self.ts('dve', o, bank[:, i * 128:(i + 1) * 128], pnw[:, k:k + 1], None, ALU.mult,
                                reads=[bk, veck], writes=[tagp + f'hT{t}'])

    def stage_A_m(self, pre):
        A = self.A
        P = self.P
        ps = self.ps
        IN, OUT = "ExternalInput", "ExternalOutput"
        w_in = self.dram(pre + "w_in", [D, 2 * E], F32, IN)
        wq_d = self.dram(pre + "wq", [H, DH, DH], F32, IN)
        wk_d = self.dram(pre + "wk", [H, DH, DH], F32, IN)
        wv_d = self.dram(pre + "wv", [H, DH, DH], F32, IN)
        wvT_d = self.dram(pre + "wvT", [H, DH, DH], F32, IN)
        wg_d = self.dram(pre + "wg", [128, 48 * 8], F32, IN)
        bg_d = self.dram(pre + "bg", [1, 8], F32, IN)
        vec_d = self.dram(pre + "vec", [128, NVM], F32, IN)
        xh_d = self.dram(pre + "xh", [128, D], F32, IN)
        qT_d = self.dram(pre + "qT", [NT, 128, NFT * 128], BF16, OUT)
        kT_d = self.dram(pre + "kT", [NT, 128, NFT * 128], BF16, OUT)
        v_d = self.dram(pre + "v", [NT, 128, E], BF16, OUT)
        sz_d = self.dram(pre + "sz", [NT, 128, NFT * 128], BF16, OUT)
        uc_d = self.dram(pre + "uc", [NT, 128, NFT * 128], BF16, OUT)
        gsc_d = self.dram(pre + "gsc", [128, GSC_M], F32, OUT)
        sumS_d = self.dram(pre + "sumS", [NFT * 128, DH], F32, OUT)
        sumn_d = self.dram(pre + "sumn", [128, 20], F32, OUT)

        vec = A.alloc([NVM], F32)
        pnw = vec[:, 0:8]
        cw = vec[:, 8:72]
        cb = vec[:, 72:88]
        self.dma('sp', vec, vec_d, writes=[pre + 'vec'])
        wg = A.alloc([48 * 8], BF16)
        self.dma('pool', wg, wg_d, writes=[pre + 'wg'])
        bgb = A.alloc([NT, 8], F32)
        self.dma('sp', bgb, bg_d.partition_broadcast(128).broadcast_to([128, NT, 8]) if False else
                 bass.AP(bg_d.tensor, 0, [[0, 128], [0, NT], [1, 8]]), writes=[pre + 'bgb'])
        gacc = A.alloc([NT, 8], F32)
        self.memset('dve', gacc, 0.0, writes=[pre + 'gacc'])
        wvg = A.alloc([128], BF16)
        gsc = A.alloc([GSC_M], F32)
        kap = gsc[:, 0:64]
        kap2 = gsc[:, 64:128]
        invlam = gsc[:, 128:192]
        rho = gsc[:, 192:256]
        rhon = gsc[:, 256:320]
        G = A.alloc([NT, 8], F32)
        sp = A.alloc([NT, 4], F32)
        cum = A.alloc([64], F32)
        tot = A.alloc([64], F32)
        totn = A.alloc([64], F32)
        e1 = A.alloc([64], F32)
        e2 = A.alloc([64], F32)
        sumn = A.alloc([20], F32)
        mP1 = A.mark()
        hT = A.alloc([KD, TL], BF16)
        hTh = A.alloc([KD, 128], BF16)
        xh = A.alloc([D], F32)
        stg = [A.alloc([TL], BF16) for _ in range(2)]

        wvT = stg[0][:].rearrange("p (a b) -> p a b", a=4)
        for h in range(H):
            self.dma('pool', wvT, wvT_d[h].rearrange("(t p) d -> p t d", p=128), writes=[pre + 'stg0'])
            groups = []
            for dt in range(4):
                groups.append((ps[2][:, (h * 4 + dt) * 8:(h * 4 + dt + 1) * 8],
                               [(wvT[:, et, dt * 128:(dt + 1) * 128], wg[:, (32 + h * 4 + et) * 8:(32 + h * 4 + et + 1) * 8])
                                for et in range(4)]))
            self.mms(groups, reads=[pre + 'stg0', pre + 'wg'], writes=['ps2'])
        self.cp('dve', wvg, ps[2][:, 0:128], reads=['ps2'], writes=[pre + 'wvg'])

        self.dma('sp', xh, xh_d, writes=[pre + 'xh'])
        self.prenorm(hT, pnw, [(self.x[:, t, :], f'x{t}') for t in range(NT)], NT, pre, pre + 'vec')
        self.prenorm(hTh, pnw, [(xh, pre + 'xh')], 1, pre + 'h', pre + 'vec')
        hkeys = [pre + f'hT{t}' for t in range(NT)]

        wblk = [A.alloc([KD, 512], BF16) for _ in range(2)]
        wq = A.alloc([4, DH], BF16)
        wk = A.alloc([4, DH], BF16)
        wv = A.alloc([4, DH], BF16)
        ucT = A.alloc([4, TL], BF16)
        uT = A.alloc([4, TL], BF16)
        uf = A.alloc([TL + 3], F32)
        acc = [A.alloc([512], F32) for _ in range(2)]
        vst = [A.alloc([512], BF16) for _ in range(2)]
        nblk = 0
        nstg = 0
        nv = 0
        bank_rr = 0

        def next_bank():
            nonlocal bank_rr
            b = 3 + bank_rr % 4
            bank_rr += 1
            return b

        ws = self.wstream(wblk, [pre + 'wblk0', pre + 'wblk1'],
                          [w_in[:, i * 512:(i + 1) * 512] for i in range(8)])
        self.dma('pool', wq, wq_d[0].rearrange("(k p) e -> p k e", p=128), writes=[pre + 'wq'])
        self.dma('pool', wk, wk_d[0].rearrange("(k p) e -> p k e", p=128), writes=[pre + 'wk'])
        self.dma('pool', wv, wv_d[0].rearrange("(k p) e -> p k e", p=128), writes=[pre + 'wv'])
        for h in range(H):
            ws.start(h)
            wb, wbk = ws.get(h)
            for ft in range(4):
                ftg = h * 4 + ft
                self.mm(ps[2][:, 128 + ftg * 4:128 + ftg * 4 + 3],
                        [(wb[:, k, ft * 128:(ft + 1) * 128], hTh[:, k, 0:3]) for k in range(KD)],
                        reads=[wbk, pre + 'hhT0'], writes=['ps2'])
                self.cp('act', uf[:, 0:3], ps[2][:, 128 + ftg * 4:128 + ftg * 4 + 3], reads=['ps2'],
                        writes=[pre + 'uf_h'])
                for st4 in range(4):
                    b = next_bank()
                    bk = f'ps{b}'
                    self.mm(ps[b][:], [(wb[:, k, ft * 128:(ft + 1) * 128], hT[:, k, st4 * 512:(st4 + 1) * 512])
                                       for k in range(KD)],
                            reads=[wbk] + hkeys[st4 * 4:(st4 + 1) * 4], writes=[bk])
                    self.cp('act', uf[:, 3 + st4 * 512:3 + (st4 + 1) * 512], ps[b][:], reads=[bk],
                            writes=[pre + f'uf{st4}'])
                    self.cp('dve', uT[:, ft, st4 * 512:(st4 + 1) * 512], uf[:, 3 + st4 * 512:3 + (st4 + 1) * 512],
                            reads=[pre + f'uf{st4}'], writes=[pre + f'uT{st4}'])
                    a = acc[st4 % 2]
                    ak = pre + f'acc{st4 % 2}'
                    rk = [pre + f'uf{st4}', pre + (f'uf{st4 - 1}' if st4 else 'uf_h'), pre + 'vec']
                    o0 = st4 * 512
                    self.ts('dve', a, uf[:, o0:o0 + 512], cw[:, ftg:ftg + 1], None, ALU.mult, reads=rk, writes=[ak])
                    for kk in range(1, 4):
                        self.stt(a, uf[:, o0 + kk:o0 + kk + 512], cw[:, kk * 16 + ftg:kk * 16 + ftg + 1], a,
                                 ALU.mult, ALU.add, reads=rk + [ak], writes=[ak])
                    self.act(ucT[:, ft, st4 * 512:(st4 + 1) * 512], a, AF.Silu, reads=[ak, pre + 'vec'],
                             writes=[pre + f'ucT{st4}'], bias=cb[:, ftg:ftg + 1])
                self.dma('pool', uc_d[:, :, ftg * 128:(ftg + 1) * 128].rearrange("c p t -> p c t"),
                         ucT[:, ft, :].rearrange("p (c t) -> p c t", t=128),
                         reads=[pre + f'ucT{s}' for s in range(4)], writes=[pre + 'uc_d'])
            uck = [pre + f'ucT{s}' for s in range(4)]
            utk = [pre + f'uT{s}' for s in range(4)]
            for which, wsb, wkey, dst, goff in ((0, wq, pre + 'wq', qT_d, 0), (1, wk, pre + 'wk', kT_d, 16)):
                for et in range(4):
                    sg = stg[nstg % 2]
                    sk = pre + f'stg{nstg % 2}'
                    nstg += 1
                    for st4 in range(4):
                        b = next_bank()
                        bk = f'ps{b}'
                        self.mm(ps[b][:], [(wsb[:, k, et * 128:(et + 1) * 128], ucT[:, k, st4 * 512:(st4 + 1) * 512])
                                           for k in range(4)], reads=[wkey, uck[st4]], writes=[bk])
                        self.evac(sg[:, st4 * 512:(st4 + 1) * 512], ps[b][:], reads=[bk], writes=[sk])
                    tile_i = h * 4 + et
                    self.dma('pool', dst[:, :, tile_i * 128:(tile_i + 1) * 128].rearrange("c p t -> p c t"),
                             sg[:].rearrange("p (c t) -> p c t", t=128), reads=[sk], writes=[pre + f'qk_d{which}'])
                    gt = goff + tile_i
                    self.mms([(ps[2][:, tt * 8:(tt + 1) * 8], [(sg[:, tt * 128:(tt + 1) * 128], wg[:, gt * 8:(gt + 1) * 8])])
                              for tt in range(NT)], reads=[sk, pre + 'wg'], writes=['ps2'])
                    self.tt('dve', gacc[:].rearrange("p a b -> p (a b)"), gacc[:].rearrange("p a b -> p (a b)"),
                            ps[2][:, 0:128], ALU.add, reads=['ps2', pre + 'gacc'], writes=[pre + 'gacc'])
            if h + 1 < H:
                self.dma('pool', wq, wq_d[h + 1].rearrange("(k p) e -> p k e", p=128), writes=[pre + 'wq'])
                self.dma('pool', wk, wk_d[h + 1].rearrange("(k p) e -> p k e", p=128), writes=[pre + 'wk'])
            for tt in range(NT):
                b = next_bank()
                bk = f'ps{b}'
                self.mm(ps[b][:], [(uT[:, k, tt * 128:(tt + 1) * 128], wv[:, k, :]) for k in range(4)],
                        reads=[pre + 'wv', utk[tt // 4]], writes=[bk])
                vs = vst[nv % 2]
                vk = pre + f'vst{nv % 2}'
                nv += 1
                self.evac(vs, ps[b][:], reads=[bk], writes=[vk])
                self.dma('pool', v_d[tt, :, h * 512:(h + 1) * 512], vs, reads=[vk], writes=[pre + 'v_d'])
            if h + 1 < H:
                self.dma('pool', wv, wv_d[h + 1].rearrange("(k p) e -> p k e", p=128), writes=[pre + 'wv'])
            self.mms([(ps[2][:, tt * 8:(tt + 1) * 8],
                       [(uT[:, k, tt * 128:(tt + 1) * 128], wvg[:, (h * 4 + k) * 8:(h * 4 + k + 1) * 8]) for k in range(4)])
                      for tt in range(NT)], reads=utk + [pre + 'wvg'], writes=['ps2'])
            self.tt('dve', gacc[:].rearrange("p a b -> p (a b)"), gacc[:].rearrange("p a b -> p (a b)"),
                    ps[2][:, 0:128], ALU.add, reads=['ps2', pre + 'gacc'], writes=[pre + 'gacc'])

        for zb in range(4):
            ws.start(4 + zb)
            wb, wbk = ws.get(4 + zb)
            for ft in range(4):
                sg = stg[nstg % 2]
                sk = pre + f'stg{nstg % 2}'
                nstg += 1
                for st4 in range(4):
                    b = next_bank()
                    bk = f'ps{b}'
                    self.mm(ps[b][:], [(wb[:, k, ft * 128:(ft + 1) * 128], hT[:, k, st4 * 512:(st4 + 1) * 512])
                                       for k in range(KD)], reads=[wbk] + hkeys[st4 * 4:(st4 + 1) * 4], writes=[bk])
                    self.act(sg[:, st4 * 512:(st4 + 1) * 512], ps[b][:], AF.Silu, reads=[bk], writes=[sk])
                tile_i = zb * 4 + ft
                self.dma('pool', sz_d[:, :, tile_i * 128:(tile_i + 1) * 128].rearrange("c p t -> p c t"),
                         sg[:].rearrange("p (c t) -> p c t", t=128), reads=[sk], writes=[pre + 'sz_d'])

        gk = pre + 'gs'
        self.tt('dve', G, gacc, bgb, ALU.add, reads=[pre + 'gacc', pre + 'bgb'], writes=[gk])
        self.act(sp, G[:, :, 4:8], AF.Exp, reads=[gk], writes=[gk], scale=-1.0)
        self.act(sp, sp, AF.Ln, reads=[gk], writes=[gk], bias=1.0)
        spf = sp[:].rearrange("p a b -> p (a b)")
        self.mms([(ps[2][:, c * 4:(c + 1) * 4], [(self.tri, spf[:, c * 4:(c + 1) * 4])]) for c in range(NT)] +
                 [(ps[2][:, 64 + c * 4:64 + (c + 1) * 4], [(self.ones, spf[:, c * 4:(c + 1) * 4])]) for c in range(NT)],
                 reads=[gk, 'cst'], writes=['ps2'])
        self.cp('dve', cum, ps[2][:, 0:64], reads=['ps2'], writes=[gk])
        self.cp('dve', tot, ps[2][:, 64:128], reads=['ps2'], writes=[gk])
        self.memset('dve', totn, 0.0, writes=[gk])
        self.cp('dve', totn[:, 0:60], tot[:, 4:64], reads=[gk], writes=[gk])
        li = G[:, :, 0:4]
        e1v = e1[:].rearrange("p (a b) -> p a b", b=4)
        self.tt('dve', e1v, li, cum[:].rearrange("p (a b) -> p a b", b=4), ALU.add, reads=[gk], writes=[gk])
        self.tt('dve', e1, e1, tot, ALU.subtract, reads=[gk], writes=[gk])
        lnsc = math.log(DH ** -0.5)
        self.act(kap, e1, AF.Exp, reads=[gk], writes=[gk], bias=lnsc)
        self.tt('dve', e2, e1, totn, ALU.subtract, reads=[gk], writes=[gk])
        self.act(kap2, e2, AF.Exp, reads=[gk], writes=[gk], bias=lnsc)
        self.tt('dve', e2, cum, tot, ALU.subtract, reads=[gk], writes=[gk])
        self.act(invlam, e2, AF.Exp, reads=[gk], writes=[gk])
        self.act(rho, tot, AF.Exp, reads=[gk], writes=[gk], scale=-1.0)
        self.act(rhon, totn, AF.Exp, reads=[gk], writes=[gk], scale=-1.0)
        self.P.add('dve', lambda e: e.tensor_reduce(out=sumn[:, 16:20], in_=tot[:].rearrange("p (c h) -> p h c", h=4),
                                                    axis=AX.X, op=ALU.add), [gk], [pre + 'sumn'])
        self.outs.append(self.dma('sp', gsc_d, gsc, reads=[gk], writes=[pre + 'gsc_d']))

        A.release(mP1)
        self.P.barrier()
        S = A.alloc([NFT, DH], F32)
        nst = sumn[:, 0:16]
        self.memset('dve', S, 0.0, writes=[pre + f'S{i}' for i in range(NFT)])
        self.memset('dve', nst, 0.0, writes=[pre + 'sumn'])
        self.scan_m(pre, kT_d, v_d, None, gsc, S, None, nst, None, summary_only=True)
        for i in range(NFT):
            self.outs.append(self.dma('sp', sumS_d[i * 128:(i + 1) * 128, :], S[:, i, :], reads=[pre + f'S{i}'],
                                      writes=[pre + 'sumS_d']))
        self.outs.append(self.dma('sp', sumn_d, sumn, reads=[pre + 'sumn'], writes=[pre + 'sumn_d']))

    def scan_m(self, pre, kT_d, v_d, qT_d, gsc, S, Sb, nst, nb, summary_only, extra=None):
        A = self.A
        ps = self.ps
        kap = gsc[:, 0:64]
        kap2 = gsc[:, 64:128]
        invlam = gsc[:, 128:192]
        rhon = gsc[:, 256:320]
        gk = pre + 'gs'
        kc = [A.alloc([NFT, 128], BF16) for _ in range(2)]
        vc = [A.alloc([E], BF16) for _ in range(2)]
        kt = [A.alloc([E], BF16) for _ in range(2)]
        if not summary_only:
            qc = [A.alloc([NFT, 128], BF16) for _ in range(2)]
            sTb = [A.alloc([512], BF16) for _ in range(2)]
            hn = [A.alloc([DH], F32) for _ in range(2)]
            sm = A.alloc([H, 16], F32)
            (szc, ucc, yc, y_d, nw, skp) = extra
        dsr = 0
        pending = None
        for c in range(NT):
            s = c % 2
            kck, vck, ktk = pre + f'kc{s}', pre + f'vc{s}', pre + f'kt{s}'
            self.dma('sp', kc[s][:].rearrange("p a b -> p (a b)"), kT_d[c], reads=[pre + 'qk_d1'], writes=[kck])
            self.dma('sp', vc[s], v_d[c], reads=[pre + 'v_d'], writes=[vck])
            if not summary_only:
                qck = pre + f'qc{s}'
                self.dma('sp', qc[s][:].rearrange("p a b -> p (a b)"), qT_d[c], reads=[pre + 'qk_d0'], writes=[qck])
                self.dma('sp', szc[s][:].rearrange("p a b -> p (a b)"), extra_sz(self, pre)[c], reads=[pre + 'sz_d'],
                         writes=[pre + f'szc{s}'])
                self.dma('sp', ucc[s][:].rearrange("p a b -> p (a b)"), extra_uc(self, pre)[c], reads=[pre + 'uc_d'],
                         writes=[pre + f'ucc{s}'])
                self.mms([(ps[0][:, h * 128:(h + 1) * 128],
                           [(kc[s][:, h * 4 + dt, :], qc[s][:, h * 4 + dt, :]) for dt in range(4)]) for h in range(H)],
                         reads=[kck, qck], writes=['ps0'])
                for h in range(H):
                    self.stt(sTb[s][:, h * 128:(h + 1) * 128], ps[0][:, h * 128:(h + 1) * 128],
                             kap[:, c * 4 + h:c * 4 + h + 1], self.tri, ALU.mult, ALU.mult,
                             reads=['ps0', gk, 'cst'], writes=[pre + f'sTb{s}'])
            for r in range(2):
                self.transposes([(self.psb[:, i * 128:(i + 1) * 128], kc[s][:, r * 8 + i, :]) for i in range(8)],
                                self.identb, reads=[kck, 'identb'], writes=['psb'])
                for hh in range(2):
                    h = r * 2 + hh
                    self.act(kt[s][:, h * 512:(h + 1) * 512], self.psb[:, hh * 512:(hh + 1) * 512], AF.Copy,
                             reads=['psb', gk], writes=[ktk], scale=kap2[:, c * 4 + h:c * 4 + h + 1])
            for h in range(H):
                vh = vc[s][:, h * 512:(h + 1) * 512]
                if not summary_only:
                    nb_ = 3 + h % 2
                    nbk = f'ps{nb_}'
                    self.mm(ps[nb_][:], [(sTb[s][:, h * 128:(h + 1) * 128], vh)] +
                            [(qc[s][:, h * 4 + dt, :], Sb[:, h * 4 + dt, :]) for dt in range(4)],
                            reads=[pre + f'sTb{s}', vck, qck] + [pre + f'Sb{h * 4 + dt}' for dt in range(4)],
                            writes=[nbk])
                    self.mm(ps[2][:, 64 + h:64 + h + 1], [(sTb[s][:, h * 128:(h + 1) * 128], self.onesb[:, 0:1])] +
                            [(qc[s][:, h * 4 + dt, :], nb[:, h * 4 + dt:h * 4 + dt + 1]) for dt in range(4)],
                            reads=[pre + f'sTb{s}', qck, 'onesb', pre + 'nb'], writes=['ps2'])
                for dt in range(4):
                    i = h * 4 + dt
                    db = 5 + dsr % 2
                    dsr += 1
                    dbk = f'ps{db}'
                    self.mm(ps[db][:], [(kt[s][:, i * 128:(i + 1) * 128], vh)], reads=[ktk, vck], writes=[dbk])
                    self.stt(S[:, i, :], S[:, i, :], rhon[:, c * 4 + h:c * 4 + h + 1], ps[db][:], ALU.mult, ALU.add,
                             reads=[dbk, gk, pre + f'S{i}'], writes=[pre + f'S{i}'])
                    if not summary_only:
                        self.cp('act', Sb[:, i, :], S[:, i, :], reads=[pre + f'S{i}'], writes=[pre + f'Sb{i}'])
                self.mms([(ps[2][:, h * 4 + dt:h * 4 + dt + 1], [(kt[s][:, (h * 4 + dt) * 128:(h * 4 + dt + 1) * 128],
                                                                  self.onesb[:, 0:1])]) for dt in range(4)],
                         reads=[ktk, 'onesb'], writes=['ps2'])
                self.stt(nst[:, h * 4:(h + 1) * 4], nst[:, h * 4:(h + 1) * 4], rhon[:, c * 4 + h:c * 4 + h + 1],
                         ps[2][:, h * 4:(h + 1) * 4], ALU.mult, ALU.add, reads=['ps2', gk, pre + 'sumn'],
                         writes=[pre + 'sumn'])
                if not summary_only:
                    self.cp('dve', nb[:, h * 4:(h + 1) * 4], nst[:, h * 4:(h + 1) * 4], reads=[pre + 'sumn'],
                            writes=[pre + 'nb'])
                    p2 = self.head_out_m(pre, c, s, h, ps[nb_], nbk, sm, invlam, hn[h % 2], pre + f'hn{h % 2}',
                                         szc[s], ucc[s], yc[s], nw, skp, y_d)
                    if pending is not None:
                        pending()
                    pending = p2
        if pending is not None:
            pending()

    def head_out_m(self, pre, c, s, h, numb, nbk, sm, invlam, hn, hnk, szc, ucc, yc, nw, skp, y_d):
        ps = self.ps
        gk = pre + 'gs'
        smk = pre + 'sm'
        v = sm[:, h, :]
        self.cp('dve', v[:, 14:15], ps[2][:, 64 + h:64 + h + 1], reads=['ps2'], writes=[smk])
        self.ts('dve', v[:, 0:1], v[:, 14:15], -1.0, v[:, 14:15], ALU.mult, ALU.max, reads=[smk], writes=[smk])
        self.tt('dve', v[:, 0:1], v[:, 0:1], invlam[:, c * 4 + h:c * 4 + h + 1], ALU.max, reads=[smk, gk], writes=[smk])
        self.P.add('dve', lambda e: e.reciprocal(out=v[:, 1:2], in_=v[:, 0:1]), [smk], [smk])
        self.P.add('dve', lambda e: e.bn_stats(out=v[:, 2:8], in_=numb[:]), [nbk], [smk])
        self.P.add('dve', lambda e: e.bn_aggr(out=v[:, 8:10], in_=v[:, 2:8]), [smk], [smk])
        self.ts('dve', v[:, 10:11], v[:, 9:10], v[:, 1:2], v[:, 1:2], ALU.mult, ALU.mult, reads=[smk], writes=[smk])
        self.ts('dve', v[:, 10:11], v[:, 10:11], EPS, None, ALU.add, reads=[smk], writes=[smk])
        self.act(v[:, 10:11], v[:, 10:11], AF.Sqrt, reads=[smk], writes=[smk])
        self.P.add('dve', lambda e: e.reciprocal(out=v[:, 11:12], in_=v[:, 10:11]), [smk], [smk])
        self.tt('dve', v[:, 12:13], v[:, 11:12], v[:, 1:2], ALU.mult, reads=[smk], writes=[smk])
        self.ts('dve', v[:, 13:14], v[:, 8:9], v[:, 12:13], -1.0, ALU.mult, ALU.mult, reads=[smk], writes=[smk])
        self.act(hn, numb[:], AF.Identity, reads=[nbk, smk], writes=[hnk], bias=v[:, 13:14], scale=v[:, 12:13])

        def part2():
            self.transposes([(ps[7][:, i * 128:(i + 1) * 128], hn[:, i * 128:(i + 1) * 128]) for i in range(4)],
                            self.ident, reads=[hnk, 'cst'], writes=['ps7'])
            yk = pre + f'yc{s}'
            for i in range(4):
                ft = h * 4 + i
                tmp = self.gt[(h * 4 + i) % 2]
                tk = pre + f'gt{(h * 4 + i) % 2}'
                self.act(tmp, ps[7][:, i * 128:(i + 1) * 128], AF.Copy, reads=['ps7', pre + 'vec'], writes=[tk],
                         scale=nw[:, ft:ft + 1])
                self.stt(tmp, ucc[:, ft, :], skp[:, ft:ft + 1], tmp, ALU.mult, ALU.add,
                         reads=[tk, pre + f'ucc{s}', pre + 'vec'], writes=[tk])
                self.tt('dve', yc[:, ft, :], tmp, szc[:, ft, :], ALU.mult, reads=[tk, pre + f'szc{s}'], writes=[yk])
            if h == H - 1:
                self.dma('pool', y_d[c], yc[:].rearrange("p a b -> p (a b)"), reads=[yk], writes=[pre + 'y_d'])
        return part2

    def stage_B(self, pre, mix):
        A = self.A
        ps = self.ps
        IN, OUT = "ExternalInput", "ExternalOutput"
        ntile = NFT if mix == 'm' else NG
        nsm = 20 if mix == 'm' else 8
        v_d = self.dram(pre + "v", [NT, 128, E], BF16, IN)
        sz_d = self.dram(pre + "sz", [NT, 128, NFT * 128], BF16, IN)
        sumS_all = self.dram(pre + "sumS_all", [NCORES, ntile * 128, DH], F32, IN)
        sumn_all = self.dram(pre + "sumn_all", [128, NCORES * nsm], F32, IN)
        wout_d = self.dram(pre + "w_out", [E, D], F32, IN)
        postw_d = self.dram(pre + "post_w", [1, D], F32, IN)
        xo_d = self.dram(pre + "xo", [TL, D], F32, OUT)
        y_d = self.dram(pre + "y", [NT, 128, NFT * 128], BF16, "Internal")
        self._sz_d = sz_d
        if mix == 'm':
            qT_d = self.dram(pre + "qT", [NT, 128, NFT * 128], BF16, IN)
            kT_d = self.dram(pre + "kT", [NT, 128, NFT * 128], BF16, IN)
            uc_d = self.dram(pre + "uc", [NT, 128, NFT * 128], BF16, IN)
            gsc_d = self.dram(pre + "gsc", [128, GSC_M], F32, IN)
            vec_d = self.dram(pre + "vec", [128, NVM], F32, IN)
            self._uc_d = uc_d
            nvec = NVM
        else:
            qg_d = self.dram(pre + "qg", [NT, 128, NG * 128], BF16, IN)
            kg_d = self.dram(pre + "kg", [NT, 128, NG * 128], BF16, IN)
            kh_d = self.dram(pre + "kh", [NT, 128, NG * 128], BF16, IN)
            egl_d = self.dram(pre + "egl", [128, NG * NT], F32, IN)
            vec_d = self.dram(pre + "vec", [128, NVG], F32, IN)
            nvec = NVG
        vec = A.alloc([nvec], F32)
        self.dma('sp', vec, vec_d, writes=[pre + 'vec'])
        wo_early = None
        if mix == 'g':
            wo_early = A.alloc([NFT, D], BF16)
            for q4 in range(4):
                self.dma('pool', wo_early[:, q4 * 4:(q4 + 1) * 4, :],
                         wout_d[q4 * 512:(q4 + 1) * 512, :].rearrange("(k p) d -> p k d", p=128), writes=[pre + 'wo'])
        gk = pre + 'gs'
        if mix == 'm':
            gsc = A.alloc([GSC_M], F32)
            self.dma('sp', gsc, gsc_d, writes=[gk])
            nw = vec[:, 88:104]
            skp = vec[:, 104:120]
        else:
            egl = A.alloc([NG, NT], F32)
            self.dma('sp', egl[:].rearrange("p a b -> p (a b)"), egl_d, writes=[gk])
            nw = vec[:, 16:32]
        S = A.alloc([ntile, DH], F32)
        Sb = A.alloc([ntile, DH], BF16)
        sna = A.alloc([NCORES, nsm], F32)
        mj = A.alloc([NCORES, 8], F32)
        self.dma('sp', sna[:].rearrange("p a b -> p (a b)"), sumn_all, writes=[pre + 'sna'])
        nd = 4 if mix == 'm' else 8
        dec = sna[:, :, 16:20] if mix == 'm' else sna[:, :, 0:8]
        self.act(mj[:, :, 0:nd], dec, AF.Exp, reads=[pre + 'sna'], writes=[pre + 'mj'],
                 scale=(-1.0 if mix == 'm' else -1.0 / TAU))
        self.ts('dve', mj[:, :, 0:nd], mj[:, :, 0:nd], -1.0, None, ALU.add, reads=[pre + 'mj'], writes=[pre + 'mj'])
        for j in range(NCORES):
            self.ts('dve', mj[:, j, 0:nd], mj[:, j, 0:nd], self.sel[:, j:j + 1], 1.0, ALU.mult, ALU.add,
                    reads=[pre + 'mj', 'cst'], writes=[pre + 'mj'])
        Skeys = [pre + f'S{i}' for i in range(ntile)]
        self.memset('dve', S, 0.0, writes=Skeys)
        if mix == 'm':
            nst = A.alloc([16], F32)
            nb = A.alloc([16], BF16)
            self.memset('dve', nst, 0.0, writes=[pre + 'sumn'])
        m0 = A.mark()
        gld = [A.alloc([4, DH], F32) for _ in range(2)]
        nl = 0
        for j in range(NCORES - 1):
            for q4 in range(ntile // 4):
                g = gld[nl % 2]
                gkey = pre + f'gld{nl % 2}'
                nl += 1
                self.dma('sp', g, sumS_all[j, q4 * 512:(q4 + 1) * 512, :].rearrange("(a p) d -> p a d", p=128),
                         writes=[gkey])
                for a4 in range(4):
                    i = q4 * 4 + a4
                    hh = (i // 4) if mix == 'm' else i
                    self.act(g[:, a4, :], g[:, a4, :], AF.Copy, reads=[gkey, 'cst'], writes=[gkey],
                             scale=self.sel[:, j:j + 1])
                    self.stt(S[:, i, :], S[:, i, :], mj[:, j, hh:hh + 1], g[:, a4, :], ALU.mult, ALU.add,
                             reads=[gkey, pre + 'mj', Skeys[i]], writes=[Skeys[i]])
            if mix == 'm':
                for h in range(H):
                    self.ts('dve', sna[:, j, h * 4:(h + 1) * 4], sna[:, j, h * 4:(h + 1) * 4], self.sel[:, j:j + 1],
                            None, ALU.mult, reads=[pre + 'sna', 'cst'], writes=[pre + 'sna'])
                    self.stt(nst[:, h * 4:(h + 1) * 4], nst[:, h * 4:(h + 1) * 4], mj[:, j, h:h + 1],
                             sna[:, j, h * 4:(h + 1) * 4], ALU.mult, ALU.add, reads=[pre + 'sna', pre + 'mj', pre + 'sumn'],
                             writes=[pre + 'sumn'])
        A.release(m0)
        self.P.barrier()
        if mix == 'm':
            rho = gsc[:, 192:256]
            for i in range(ntile):
                h = i // 4
                self.ts('dve', S[:, i, :], S[:, i, :], rho[:, h:h + 1], None, ALU.mult, reads=[Skeys[i], gk],
                        writes=[Skeys[i]])
            for h in range(H):
                self.ts('dve', nst[:, h * 4:(h + 1) * 4], nst[:, h * 4:(h + 1) * 4], rho[:, h:h + 1], None, ALU.mult,
                        reads=[pre + 'sumn', gk], writes=[pre + 'sumn'])
            self.cp('dve', nb, nst, reads=[pre + 'sumn'], writes=[pre + 'nb'])
        for i in range(ntile):
            self.cp('act', Sb[:, i, :], S[:, i, :], reads=[Skeys[i]], writes=[pre + f'Sb{i}'])

        m1 = A.mark()
        szc = [A.alloc([NFT, 128], BF16) for _ in range(2)]
        yc = [A.alloc([NFT, 128], BF16) for _ in range(2)]
        self.gt = [A.alloc([128], F32) for _ in range(2)]
        if mix == 'm':
            ucc = [A.alloc([NFT, 128], BF16) for _ in range(2)]
            self.scan_m(pre, kT_d, v_d, qT_d, gsc, S, Sb, nst, nb, summary_only=False,
                        extra=(szc, ucc, yc, y_d, nw, skp))
        else:
            self.scan_g(pre, qg_d, kg_d, kh_d, v_d, egl, S, Sb, summary_only=False, extra=(szc, yc, y_d, nw))
        A.release(m1)
        self.P.barrier()

        if wo_early is not None:
            wo = wo_early
        else:
            wo = A.alloc([NFT, D], BF16)
            for q4 in range(4):
                self.dma('pool', wo[:, q4 * 4:(q4 + 1) * 4, :],
                         wout_d[q4 * 512:(q4 + 1) * 512, :].rearrange("(k p) d -> p k d", p=128), writes=[pre + 'wo'])
        pw = A.alloc([D], F32)
        self.dma('sp', pw, bass.AP(postw_d.tensor, 0, [[0, 128], [1, D]]), writes=[pre + 'pw'])
        yl = [A.alloc([NFT, 128], BF16) for _ in range(4)]
        tmp = [A.alloc([512], F32) for _ in range(8)]
        junk = A.alloc([512], BF16)
        s2 = A.alloc([NT, 4], F32)
        fin = []
        pbanks = [(ps[3][:], 'ps3'), (ps[4][:], 'ps4'), (ps[5][:], 'ps5'), (ps[6][:], 'ps6'),
                  (ps[0][:], 'ps0'), (ps[7][:], 'ps7'), (ps[2][:], 'ps2'), (self.psb[:].bitcast(F32), 'psb')]
        for c in range(NT):
            s = c % 4
            self.dma('sp', yl[s][:].rearrange("p a b -> p (a b)"), y_d[c], reads=[pre + 'y_d'], writes=[pre + f'yl{s}'])
            for half in range(2):
                bap, bkey = pbanks[2 * s + half]
                self.mm(bap, [(yl[s][:, ft, :], wo[:, ft, half * 512:(half + 1) * 512]) for ft in range(NFT)],
                        reads=[pre + f'yl{s}', pre + 'wo'], writes=[bkey])
                self.act(junk, bap, AF.Square, reads=[bkey], writes=[pre + f's2_{c}'],
                         accum=s2[:, c, half:half + 1])
            sk2 = pre + f's2_{c}'
            self.tt('dve', s2[:, c, 2:3], s2[:, c, 0:1], s2[:, c, 1:2], ALU.add, reads=[sk2], writes=[sk2])
            self.ts('dve', s2[:, c, 2:3], s2[:, c, 2:3], 1.0 / D, EPS, ALU.mult, ALU.add, reads=[sk2], writes=[sk2])
            self.act(s2[:, c, 2:3], s2[:, c, 2:3], AF.Sqrt, reads=[sk2], writes=[sk2])
            self.P.add('dve', lambda e, c=c: e.reciprocal(out=s2[:, c, 3:4], in_=s2[:, c, 2:3]), [sk2], [sk2])
            for half in range(2):
                bap, bkey = pbanks[2 * s + half]
                t_ = tmp[2 * s + half]
                tkey = pre + f'tmp{2 * s + half}'
                self.stt(t_, bap, s2[:, c, 3:4], pw[:, half * 512:(half + 1) * 512], ALU.mult, ALU.mult,
                         reads=[bkey, sk2, pre + 'pw'], writes=[tkey])
                self.tt('dve', self.x[:, c, half * 512:(half + 1) * 512], self.x[:, c, half * 512:(half + 1) * 512], t_,
                        ALU.add, reads=[tkey, f'x{c}'], writes=[f'x{c}'])
            fin.append(self.dma('pool', xo_d[c * 128:(c + 1) * 128, :], self.x[:, c, :], reads=[f'x{c}'],
                                writes=[pre + 'xo_d']))
        return fin

    def stage_A_g(self, pre):
        A = self.A
        ps = self.ps
        IN, OUT = "ExternalInput", "ExternalOutput"
        GW = 2 * 1024 + 2 * E + 16
        w_in = self.dram(pre + "w_in", [D, GW], F32, IN)
        wgu_d = self.dram(pre + "wgu", [16, 1024], F32, IN)
        vec_d = self.dram(pre + "vec", [128, NVG], F32, IN)
        qg_d = self.dram(pre + "qg", [NT, 128, NG * 128], BF16, OUT)
        kg_d = self.dram(pre + "kg", [NT, 128, NG * 128], BF16, OUT)
        kh_d = self.dram(pre + "kh", [NT, 128, NG * 128], BF16, OUT)
        v_d = self.dram(pre + "v", [NT, 128, E], BF16, OUT)
        sz_d = self.dram(pre + "sz", [NT, 128, NFT * 128], BF16, OUT)
        egl_d = self.dram(pre + "egl", [128, NG * NT], F32, OUT)
        sumS_d = self.dram(pre + "sumS", [NG * 128, DH], F32, OUT)
        sumn_d = self.dram(pre + "sumn", [128, 8], F32, OUT)

        vec = A.alloc([NVG], F32)
        pnw = vec[:, 0:8]
        nbg = vec[:, 8:16]
        self.dma('sp', vec, vec_d, writes=[pre + 'vec'])
        egl = A.alloc([NG, NT], F32)
        gseg = A.alloc([NG], F32)
        mP1 = A.mark()
        hT = A.alloc([KD, TL], BF16)
        self.prenorm(hT, pnw, [(self.x[:, t, :], f'x{t}') for t in range(NT)], NT, pre, pre + 'vec')
        hkeys = [pre + f'hT{t}' for t in range(NT)]
        wr = A.alloc([KD, 16], BF16)
        self.dma('pool', wr, w_in[:, GW - 16:GW].rearrange("(k p) c -> p k c", p=128), writes=[pre + 'wr'])
        wgu = A.alloc([1024], F32)
        self.dma('sp', wgu[0:16, :], wgu_d, writes=[pre + 'wgu'])
        r_sb = A.alloc([TL], F32)
        for st4 in range(4):
            self.mm(ps[2][0:16, :], [(wr[:, k, :], hT[:, k, st4 * 512:(st4 + 1) * 512]) for k in range(KD)],
                    reads=[pre + 'wr'] + hkeys[st4 * 4:(st4 + 1) * 4], writes=['ps2'])
            self.cp('act', r_sb[0:16, st4 * 512:(st4 + 1) * 512], ps[2][0:16, :], reads=['ps2'], writes=[pre + 'r'])
        spt = A.alloc([TL], F32)
        cs = A.alloc([TL], F32)
        ex = [A.alloc([TL], F32) for _ in range(3)]
        cl = A.alloc([NT], F32)
        ncl = A.alloc([NT], F32)
        wblk = [A.alloc([KD, 512], BF16) for _ in range(3)]
        wkeys = [pre + f'wblk{i}' for i in range(3)]
        srcs = [w_in[:, 0:512], w_in[:, 1024:1536], w_in[:, 512:1024], w_in[:, 1536:2048]] + \
            [w_in[:, 2048 + i * 512:2048 + (i + 1) * 512] for i in range(8)]
        ws = self.wstream(wblk, wkeys, srcs)
        stg = [A.alloc([TL], BF16) for _ in range(3)]
        vst = [A.alloc([512], BF16) for _ in range(2)]
        bank_rr = 0

        def next_bank():
            nonlocal bank_rr
            b = 3 + bank_rr % 4
            bank_rr += 1
            return b
        lq = math.log(GK ** -0.5)
        gk = pre + 'gs'
        for blk in range(2):
            ws.start(2 * blk)
            wqb, wqk_ = ws.get(2 * blk)
            wkb, wkk_ = ws.get(2 * blk + 1)
            for gi in range(4):
                g = blk * 4 + gi
                for st4 in range(4):
                    b = next_bank()
                    self.mm(ps[b][:], [(wgu[0:16, g * 128:(g + 1) * 128], r_sb[0:16, st4 * 512:(st4 + 1) * 512])],
                            reads=[pre + 'wgu', pre + 'r'], writes=[f'ps{b}'])
                    self.act(spt[:, st4 * 512:(st4 + 1) * 512], ps[b][:], AF.Exp, reads=[f'ps{b}', pre + 'vec'],
                             writes=[pre + 'spt'], bias=nbg[:, g:g + 1], scale=-1.0)
                self.act(spt, spt, AF.Ln, reads=[pre + 'spt'], writes=[pre + 'spt'], bias=1.0)
                for c in range(NT):
                    self.P.add('dve', lambda e, c=c: e.tensor_tensor_scan(
                        out=cs[:, c * 128:(c + 1) * 128], data0=self.ones, data1=spt[:, c * 128:(c + 1) * 128],
                        initial=0.0, op0=ALU.mult, op1=ALU.add), [pre + 'spt', 'cst'], [pre + 'cs'])
                self.cp('dve', cl[:].rearrange("p (c o) -> p c o", o=1), cs[:].rearrange("p (c t) -> p c t", t=128)[:, :, 127:128], reads=[pre + 'cs'],
                        writes=[pre + 'cl'])
                self.ts('dve', ncl, cl, -1.0 / TAU, None, ALU.mult, reads=[pre + 'cl'], writes=[pre + 'cl'])
                self.act(egl[:, g, :], cl, AF.Exp, reads=[pre + 'cl'], writes=[gk], scale=-1.0 / TAU)
                self.P.add('dve', lambda e, g=g: e.tensor_reduce(out=gseg[:, g:g + 1], in_=cl, axis=AX.X, op=ALU.add),
                           [pre + 'cl'], [pre + 'gseg'])
                self.act(ex[0], cs, AF.Exp, reads=[pre + 'cs'], writes=[pre + 'ex0'], scale=-1.0 / TAU, bias=lq)
                self.act(ex[1], cs, AF.Exp, reads=[pre + 'cs'], writes=[pre + 'ex1'], scale=1.0 / TAU)
                for c in range(NT):
                    self.act(ex[2][:, c * 128:(c + 1) * 128], cs[:, c * 128:(c + 1) * 128], AF.Exp,
                             reads=[pre + 'cs', pre + 'cl'], writes=[pre + 'ex2'], scale=1.0 / TAU, bias=ncl[:, c:c + 1])
                for st4 in range(4):
                    sl = slice(st4 * 512, (st4 + 1) * 512)
                    b = next_bank()
                    self.mm(ps[b][:], [(wqb[:, k, gi * 128:(gi + 1) * 128], hT[:, k, sl]) for k in range(KD)],
                            reads=[wqk_] + hkeys[st4 * 4:(st4 + 1) * 4], writes=[f'ps{b}'])
                    self.tt('dve', stg[0][:, sl], ps[b][:], ex[0][:, sl], ALU.mult, reads=[f'ps{b}', pre + 'ex0'],
                            writes=[pre + 'stg0'])
                    b = next_bank()
                    self.mm(ps[b][:], [(wkb[:, k, gi * 128:(gi + 1) * 128], hT[:, k, sl]) for k in range(KD)],
                            reads=[wkk_] + hkeys[st4 * 4:(st4 + 1) * 4], writes=[f'ps{b}'])
                    self.tt('dve', stg[1][:, sl], ps[b][:], ex[1][:, sl], ALU.mult, reads=[f'ps{b}', pre + 'ex1'],
                            writes=[pre + 'stg1'])
                    self.tt('dve', stg[2][:, sl], ps[b][:], ex[2][:, sl], ALU.mult, reads=[f'ps{b}', pre + 'ex2'],
                            writes=[pre + 'stg2'])
                for w_, dst in enumerate((qg_d, kg_d, kh_d)):
                    self.dma('pool', dst[:, :, g * 128:(g + 1) * 128].rearrange("c p t -> p c t"),
                             stg[w_][:].rearrange("p (c t) -> p c t", t=128), reads=[pre + f'stg{w_}'],
                             writes=[pre + f'qk_d{w_}'])
        self.outs.append(self.dma('sp', egl_d, egl[:].rearrange("p a b -> p (a b)"), reads=[gk], writes=[pre + 'egl_d']))
        self.outs.append(self.dma('sp', sumn_d, gseg, reads=[pre + 'gseg'], writes=[pre + 'sumn_d']))
        nblk = 0
        nv = 0
        for vb in range(4):
            ws.start(4 + vb)
            wb, wbk = ws.get(4 + vb)
            for tt in range(NT):
                b = next_bank()
                self.mm(ps[b][:], [(hT[:, k, tt * 128:(tt + 1) * 128], wb[:, k, :]) for k in range(KD)],
                        reads=[wbk, hkeys[tt]], writes=[f'ps{b}'])
                vs = vst[nv % 2]
                vk = pre + f'vst{nv % 2}'
                nv += 1
                self.evac(vs, ps[b][:], reads=[f'ps{b}'], writes=[vk])
                self.dma('pool', v_d[tt, :, vb * 512:(vb + 1) * 512], vs, reads=[vk], writes=[pre + 'v_d'])
        nstg = 0
        for zb in range(4):
            ws.start(8 + zb)
            wb, wbk = ws.get(8 + zb)
            for ft in range(4):
                sg = stg[nstg % 2]
                sk = pre + f'stg{nstg % 2}'
                nstg += 1
                for st4 in range(4):
                    b = next_bank()
                    self.mm(ps[b][:], [(wb[:, k, ft * 128:(ft + 1) * 128], hT[:, k, st4 * 512:(st4 + 1) * 512])
                                       for k in range(KD)], reads=[wbk] + hkeys[st4 * 4:(st4 + 1) * 4], writes=[f'ps{b}'])
                    self.act(sg[:, st4 * 512:(st4 + 1) * 512], ps[b][:], AF.Silu, reads=[f'ps{b}'], writes=[sk])
                tile_i = zb * 4 + ft
                self.dma('pool', sz_d[:, :, tile_i * 128:(tile_i + 1) * 128].rearrange("c p t -> p c t"),
                         sg[:].rearrange("p (c t) -> p c t", t=128), reads=[sk], writes=[pre + 'sz_d'])
        A.release(mP1)
        self.P.barrier()
        S = A.alloc([NG, DH], F32)
        self.memset('dve', S, 0.0, writes=[pre + f'S{i}' for i in range(NG)])
        self.scan_g(pre, None, None, kh_d, v_d, egl, S, None, summary_only=True)
        for i in range(NG):
            self.outs.append(self.dma('sp', sumS_d[i * 128:(i + 1) * 128, :], S[:, i, :], reads=[pre + f'S{i}'],
                                      writes=[pre + 'sumS_d']))

    def scan_g(self, pre, qg_d, kg_d, kh_d, v_d, egl, S, Sb, summary_only, extra=None):
        A = self.A
        ps = self.ps
        gk = pre + 'gs'
        khc = [A.alloc([NG, 128], BF16) for _ in range(2)]
        vc = [A.alloc([E], BF16) for _ in range(2)]
        kh = [A.alloc([1024], BF16) for _ in range(2)]
        if not summary_only:
            qc = [A.alloc([NG, 128], BF16) for _ in range(2)]
            kc = [A.alloc([NG, 128], BF16) for _ in range(2)]
            Ab = [A.alloc([512], BF16) for _ in range(2)]
            on = [A.alloc([DH], F32) for _ in range(2)]
            sm = A.alloc([H, 8], F32)
            junk = A.alloc([DH], F32)
            (szc, yc, y_d, nw) = extra
        dsr = 0
        pending = None
        for c in range(NT):
            s = c % 2
            khk, vck, kk = pre + f'khc{s}', pre + f'vc{s}', pre + f'kh{s}'
            self.dma('sp', khc[s][:].rearrange("p a b -> p (a b)"), kh_d[c], reads=[pre + 'qk_d2'], writes=[khk])
            self.dma('sp', vc[s], v_d[c], reads=[pre + 'v_d'], writes=[vck])
            if not summary_only:
                qck, kck = pre + f'qc{s}', pre + f'kc{s}'
                self.dma('sp', qc[s][:].rearrange("p a b -> p (a b)"), qg_d[c], reads=[pre + 'qk_d0'], writes=[qck])
                self.dma('sp', kc[s][:].rearrange("p a b -> p (a b)"), kg_d[c], reads=[pre + 'qk_d1'], writes=[kck])
                self.dma('sp', szc[s][:].rearrange("p a b -> p (a b)"), self._sz_d[c], reads=[pre + 'sz_d'],
                         writes=[pre + f'szc{s}'])
                self.mms([(ps[0][:, h * 128:(h + 1) * 128],
                           [(kc[s][:, h * 2 + dt, :], qc[s][:, h * 2 + dt, :]) for dt in range(2)]) for h in range(H)],
                         reads=[kck, qck], writes=['ps0'])
                self.tt('dve', Ab[s], ps[0][:], self.tri4, ALU.mult, reads=['ps0', 'cst'], writes=[pre + f'Ab{s}'])
            self.transposes([(self.psb[:, i * 128:(i + 1) * 128], khc[s][:, i, :]) for i in range(NG)], self.identb,
                            reads=[khk, 'identb'], writes=['psb'])
            self.cp('act', kh[s], self.psb[:], reads=['psb'], writes=[kk])
            for h in range(H):
                vh = vc[s][:, h * 512:(h + 1) * 512]
                if not summary_only:
                    nb_ = 3 + h % 2
                    nbk = f'ps{nb_}'
                    self.mm(ps[nb_][:], [(Ab[s][:, h * 128:(h + 1) * 128], vh)] +
                            [(qc[s][:, h * 2 + dt, :], Sb[:, h * 2 + dt, :]) for dt in range(2)],
                            reads=[pre + f'Ab{s}', vck, qck] + [pre + f'Sb{h * 2 + dt}' for dt in range(2)], writes=[nbk])
                for dt in range(2):
                    i = h * 2 + dt
                    db = 5 + dsr % 2
                    dsr += 1
                    dbk = f'ps{db}'
                    self.mm(ps[db][:], [(kh[s][:, i * 128:(i + 1) * 128], vh)], reads=[kk, vck], writes=[dbk])
                    self.stt(S[:, i, :], S[:, i, :], egl[:, i, c:c + 1], ps[db][:], ALU.mult, ALU.add,
                             reads=[dbk, gk, pre + f'S{i}'], writes=[pre + f'S{i}'])
                    if not summary_only:
                        self.cp('act', Sb[:, i, :], S[:, i, :], reads=[pre + f'S{i}'], writes=[pre + f'Sb{i}'])
                if not summary_only:
                    v = sm[:, h, :]
                    smk = pre + 'sm'
                    o = on[h % 2]
                    ok = pre + f'on{h % 2}'
                    self.act(junk, ps[nb_][:], AF.Square, reads=[nbk], writes=[pre + 'junk3', smk], accum=v[:, 0:1])
                    self.ts('dve', v[:, 1:2], v[:, 0:1], 1.0 / DH, EPS, ALU.mult, ALU.add, reads=[smk], writes=[smk])
                    self.act(v[:, 1:2], v[:, 1:2], AF.Sqrt, reads=[smk], writes=[smk])
                    self.P.add('dve', lambda e, v=v: e.reciprocal(out=v[:, 2:3], in_=v[:, 1:2]), [smk], [smk])
                    self.act(o, ps[nb_][:], AF.Copy, reads=[nbk, smk], writes=[ok], scale=v[:, 2:3])

                    def part2(c=c, s=s, h=h, o=o, ok=ok):
                        self.transposes([(ps[7][:, i * 128:(i + 1) * 128], o[:, i * 128:(i + 1) * 128])
                                         for i in range(4)], self.ident, reads=[ok, 'cst'], writes=['ps7'])
                        for i in range(4):
                            ft = h * 4 + i
                            self.stt(yc[s][:, ft, :], ps[7][:, i * 128:(i + 1) * 128], nw[:, ft:ft + 1],
                                     szc[s][:, ft, :], ALU.mult, ALU.mult,
                                     reads=['ps7', pre + 'vec', pre + f'szc{s}'], writes=[pre + f'yc{s}'])
                        if h == H - 1:
                            self.dma('pool', y_d[c], yc[s][:].rearrange("p a b -> p (a b)"), reads=[pre + f'yc{s}'],
                                     writes=[pre + 'y_d'])
                    if pending is not None:
                        pending()
                    pending = part2
        if pending is not None:
            pending()


def extra_sz(kb, pre):
    return kb._sz_d


def extra_uc(kb, pre):
    return kb._uc_d


_PROGS = {}


def get_prog(stages):
    key = tuple(stages)
    if key not in _PROGS:
        nc = bass.Bass("TRN2", target_bir_lowering=False)
        kb = KB(nc, list(stages))
        kb.build()
        _PROGS[key] = nc
    return _PROGS[key]


def _consts(core):
    c = np.zeros((128, CST_W), np.float32)
    c[:, 0:128] = np.eye(128, dtype=np.float32)
    tri = np.triu(np.ones((128, 128), np.float32))
    c[:, 128:640] = np.tile(tri, (1, 4))
    c[:, 640:768] = 1.0
    c[:, 768:776] = (np.arange(8) < core).astype(np.float32)[None, :]
    return c


def _cols(v, ntile):
    return np.ascontiguousarray(v.reshape(ntile, 128).T)


def _f(a):
    return np.ascontiguousarray(a, dtype=np.float32)


def kernel(x, pre_norm_w, post_norm_w,
           m_w_in, m_conv_w, m_conv_b, m_wq, m_wk, m_wv, m_w_gate, m_b_gate,
           m_norm_w, m_skip, m_w_out,
           g_w_in, g_w_gate_up, g_b_gate, g_norm_w, g_w_out, _depth=4, _debug=None):
    x = _f(x)
    xs = x.reshape(SEQ, D)
    cur = [np.ascontiguousarray(xs[c * TL:(c + 1) * TL]) for c in range(NCORES)]
    consts = [_consts(c) for c in range(NCORES)]

    def vec_m(i, j):
        v = np.zeros((128, NVM), np.float32)
        v[:, 0:8] = _cols(pre_norm_w[i], 8)
        for k in range(4):
            v[:, 8 + k * 16:8 + (k + 1) * 16] = _cols(m_conv_w[j, k], 16)
        v[:, 72:88] = _cols(m_conv_b[j], 16)
        v[:, 88:104] = _cols(m_norm_w[j], 16)
        v[:, 104:120] = _cols(m_skip[j], 16)
        return v

    def vec_g(i, j):
        v = np.zeros((128, NVG), np.float32)
        v[:, 0:8] = _cols(pre_norm_w[i], 8)
        v[:, 8:16] = -_cols(g_b_gate[j], 8)
        v[:, 16:32] = _cols(g_norm_w[j], 16)
        return v

    def a_inputs(i, pre, c, full_x):
        j = i // 2
        d = {}
        if i % 2 == 0:
            xh = np.zeros((128, D), np.float32)
            if c > 0:
                xh[0:3] = full_x[c * TL - 3:c * TL]
            d.update({pre + "w_in": _f(m_w_in[j]), pre + "wq": _f(m_wq[j]), pre + "wk": _f(m_wk[j]),
                      pre + "wv": _f(m_wv[j]), pre + "wvT": _f(np.transpose(m_wv[j], (0, 2, 1))),
                      pre + "wg": _f(m_w_gate[j].reshape(48, 128, 8).transpose(1, 0, 2).reshape(128, 384)),
                      pre + "bg": _f(m_b_gate[j].reshape(1, 8)), pre + "vec": vec_m(i, j), pre + "xh": xh})
        else:
            d.update({pre + "w_in": _f(g_w_in[j]), pre + "wgu": _f(g_w_gate_up[j]), pre + "vec": vec_g(i, j)})
        return d

    def b_inputs(i, pre, c, aout, sumS_all, sumn_all):
        j = i // 2
        d = {pre + "sumS_all": sumS_all, pre + "sumn_all": sumn_all,
             pre + "post_w": _f(post_norm_w[i].reshape(1, D))}
        if i % 2 == 0:
            d.update({pre + "w_out": _f(m_w_out[j]), pre + "vec": vec_m(i, j)})
            names = ["qT", "kT", "v", "sz", "uc", "gsc"]
        else:
            d.update({pre + "w_out": _f(g_w_out[j]), pre + "vec": vec_g(i, j)})
            names = ["qg", "kg", "kh", "v", "sz", "egl"]
        for n in names:
            d[pre + n] = aout[c][n]
        return d

    def grab(res, pre, i):
        names = (["qT", "kT", "v", "sz", "uc", "gsc"] if i % 2 == 0 else ["qg", "kg", "kh", "v", "sz", "egl"]) + \
            ["sumS", "sumn"]
        return [{n: res.results[c][pre + n] for n in names} for c in range(NCORES)]

    def launch(stages, maps):
        nc = get_prog(stages)
        return run_bass_kernel_spmd(nc, maps, core_ids=list(range(NCORES)))

    def gathered(aout):
        sumS_all = np.ascontiguousarray(np.stack([aout[c]["sumS"] for c in range(NCORES)], axis=0))
        sumn_all = np.ascontiguousarray(np.concatenate([aout[c]["sumn"] for c in range(NCORES)], axis=1))
        return sumS_all, sumn_all

    dbg = {}
    plan = [[('A', 0)]]
    for i in range(_depth):
        if i + 1 < _depth and (i + 1) % 2 == 1:
            plan.append([('B', i), ('A', i + 1)])
        else:
            plan.append([('B', i)])
            if i + 1 < _depth:
                plan.append([('A', i + 1)])
    aout = None
    pend = None
    for stages in plan:
        full_x = np.concatenate(cur, axis=0)
        prog = tuple((k, 'm' if i % 2 == 0 else 'g') for k, i in stages)
        if any(k == 'B' for k, _ in stages):
            sumS_all, sumn_all = gathered(aout)
        maps = []
        for c in range(NCORES):
            d = {"cst": consts[c], "x": cur[c]}
            for si, (k, i) in enumerate(stages):
                pre = f"s{si}_"
                if k == 'A':
                    d.update(a_inputs(i, pre, c, full_x))
                else:
                    d.update(b_inputs(i, pre, c, aout, sumS_all, sumn_all))
            maps.append(d)
        res = launch(prog, maps)
        for si, (k, i) in enumerate(stages):
            pre = f"s{si}_"
            if k == 'A':
                aout = grab(res, pre, i)
                if _debug is not None:
                    dbg[f"A{i}"] = aout
            else:
                cur = [res.results[c][pre + "xo"] for c in range(NCORES)]
    out = np.concatenate(cur, axis=0).reshape(1, SEQ, D).astype(np.float32)
    if _debug is not None:
        _debug.update(dbg)
    return out
```
